# Optimizing a Trainium2 kernel written in Bass

```python
import math
import jax
import jax.numpy as jnp
from jax import lax
import numpy as np

D_MODEL = 1024
BATCH = 8
SEQ = 2048
DEPTH = 4

HEAD_DIM = 64
CONV_CH = 512
CONV_K = 3
FOX_HEADS = 8
FOX_WIDTH = FOX_HEADS * HEAD_DIM
EVEN_IN = 3 * CONV_CH + 3 * FOX_WIDTH + FOX_HEADS
Q_BLOCK = 128
NSA_GROUPS = 4
NSA_HPG = 4
NSA_HEADS = NSA_GROUPS * NSA_HPG
NSA_WIDTH = NSA_HEADS * HEAD_DIM
NSA_KV = NSA_GROUPS * HEAD_DIM
CMP_LEN = 32
CMP_STRIDE = 16
SLC_LEN = 64
SLC_TOPN = 16
WINDOW = 512
SLC_QCHUNK = 16
ODD_IN = NSA_WIDTH + 6 * NSA_KV + 3 * NSA_HEADS
REL_BUCKETS = 32
REL_MAX_DIST = 128
PEER_HEADS = 8
PEER_NKEYS = 128
PEER_EXPERTS = PEER_NKEYS * PEER_NKEYS
PEER_QDIM = 128
PEER_TOPK = 16
PEER_CHUNK = 128
N_EVEN = (DEPTH + 1) // 2
N_ODD = DEPTH // 2
EPS = 1e-6
NEG_INF = -1e30
FORCE_SCORE = 1e6

kernel_name = 'hybrid_conv_fox_nsa_peer_block'


def rms_norm(x, g):
    xf = x.astype(jnp.float32)
    y = xf * lax.rsqrt(jnp.mean(xf * xf, axis=-1, keepdims=True) + EPS)
    return (y * g.astype(jnp.float32)).astype(x.dtype)


def modulate(h, shift, scale):
    return h * (1 + scale[:, None, :]) + shift[:, None, :]


def masked_softmax(s, mask):
    s = jnp.where(mask, s.astype(jnp.float32), NEG_INF)
    return jax.nn.softmax(s, axis=-1) * mask


def rel_bucket(dist):
    n = jnp.maximum(dist, 0)
    max_exact = REL_BUCKETS // 2
    nf = jnp.maximum(n, 1).astype(jnp.float32)
    large = max_exact + (jnp.log(nf / max_exact) / math.log(REL_MAX_DIST / max_exact)
                         * (REL_BUCKETS - max_exact)).astype(jnp.int32)
    large = jnp.minimum(large, REL_BUCKETS - 1)
    return jnp.where(n < max_exact, n, large)


def short_conv(u, w):
    return lax.conv_general_dilated(
        u, w.astype(u.dtype)[:, None, :], window_strides=(1,), padding=((CONV_K - 1, 0),),
        dimension_numbers=('NWC', 'WIO', 'NWC'), feature_group_count=u.shape[-1])


def fox_attention(q, k, v, logf):
    B, S, H, Dh = q.shape
    nb = S // Q_BLOCK
    cum_t = jnp.cumsum(logf, axis=1).transpose(0, 2, 1)
    kpos = jnp.arange(S)
    scale = Dh ** -0.5
    qb = q.reshape(B, nb, Q_BLOCK, H, Dh).swapaxes(0, 1)
    cb = cum_t.reshape(B, H, nb, Q_BLOCK).transpose(2, 0, 1, 3)
    tb = kpos.reshape(nb, Q_BLOCK)

    def block(args):
        qi, ci, ti = args
        s = jnp.einsum('bqhd,bkhd->bhqk', qi, k).astype(jnp.float32) * scale
        s = s + (ci[..., None] - cum_t[:, :, None, :])
        p = masked_softmax(s, ti[:, None] >= kpos[None, :])
        return jnp.einsum('bhqk,bkhd->bqhd', p.astype(v.dtype), v)

    out = lax.map(block, (qb, cb, tb))
    return out.swapaxes(0, 1).reshape(B, S, H, Dh)


def conv_fox_mixer(h, w_in, b_f, conv_w, q_g, k_g, w_out):
    B, S, _ = h.shape
    splits = list(np.cumsum([CONV_CH] * 3 + [FOX_WIDTH] * 3))
    cb, cc, ch, q, k, v, f = jnp.split(h @ w_in, splits, axis=-1)
    conv_out = cb * short_conv(cc * ch, conv_w)
    q = rms_norm(q.reshape(B, S, FOX_HEADS, HEAD_DIM), q_g)
    k = rms_norm(k.reshape(B, S, FOX_HEADS, HEAD_DIM), k_g)
    v = v.reshape(B, S, FOX_HEADS, HEAD_DIM)
    logf = jax.nn.log_sigmoid((f + b_f).astype(jnp.float32))
    attn = fox_attention(q, k, v, logf).reshape(B, S, FOX_WIDTH)
    return jnp.concatenate([conv_out, attn], axis=-1) @ w_out


def nsa_mixer(h, w_in, b_gate, q_g, k_g, cmp_pos, cmp_w1, cmp_w2, rel_table, w_out):
    B, S, _ = h.shape
    G, J, Dh = NSA_GROUPS, NSA_HPG, HEAD_DIM
    f32 = jnp.float32
    splits = list(np.cumsum([NSA_WIDTH] + [NSA_KV] * 6))
    q, kc, vc, ks, vs, kw, vw, gl = jnp.split(h @ w_in, splits, axis=-1)
    q = rms_norm(q.reshape(B, S, G, J, Dh), q_g)
    kc, vc, ks, vs, kw, vw = [a.reshape(B, S, G, Dh) for a in (kc, vc, ks, vs, kw, vw)]
    ks = rms_norm(ks, k_g[1])
    kw = rms_norm(kw, k_g[2])
    gates = jax.nn.sigmoid((gl + b_gate).astype(f32)).reshape(B, S, G, J, 3).astype(h.dtype)
    tpos = jnp.arange(S)
    table = rel_table.reshape(REL_BUCKETS, G, J)
    scale = Dh ** -0.5

    n_cmp = (S - CMP_LEN) // CMP_STRIDE + 1
    starts = jnp.arange(n_cmp) * CMP_STRIDE
    ends = starts + CMP_LEN - 1
    tok = starts[:, None] + jnp.arange(CMP_LEN)[None, :]

    def compress(a, pos, w1, w2):
        blk = a[:, tok] + pos[:, None, :]
        blk = blk.transpose(0, 1, 3, 2, 4).reshape(B, n_cmp, G, CMP_LEN * Dh)
        return jax.nn.gelu(blk @ w1) @ w2

    Kc = rms_norm(compress(kc, cmp_pos[0], cmp_w1[0], cmp_w2[0]), k_g[0])
    Vc = compress(vc, cmp_pos[1], cmp_w1[1], cmp_w2[1])
    s_c = jnp.einsum('bsgjd,bngd->bgjsn', q, Kc).astype(f32) * scale
    s_c = s_c + table[rel_bucket(tpos[:, None] - ends[None, :])].transpose(2, 3, 0, 1)
    p_c = masked_softmax(s_c, ends[None, :] <= tpos[:, None])
    o_c = jnp.einsum('bgjsn,bngd->bsgjd', p_c.astype(Vc.dtype), Vc)

    n_slc = S // SLC_LEN
    top_n = min(SLC_TOPN, n_slc)
    blk_ids = jnp.arange(n_slc)
    bstart = blk_ids * SLC_LEN
    overlap = ((starts[:, None] < bstart[None, :] + SLC_LEN)
               & (starts[:, None] + CMP_LEN > bstart[None, :])).astype(f32)
    imp = jnp.einsum('bgjsn,nm->bgsm', p_c, overlap)
    cur = tpos // SLC_LEN
    forced = (blk_ids[None, :] == 0) | (blk_ids[None, :] == cur[:, None]) | (blk_ids[None, :] == cur[:, None] - 1)
    imp = jnp.where(forced, FORCE_SCORE, jnp.where(bstart[None, :] <= tpos[:, None], imp, -1.0))
    sel_score, sel_idx = lax.top_k(imp, top_n)
    sel_valid = sel_score >= 0

    Ks = ks.reshape(B, n_slc, SLC_LEN, G, Dh).transpose(0, 3, 1, 2, 4)
    Vs = vs.reshape(B, n_slc, SLC_LEN, G, Dh).transpose(0, 3, 1, 2, 4)
    nq = S // SLC_QCHUNK
    bi = jnp.arange(B)[:, None, None, None]
    gi = jnp.arange(G)[None, :, None, None]

    def slc_chunk(args):
        qi, idx, val, ti = args
        Kg = Ks[bi, gi, idx]
        Vg = Vs[bi, gi, idx]
        kpos = idx[..., None] * SLC_LEN + jnp.arange(SLC_LEN)
        dist = ti[None, None, :, None, None] - kpos
        mask = val[..., None] & (dist >= 0)
        bias = jnp.moveaxis(table[rel_bucket(dist), gi[..., None]], -1, 2)
        s = jnp.einsum('bqgjd,bgqnld->bgjqnl', qi, Kg).astype(f32) * scale + bias
        sh = s.shape
        m = jnp.broadcast_to(mask[:, :, None], sh).reshape(sh[:4] + (-1,))
        p = masked_softmax(s.reshape(sh[:4] + (-1,)), m).reshape(sh)
        return jnp.einsum('bgjqnl,bgqnld->bqgjd', p.astype(Vg.dtype), Vg)

    qs = q.reshape(B, nq, SLC_QCHUNK, G, J, Dh).swapaxes(0, 1)
    idx_s = sel_idx.reshape(B, G, nq, SLC_QCHUNK, top_n).transpose(2, 0, 1, 3, 4)
    val_s = sel_valid.reshape(B, G, nq, SLC_QCHUNK, top_n).transpose(2, 0, 1, 3, 4)
    t_s = tpos.reshape(nq, SLC_QCHUNK)
    o_s = lax.map(slc_chunk, (qs, idx_s, val_s, t_s)).swapaxes(0, 1).reshape(B, S, G, J, Dh)

    kwp = jnp.pad(kw, ((0, 0), (WINDOW, 0), (0, 0), (0, 0)))
    vwp = jnp.pad(vw, ((0, 0), (WINDOW, 0), (0, 0), (0, 0)))
    nb = S // Q_BLOCK
    span = Q_BLOCK + WINDOW
    koff = jnp.arange(span)
    dist_w = jnp.arange(Q_BLOCK)[:, None] + WINDOW - koff[None, :]
    band = (dist_w >= 0) & (dist_w < WINDOW)
    bias_w = table[rel_bucket(dist_w)].transpose(2, 3, 0, 1)

    def win_block(args):
        qi, i = args
        start = i * Q_BLOCK
        kb = lax.dynamic_slice_in_dim(kwp, start, span, axis=1)
        vb = lax.dynamic_slice_in_dim(vwp, start, span, axis=1)
        s = jnp.einsum('bqgjd,bkgd->bgjqk', qi, kb).astype(f32) * scale + bias_w
        mask = band & ((start - WINDOW + koff) >= 0)[None, :]
        p = masked_softmax(s, mask)
        return jnp.einsum('bgjqk,bkgd->bqgjd', p.astype(vb.dtype), vb)

    qw = q.reshape(B, nb, Q_BLOCK, G, J, Dh).swapaxes(0, 1)
    o_w = lax.map(win_block, (qw, jnp.arange(nb))).swapaxes(0, 1).reshape(B, S, G, J, Dh)

    o = gates[..., 0:1] * o_c + gates[..., 1:2] * o_s + gates[..., 2:3] * o_w
    return o.reshape(B, S, NSA_WIDTH) @ w_out


def peer_ffn(h, w_q, sub_keys, u, v):
    B, S, D = h.shape
    T = B * S
    half = PEER_QDIM // 2
    hc = h.reshape(T // PEER_CHUNK, PEER_CHUNK, D)

    def chunk(xc):
        q = (xc @ w_q).reshape(-1, PEER_HEADS, 2, half)
        s = jnp.einsum('thpd,pkd->thpk', q, sub_keys).astype(jnp.float32)
        sv, si = lax.top_k(s, PEER_TOPK)
        cand = (sv[:, :, 0, :, None] + sv[:, :, 1, None, :]).reshape(-1, PEER_HEADS, PEER_TOPK * PEER_TOPK)
        cand_id = (si[:, :, 0, :, None] * PEER_NKEYS + si[:, :, 1, None, :]).reshape(-1, PEER_HEADS, PEER_TOPK * PEER_TOPK)
        cv, ci = lax.top_k(cand, PEER_TOPK)
        eid = jnp.take_along_axis(cand_id, ci, axis=-1)
        g = jax.nn.softmax(cv, axis=-1)
        ue = u[eid]
        ve = v[eid]
        a = jax.nn.gelu(jnp.einsum('td,thkd->thk', xc, ue)).astype(jnp.float32)
        return jnp.einsum('thk,thkd->td', (g * a).astype(ve.dtype), ve)

    return lax.map(chunk, hc).reshape(B, S, D)


def setup_inputs(seed: int = 0) -> dict:
    key = jax.random.key(seed)
    ks = jax.random.split(key, 24)
    D = D_MODEL

    def nrm(k, shape, s):
        return jax.random.normal(k, shape, jnp.float32) * s

    return {
        'x': nrm(ks[0], (BATCH, SEQ, D), 1.0),
        'c': nrm(ks[1], (BATCH, D), 1.0),
        'ada_w': nrm(ks[2], (DEPTH, D, 6 * D), 0.5 * D ** -0.5),
        'ada_b': nrm(ks[3], (DEPTH, 6 * D), 0.01),
        'norm_g': 1.0 + nrm(ks[4], (DEPTH, 2, D), 0.05),
        'even_w_in': nrm(ks[5], (N_EVEN, D, EVEN_IN), D ** -0.5),
        'even_b_f': jax.random.uniform(ks[6], (N_EVEN, FOX_HEADS), jnp.float32, 1.0, 6.0),
        'even_conv_w': nrm(ks[7], (N_EVEN, CONV_K, CONV_CH), CONV_K ** -0.5),
        'even_q_g': 1.0 + nrm(ks[8], (N_EVEN, HEAD_DIM), 0.05),
        'even_k_g': 1.0 + nrm(ks[9], (N_EVEN, HEAD_DIM), 0.05),
        'even_w_out': nrm(ks[10], (N_EVEN, CONV_CH + FOX_WIDTH, D), (CONV_CH + FOX_WIDTH) ** -0.5),
        'odd_w_in': nrm(ks[11], (N_ODD, D, ODD_IN), D ** -0.5),
        'odd_b_gate': nrm(ks[12], (N_ODD, 3 * NSA_HEADS), 0.01),
        'odd_q_g': 1.0 + nrm(ks[13], (N_ODD, HEAD_DIM), 0.05),
        'odd_k_g': 1.0 + nrm(ks[14], (N_ODD, 3, HEAD_DIM), 0.05),
        'odd_cmp_pos': nrm(ks[15], (N_ODD, 2, CMP_LEN, HEAD_DIM), 0.1),
        'odd_cmp_w1': nrm(ks[16], (N_ODD, 2, CMP_LEN * HEAD_DIM, HEAD_DIM), (CMP_LEN * HEAD_DIM) ** -0.5),
        'odd_cmp_w2': nrm(ks[17], (N_ODD, 2, HEAD_DIM, HEAD_DIM), HEAD_DIM ** -0.5),
        'odd_w_out': nrm(ks[18], (N_ODD, NSA_WIDTH, D), NSA_WIDTH ** -0.5),
        'rel_table': nrm(ks[19], (REL_BUCKETS, NSA_HEADS), 0.5),
        'peer_w_q': nrm(ks[20], (DEPTH, D, PEER_HEADS * PEER_QDIM), D ** -0.5),
        'peer_keys': nrm(ks[21], (DEPTH, 2, PEER_NKEYS, PEER_QDIM // 2), (PEER_QDIM // 2) ** -0.5),
        'peer_u': nrm(ks[22], (DEPTH, PEER_EXPERTS, D), D ** -0.5),
        'peer_v': nrm(ks[23], (DEPTH, PEER_EXPERTS, D), PEER_HEADS ** -0.5),
    }


def reference(x, c, ada_w, ada_b, norm_g, even_w_in, even_b_f, even_conv_w, even_q_g, even_k_g,
              even_w_out, odd_w_in, odd_b_gate, odd_q_g, odd_k_g, odd_cmp_pos, odd_cmp_w1, odd_cmp_w2,
              odd_w_out, rel_table, peer_w_q, peer_keys, peer_u, peer_v):
    mods = jnp.einsum('bd,lde->lbe', jax.nn.silu(c), ada_w) + ada_b[:, None, :]
    for layer in range(DEPTH):
        sh1, sc1, g1, sh2, sc2, g2 = jnp.split(mods[layer], 6, axis=-1)
        h = modulate(rms_norm(x, norm_g[layer, 0]), sh1, sc1)
        i = layer // 2
        if layer % 2 == 0:
            y = conv_fox_mixer(h, even_w_in[i], even_b_f[i], even_conv_w[i], even_q_g[i], even_k_g[i],
                               even_w_out[i])
        else:
            y = nsa_mixer(h, odd_w_in[i], odd_b_gate[i], odd_q_g[i], odd_k_g[i], odd_cmp_pos[i],
                          odd_cmp_w1[i], odd_cmp_w2[i], rel_table, odd_w_out[i])
        x = x + g1[:, None, :] * y
        h = modulate(rms_norm(x, norm_g[layer, 1]), sh2, sc2)
        x = x + g2[:, None, :] * peer_ffn(h, peer_w_q[layer], peer_keys[layer], peer_u[layer], peer_v[layer])
    return x
```

```python
import math
from contextlib import ExitStack
import numpy as np
import concourse.bass as bass
import concourse.mybir as mybir
from concourse.bass_utils import run_bass_kernel_spmd

F32 = mybir.dt.float32
BF16 = mybir.dt.bfloat16
AF = mybir.ActivationFunctionType
ALU = mybir.AluOpType
AX = mybir.AxisListType

S = 2048
D = 1024
NT = 16
BIG = 240000.0
SEM_ROT = 12000


def _conflict(a, b):
    n = min(len(a), len(b))
    return a[:n] == b[:n]


class _Eng:
    def __init__(self, P, name, eng, same_wait):
        self.P = P
        self.name = name
        self.eng = eng
        self.same_wait = same_wait
        self.sem = None
        self.count = 0
        self.waited = {}

    def new_sem(self):
        self.sem = self.P.alloc_sem(self.name)
        self.count = 0


class Prog:
    def __init__(self, nc, stack, n_dma_sems=4):
        self.nc = nc
        self.stack = stack
        self.nsem = 0
        self.engs = {}
        for name, eng, sw in (("pe", nc.tensor, False), ("dve", nc.vector, True),
                              ("act", nc.scalar, True), ("pool", nc.gpsimd, True),
                              ("sp", nc.sync, False)):
            e = _Eng(self, name, eng, sw)
            e.new_sem()
            self.engs[name] = e
        self.dq = {}
        for q in ("sp", "pool"):
            self.dq[q] = {"sems": [self.alloc_sem("d" + q) for _ in range(n_dma_sems)],
                          "vals": [0] * n_dma_sems, "n": 0}
        self.state = {}
        self.out_events = []
        self.ninst = 0

    def alloc_sem(self, name):
        self.nsem += 1
        return self.stack.enter_context(self.nc.semaphore("s%s%d" % (name, self.nsem)))

    def _deps(self, reads, writes):
        deps = []
        for k in reads:
            for k2, st in self.state.get(k[0], {}).items():
                if st[0] is not None and _conflict(k, k2):
                    deps.append(st[0])
        for k in writes:
            for k2, st in self.state.get(k[0], {}).items():
                if _conflict(k, k2):
                    if st[0] is not None:
                        deps.append(st[0])
                    deps.extend(st[1])
        return deps

    def _record(self, ev, reads, writes):
        for k in reads:
            d = self.state.setdefault(k[0], {})
            st = d.setdefault(k, [None, []])
            st[1] = [e for e in st[1] if e[0] is not ev[0]] + [ev]
        for k in writes:
            d = self.state.setdefault(k[0], {})
            for k2 in [k2 for k2 in d if len(k2) > len(k) and k2[:len(k)] == k]:
                del d[k2]
            d[k] = [ev, []]

    def _wait(self, E, deps):
        need = {}
        for (sem, val, owner) in deps:
            if owner is E and not E.same_wait:
                continue
            if E.waited.get(id(sem), 0) >= val:
                continue
            if need.get(id(sem), (None, 0))[1] < val:
                need[id(sem)] = (sem, val)
        for sem, val in need.values():
            E.eng.wait_ge(sem, val)
            E.waited[id(sem)] = val

    @staticmethod
    def _keys(ks):
        return [k if isinstance(k, tuple) else (k,) for k in ks]

    def op(self, engname, fn, reads=(), writes=()):
        E = self.engs[engname]
        reads = self._keys(reads)
        writes = self._keys(writes)
        self._wait(E, self._deps(reads, writes))
        if E.count >= SEM_ROT:
            E.new_sem()
        inst = fn(E.eng)
        inst.then_inc(E.sem, 1)
        E.count += 1
        self.ninst += 1
        ev = (E.sem, E.count, E)
        self._record(ev, reads, writes)
        return ev

    def dma(self, out, in_, reads=(), writes=(), q="sp", is_output=False, **kw):
        E = self.engs[q]
        Q = self.dq[q]
        reads = self._keys(reads)
        writes = self._keys(writes)
        i = Q["n"] % len(Q["sems"])
        Q["n"] += 1
        sem = Q["sems"][i]
        deps = self._deps(reads, writes)
        if Q["vals"][i] > 0:
            deps.append((sem, Q["vals"][i], None))
        self._wait(E, deps)
        inst = E.eng.dma_start(out=out, in_=in_, **kw)
        Q["vals"][i] += 16
        inst.then_inc(sem, 16)
        self.ninst += 1
        ev = (sem, Q["vals"][i], None)
        self._record(ev, reads, writes)
        if is_output:
            self.out_events.append(ev)
        return ev

    def _all_events(self):
        evs = []
        for e in self.engs.values():
            if e.count > 0:
                evs.append((e.sem, e.count, e))
        for Q in self.dq.values():
            for sem, v in zip(Q["sems"], Q["vals"]):
                if v > 0:
                    evs.append((sem, v, None))
        return evs

    def barrier(self):
        evs = self._all_events()
        for E in self.engs.values():
            self._wait(E, [ev for ev in evs if ev[2] is not E])
        self.state = {}

    def finish(self):
        E = self.engs["sp"]
        self._wait(E, self.out_events + [ev for ev in self._all_events() if ev[2] is not E])


def host_consts():
    c = {}
    dist = np.arange(2048)
    nf = np.maximum(dist, 1).astype(np.float32)
    large = 16 + (np.log(nf / np.float32(16)) / np.float32(math.log(8.0)) * np.float32(16)).astype(np.int32)
    large = np.minimum(large, 31)
    bucket = np.where(dist < 16, dist, large)
    ohb = np.zeros((32, 2048), np.float32)
    ohb[bucket, dist] = 1.0
    c["k_ohb"] = ohb
    p = np.arange(128)[:, None, None]
    i = np.arange(16)[None, :, None]
    m = np.arange(32)[None, None, :]
    t = 128 * i + p
    cur = t // 64
    forced = (m == 0) | (m == cur) | (m == cur - 1)
    allowed = (64 * m <= t)
    c["k_a1"] = (allowed & ~forced).astype(np.float32)
    c["k_a0"] = np.where(forced, 1e6, np.where(allowed, 0.0, -1.0)).astype(np.float32)
    mm = np.arange(32)[:, None, None]
    jj = np.arange(16)[None, :, None]
    sp = np.arange(128)[None, None, :]
    c["k_ej"] = (mm == 2 * jj + sp // 64).astype(np.float32)
    starts = (np.arange(127) * 16)[:, None]
    bstart = (np.arange(32) * 64)[None, :]
    c["k_ovl"] = ((starts < bstart + 64) & (starts + 32 > bstart)).astype(np.float32)
    s_ = np.arange(128)[:, None]
    t_ = np.arange(128)[None, :]
    c["k_caus"] = np.where(s_ > t_, -BIG, 0.0).astype(np.float32)
    sel8 = np.zeros((8, 8, 128), np.float32)
    for h in range(8):
        sel8[h, h, :] = 1.0
    c["k_sel8"] = sel8
    shm = np.zeros((128, 4, 128), np.float32)
    shm[:, 0, :] = (s_ == t_ - 1)
    shm[:, 1, :] = (s_ == t_ - 2)
    shm[127, 2, 0] = 1.0
    shm[126, 3, 0] = 1.0
    shm[127, 3, 1] = 1.0
    c["k_shm"] = shm
    return c


CONST_SHAPES = {"k_ohb": [32, 2048], "k_a1": [128, 16, 32], "k_a0": [128, 16, 32], "k_ej": [32, 16, 128],
                "k_ovl": [127, 32], "k_caus": [128, 128], "k_sel8": [8, 8, 128], "k_shm": [128, 4, 128]}

IN_SHAPES = {
    "x": [S, D], "c": [1, D], "ada_w": [4, D, 6 * D], "ada_b": [1, 4 * 6 * D], "norm_g": [1, 4 * 2 * D],
    "even_w_in": [2, D, 3080], "even_b_f": [2, 8], "even_conv_w": [2, 3 * 512], "even_q_g": [2, 64],
    "even_k_g": [2, 64], "even_w_out": [2, D, D], "odd_w_in": [2, D, 2608], "odd_b_gate": [2, 48],
    "odd_q_g": [2, 64], "odd_k_g": [2, 3 * 64], "odd_cmp_pos": [2, 2, 32, 64], "odd_cmp_w1": [2, 2, 2048, 64],
    "odd_cmp_w2": [2, 2, 64, 64], "odd_w_out": [2, D, D], "rel_table": [32, 16], "peer_w_q": [4, D, D],
    "peer_keys": [4, 2, 128, 64], "peer_ut": [4, D, 16384], "peer_v": [4, 16384, D],
}


class Builder:
    def __init__(self, n_layers=4, stop=None, peer=True, snaps=False):
        self.snaps = snaps
        self.snap_names = []
        self.n_layers = n_layers
        self.stop = stop
        self.do_peer = peer
        nc = self.nc = bass.Bass("TRN2", target_bir_lowering=False)
        self.I = {}
        for k, shp in list(IN_SHAPES.items()) + list(CONST_SHAPES.items()):
            self.I[k] = nc.dram_tensor(k, list(shp), F32, kind="ExternalInput").ap()
        self.y_out = nc.dram_tensor("y", [S, D], F32, kind="ExternalOutput").ap()
        self.MODS = nc.dram_tensor("mods_s", [4, 6, D], F32, kind="Internal").ap()
        self.FV = nc.dram_tensor("fv_s", [16, 4096], BF16, kind="Internal").ap()
        self.FW = nc.dram_tensor("fw_s", [16, 4096], BF16, kind="Internal").ap()
        self.UB = nc.dram_tensor("ub_s", [32, 128, 8, 512], BF16, kind="Internal").ap()
        self.VB = nc.dram_tensor("vb_s", [32, 128, 4, 1024], BF16, kind="Internal").ap()

    def T(self, st, name, shape, dt):
        self._tn = getattr(self, "_tn", 0) + 1
        return st.enter_context(self.nc.sbuf_tensor("%s_%d" % (name, self._tn), list(shape), dt))

    def mm(self, out, lhsT, rhs, start, stop, reads, writes, skip=False):
        self.P.op("pe", lambda e: e.matmul(out, lhsT, rhs, start=start, stop=stop, skip_group_check=skip),
                  reads=reads, writes=writes)

    def tr(self, out, in_, ident, reads, writes):
        self.P.op("pe", lambda e: e.transpose(out, in_, ident), reads=reads, writes=writes)

    def bank_bf(self, b):
        return self.B[b][:].bitcast(BF16)

    def build(self):
        nc = self.nc
        with ExitStack() as g:
            P = self.P = Prog(nc, g)
            self.B = [g.enter_context(nc.psum_tensor("B%d" % i, [128, 512], F32)) for i in range(8)]
            self.X = self.T(g, "X", [128, NT, D], F32)
            self.identf = self.T(g, "identf", [128, 128], F32)
            self.identb = self.T(g, "identb", [128, 128], BF16)
            self.antib = self.T(g, "antib", [128, 128], BF16)
            self.anti127 = self.T(g, "anti127", [128, 128], BF16)
            self.ones_row = self.T(g, "ones_row", [1, 128], BF16)
            self.one11 = self.T(g, "one11", [1, 1], F32)
            self.GB = self.T(g, "GB", [128, D], BF16)
            self.SHB = self.T(g, "SHB", [128, D], BF16)
            self.GTB = self.T(g, "GTB", [128, D], BF16)
            self.onec = self.T(g, "onec", [128, 1], F32)
            self.onesf = self.T(g, "onesf", [8, 128], F32)
            self.rd = self.T(g, "rd", [128, 32], F32)
            self.ntmp = self.T(g, "ntmp", [128, D], F32)
            self.hbf = self.T(g, "hbf", [128, D], BF16)
            self.hT = self.T(g, "hT", [128, 8, 128], BF16)
            self.sm = self.T(g, "sm", [128, 64], F32)
            tmpi = self.T(g, "tmpi", [128, 128], F32)

            P.op("pool", lambda e: e.iota(tmpi[:], [[1, 128]], base=0, channel_multiplier=-1,
                                          allow_small_or_imprecise_dtypes=True), writes=["tmpi"])
            P.op("dve", lambda e: e.tensor_scalar(self.identf[:], tmpi[:], 0.0, None, ALU.is_equal), reads=["tmpi"], writes=["identf"])
            P.op("dve", lambda e: e.tensor_scalar(self.identb[:], tmpi[:], 0.0, None, ALU.is_equal), reads=["tmpi"], writes=["identb"])
            P.op("pool", lambda e: e.iota(tmpi[:], [[1, 128]], base=-127, channel_multiplier=1,
                                          allow_small_or_imprecise_dtypes=True), reads=["identb", "identf"], writes=["tmpi"])
            P.op("dve", lambda e: e.tensor_scalar(self.antib[:], tmpi[:], 0.0, None, ALU.is_equal), reads=["tmpi"], writes=["antib"])
            P.op("dve", lambda e: e.tensor_scalar(self.anti127[:], tmpi[:], -1.0, None, ALU.is_equal), reads=["tmpi"], writes=["anti127"])
            P.op("pool", lambda e: e.memset(self.ones_row[:], 1.0), writes=["ones_row"])
            P.op("pool", lambda e: e.memset(self.one11[:], 1.0), writes=["one11"])
            P.op("pool", lambda e: e.memset(self.onec[:], 1.0), writes=["onec"])
            P.op("pool", lambda e: e.memset(self.onesf[:], 1.0), writes=["onesf"])

            for tb in range(NT):
                P.dma(self.X[:, tb, :], self.I["x"][tb * 128:(tb + 1) * 128, :], writes=[("X", tb)])

            self.adaln()
            P.barrier()
            done = False
            tables_ready = False
            for l in range(self.n_layers):
                if l % 2 == 0:
                    self.even_layer(l)
                else:
                    if not tables_ready:
                        self.rel_tables()
                        tables_ready = True
                    self.odd_layer(l)
                self.snapshot("xm_%d" % l)
                if self.stop == "L%dmix" % l:
                    break
                if self.do_peer:
                    self.peer_layer(l)
                    self.snapshot("x_%d" % l)
                if self.stop == "L%d" % l:
                    break
            P.barrier()
            for tb in range(NT):
                P.dma(self.y_out[tb * 128:(tb + 1) * 128, :], self.X[:, tb, :], reads=[("X", tb)], is_output=True)
            P.finish()
        return nc

    def snapshot(self, name):
        if not self.snaps:
            return
        t = self.nc.dram_tensor("snap_" + name, [S, D], F32, kind="ExternalOutput").ap()
        self.snap_names.append(name)
        for tb in range(NT):
            self.P.dma(t[tb * 128:(tb + 1) * 128, :], self.X[:, tb, :], reads=[("X", tb)], is_output=True)

    def adaln(self):
        P = self.P
        I = self.I
        with ExitStack() as st:
            crow = self.T(st, "crow", [1, D], F32)
            srow = self.T(st, "srow", [1, D], F32)
            scol = self.T(st, "scol", [128, 8], F32)
            brow = self.T(st, "brow", [1, 6 * D], F32)
            grow = self.T(st, "grow", [1, 2 * D], F32)
            mrow = self.T(st, "mrow", [1, 6 * D], F32)
            wts = [self.T(st, "adw%d" % k, [128, 8, 512], F32) for k in range(2)]
            P.dma(crow[:], I["c"], writes=["crow"])
            P.op("act", lambda e: e.activation(srow[:], crow[:], AF.Silu), reads=["crow"], writes=["srow"])
            ps = self.B[0]
            for dc in range(8):
                self.mm(ps[:, dc:dc + 1], srow[0:1, dc * 128:(dc + 1) * 128], self.one11[:], True, True,
                        ["srow", "one11"], [("B0", dc)])
            P.op("dve", lambda e: e.tensor_copy(scol[:], ps[:, 0:8]), reads=["B0"], writes=["scol"])
            n = 0
            for l in range(self.n_layers):
                P.dma(brow[:], I["ada_b"][0:1, l * 6144:(l + 1) * 6144], writes=["brow"])
                P.dma(grow[:], I["norm_g"][0:1, l * 2048:(l + 1) * 2048], writes=["grow"])
                for nt in range(12):
                    wt = wts[n % 2]
                    wk = "adw%d" % (n % 2)
                    P.dma(wt[:], I["ada_w"][l, :, nt * 512:(nt + 1) * 512].rearrange("(c p) n -> p c n", p=128), writes=[wk])
                    pb = self.B[1 + n % 2]
                    pk = "B%d" % (1 + n % 2)
                    for dc in range(8):
                        self.mm(pb[0:1, :], scol[:, dc:dc + 1], wt[:, dc, :], dc == 0, dc == 7, ["scol", wk], [pk])
                    P.op("dve", lambda e: e.tensor_tensor(mrow[0:1, nt * 512:(nt + 1) * 512], pb[0:1, :],
                                                          brow[0:1, nt * 512:(nt + 1) * 512], ALU.add),
                         reads=[pk, "brow"], writes=[("mrow", nt)])
                    n += 1
                for k in range(2):
                    sc = mrow[0:1, (3 * k + 1) * D:(3 * k + 2) * D]
                    ng = grow[0:1, k * D:(k + 1) * D]
                    P.op("dve", lambda e: e.scalar_tensor_tensor(sc, sc, 1.0, ng, ALU.add, ALU.mult), reads=["mrow", "grow"], writes=["mrow"])
                P.dma(self.MODS[l].rearrange("k d -> (k d)").unsqueeze(0), mrow[:], reads=["mrow"], writes=[("MODS", l)])

    def load_mods(self, l, k):
        P = self.P
        for j, (t, nm) in zip((1, 0, 2), ((self.GB, "GB"), (self.SHB, "SHB"), (self.GTB, "GTB"))):
            P.dma(t[:], self.MODS[l, 3 * k + j:3 * k + j + 1, :].broadcast_to([128, D]), reads=[("MODS", l)], writes=[nm], q="pool")

    def norm_hT(self, tb):
        P = self.P
        xt = self.X[:, tb, :]
        sm = self.sm
        P.op("act", lambda e: e.activation(self.ntmp[:], xt, AF.Square, accum_out=sm[:, 0:1]), reads=[("X", tb)], writes=["ntmp", ("sm", 0)])
        P.op("dve", lambda e: e.tensor_scalar(sm[:, 1:2], sm[:, 0:1], 1.0 / D, 1e-6, ALU.mult, ALU.add), reads=[("sm", 0)], writes=[("sm", 1)])
        P.op("act", lambda e: e.activation(sm[:, 2:3], sm[:, 1:2], AF.Sqrt), reads=[("sm", 1)], writes=[("sm", 2)])
        P.op("dve", lambda e: e.reciprocal(sm[:, 3:4], sm[:, 2:3]), reads=[("sm", 2)], writes=[("sm", 3)])
        P.op("dve", lambda e: e.scalar_tensor_tensor(self.ntmp[:], xt, sm[:, 3:4], self.GB[:], ALU.mult, ALU.mult),
             reads=[("X", tb), ("sm", 3), "GB"], writes=["ntmp"])
        P.op("pool", lambda e: e.tensor_tensor(self.hbf[:], self.ntmp[:], self.SHB[:], ALU.add), reads=["ntmp", "SHB"], writes=["hbf"])
        bt = self.bank_bf(0)
        for c in range(8):
            self.tr(bt[:, c * 128:(c + 1) * 128], self.hbf[:, c * 128:(c + 1) * 128], self.identb[:], ["hbf", "identb"], [("B0", c)])
        P.op("act", lambda e: e.copy(self.hT[:], bt[:, 0:1024].rearrange("p (c t) -> p c t", c=8)), reads=["B0"], writes=["hT"])

    def proj(self, bank, ncols, w, wkey, c0):
        for dc in range(8):
            self.mm(self.B[bank][:, 0:ncols], self.hT[:, dc, :], w[:, dc, c0:c0 + ncols], dc == 0, dc == 7,
                    ["hT", wkey], ["B%d" % bank])

    def head_rmsnorm(self, src, srckey, nh, gb, gbkey, out_ap, outkey, sq, rs, npart=128):
        P = self.P
        n = nh * 64
        P.op("act", lambda e: e.activation(sq[:, 0:n], src, AF.Square), reads=[srckey], writes=["sq"])
        P.op("dve", lambda e: e.tensor_reduce(rs[:, 0:nh], sq[:, 0:n].rearrange("p (h d) -> p h d", d=64), AX.X, ALU.add), reads=["sq"], writes=[("rs", 0)])
        P.op("dve", lambda e: e.tensor_scalar(rs[:, 16:16 + nh], rs[:, 0:nh], 1.0 / 64, 1e-6, ALU.mult, ALU.add), reads=[("rs", 0)], writes=[("rs", 1)])
        P.op("act", lambda e: e.activation(rs[:, 32:32 + nh], rs[:, 16:16 + nh], AF.Sqrt), reads=[("rs", 1)], writes=[("rs", 2)])
        P.op("dve", lambda e: e.reciprocal(rs[:, 48:48 + nh], rs[:, 32:32 + nh]), reads=[("rs", 2)], writes=[("rs", 3)])
        P.op("dve", lambda e: e.tensor_tensor(sq[:, 0:n].rearrange("p (h d) -> p h d", d=64), src.rearrange("p (h d) -> p h d", d=64),
                                              rs[:, 48:48 + nh].unsqueeze(2).broadcast_to([npart, nh, 64]), ALU.mult),
             reads=[srckey, ("rs", 3), "sq"], writes=["sq"])
        P.op("pool", lambda e: e.tensor_tensor(out_ap, sq[:, 0:n].rearrange("p (h d) -> p h d", d=64),
                                               gb.unsqueeze(1).broadcast_to([npart, nh, 64]), ALU.mult),
             reads=["sq", gbkey], writes=[outkey])

    def run_attn(self, tasks):
        P = self.P

        def emit_qk(n, t):
            bi = 5 + n % 2
            sb = self.B[bi]
            key = "B%d" % bi
            for (c0, w, nsub, mms) in t["groups"]:
                out = sb[0:t["nrow"], c0:c0 + w]
                if nsub > 1:
                    out = out.rearrange("p (a b) -> p a b", a=nsub)
                for k, (lhsT, rhs, rd) in enumerate(mms):
                    self.mm(out, lhsT, rhs, k == 0, k == len(mms) - 1, rd, [key])

        def emit_rest(n, t):
            bi = 5 + n % 2
            sb = self.B[bi]
            key = "B%d" % bi
            pt = self.PT[n % 3]
            pk = ("PT", n % 3)
            nr, wd = t["nrow"], t["width"]
            exps = t.get("exps") or [(0, wd, None, [])]
            for (c0, w, bias, rd) in exps:
                if bias is None:
                    P.op("act", lambda e: e.activation(pt[0:nr, c0:c0 + w], sb[0:nr, c0:c0 + w], AF.Exp, scale=0.125), reads=[key], writes=[pk])
                else:
                    P.op("act", lambda e: e.activation(pt[0:nr, c0:c0 + w], sb[0:nr, c0:c0 + w], AF.Exp, bias=bias, scale=0.125),
                         reads=[key] + rd, writes=[pk])
            for (pc0, pw, rhs, out, outkey, start, stop, rd) in t["pv"]:
                self.mm(out, pt[0:nr, pc0:pc0 + pw], rhs, start, stop, [pk] + rd, [outkey], skip=True)

        if not tasks:
            return
        emit_qk(0, tasks[0])
        for n, t in enumerate(tasks):
            if n + 1 < len(tasks):
                emit_qk(n + 1, tasks[n + 1])
            emit_rest(n, t)

    def out_proj_residual(self, i, wout):
        P = self.P
        Obf = self.hbf
        bt = self.bank_bf(0)
        for c in range(8):
            self.tr(bt[:, c * 128:(c + 1) * 128], Obf[:, c * 128:(c + 1) * 128], self.identb[:], ["hbf", "identb"], [("B0", c)])
        P.op("act", lambda e: e.copy(self.OT[:], bt[:, 0:1024].rearrange("p (c t) -> p c t", c=8)), reads=["B0"], writes=["OT"])
        for half in range(2):
            bk = 3 + half
            for fc in range(8):
                self.mm(self.B[bk][:, :], self.OT[:, fc, :], wout[:, fc, half * 512:(half + 1) * 512], fc == 0, fc == 7,
                        ["OT", "wout"], ["B%d" % bk])
            P.op("dve", lambda e: e.tensor_tensor(self.ntmp[:, half * 512:(half + 1) * 512], self.B[bk][:, :],
                                                  self.GTB[:, half * 512:(half + 1) * 512], ALU.mult),
                 reads=["B%d" % bk, "GTB"], writes=[("ntmp", half)])
            P.op("pool", lambda e: e.tensor_tensor(self.X[:, i, half * 512:(half + 1) * 512], self.X[:, i, half * 512:(half + 1) * 512],
                                                   self.ntmp[:, half * 512:(half + 1) * 512], ALU.add),
                 reads=[("ntmp", half), ("X", i)], writes=[("X", i)])

    def even_layer(self, l):
        P = self.P
        I = self.I
        li = l // 2
        w_in = I["even_w_in"][li]
        self.load_mods(l, 0)
        with ExitStack() as lay:
            kT = self.T(lay, "kT", [128, 4, S], BF16)
            Vp = self.T(lay, "Vp", [128, NT, 8, 65], BF16)
            cTT = self.T(lay, "cTT", [128, NT, 8], F32)
            rbc = self.T(lay, "rbc", [128, NT, 8], F32)
            bcol = self.T(lay, "bcol", [128, 8, NT], F32)
            kgb = self.T(lay, "kgb", [128, 64], F32)
            qgb = self.T(lay, "qgb", [128, 64], F32)
            sq = self.T(lay, "sq", [128, 1024], F32)
            rs = self.T(lay, "rs", [128, 64], F32)
            kn = self.T(lay, "kn", [128, 512], BF16)
            qT = self.T(lay, "qT", [128, 4, 128], BF16)
            self.OT = self.T(lay, "OT", [128, 8, 128], BF16)
            self.PT = [self.T(lay, "PT%d" % k, [128, 512], BF16) for k in range(3)]
            P.dma(kgb[:], I["even_k_g"][li:li + 1, :].broadcast_to([128, 64]), writes=["kgb"])
            P.dma(qgb[:], I["even_q_g"][li:li + 1, :].broadcast_to([128, 64]), writes=["qgb"])
            P.op("pool", lambda e: e.memset(Vp[:], 1.0), writes=["Vp"])
            with ExitStack() as ph:
                wkv = self.T(ph, "wkv", [128, 8, 1032], BF16)
                fT = self.T(ph, "fT", [8, S], F32)
                cT = self.T(ph, "cT", [8, S], F32)
                rsel = self.T(ph, "rsel", [8, NT, 8], F32)
                fcol = self.T(ph, "fcol", [128, 8], F32)
                negb = self.T(ph, "negb", [8, 1], F32)
                P.dma(wkv[:], w_in[:, 2048:3080].rearrange("(c p) n -> p c n", p=128), writes=["wkv"], q="pool")
                P.dma(negb[:], I["even_b_f"][li].rearrange("(h o) -> h o", o=1), writes=["negb"])
                P.op("dve", lambda e: e.tensor_scalar(negb[:], negb[:], -1.0, None, ALU.mult), reads=["negb"], writes=["negb"])
                for tb in range(NT):
                    self.norm_hT(tb)
                    self.proj(1, 512, wkv, "wkv", 0)
                    self.proj(2, 512, wkv, "wkv", 512)
                    self.proj(3, 8, wkv, "wkv", 1024)
                    self.head_rmsnorm(self.B[1][:, :], "B1", 8, kgb[:], "kgb", kn[:].rearrange("p (h d) -> p h d", d=64), "kn", sq, rs)
                    b4 = self.bank_bf(4)
                    for c in range(4):
                        self.tr(b4[:, c * 128:(c + 1) * 128], kn[:, c * 128:(c + 1) * 128], self.identb[:], ["kn", "identb"], [("B4", c)])
                    P.op("act", lambda e: e.copy(kT[:, :, tb * 128:(tb + 1) * 128], b4[:, 0:512].rearrange("p (c t) -> p c t", c=4)),
                         reads=["B4"], writes=[("kT", tb)])
                    P.op("act", lambda e: e.copy(Vp[:, tb, :, 0:64], self.B[2][:, :].rearrange("p (h d) -> p h d", d=64)),
                         reads=["B2"], writes=[("Vp", tb)])
                    P.op("dve", lambda e: e.tensor_copy(fcol[:], self.B[3][:, 0:8]), reads=["B3"], writes=["fcol"])
                    self.tr(self.B[7][0:8, 0:128], fcol[:], self.identf[:], ["fcol", "identf"], ["B7"])
                    P.op("act", lambda e: e.copy(fT[:, tb * 128:(tb + 1) * 128], self.B[7][0:8, 0:128]), reads=["B7"], writes=[("fT", tb)])
                P.op("act", lambda e: e.activation(fT[:], fT[:], AF.Exp, bias=negb[:], scale=-1.0), reads=["fT", "negb"], writes=["fT"])
                P.op("act", lambda e: e.activation(fT[:], fT[:], AF.Ln, bias=1.0, scale=1.0), reads=["fT"], writes=["fT"])
                P.op("dve", lambda e: e.tensor_scalar(fT[:], fT[:], -1.0, None, ALU.mult), reads=["fT"], writes=["fT"])
                P.op("dve", lambda e: e.tensor_tensor_scan(cT[:], self.onec[0:8, 0:1].broadcast_to([8, S]), fT[:], 0.0, ALU.mult, ALU.add),
                     reads=["onec", "fT"], writes=["cT"])
                for j in range(NT):
                    self.tr(self.B[1][:, j * 8:(j + 1) * 8], cT[:, j * 128:(j + 1) * 128], self.identf[0:8, 0:8], ["cT", "identf"], [("B1", j)])
                P.op("dve", lambda e: e.tensor_copy(cTT[:].rearrange("p j h -> p (j h)"), self.B[1][:, 0:128]), reads=["B1"], writes=["cTT"])
                P.op("dve", lambda e: e.tensor_tensor(rsel[:], cT[:, 64:64 + 128 * 15 + 1:128].unsqueeze(2).broadcast_to([8, NT, 8]),
                                                      self.identf[0:8, 0:8].unsqueeze(1).broadcast_to([8, NT, 8]), ALU.mult),
                     reads=["cT", "identf"], writes=["rsel"])
                self.mm(self.B[2][:, 0:128], self.onesf[:], rsel[:].rearrange("p i h -> p (i h)"), True, True, ["onesf", "rsel"], ["B2"])
                P.op("dve", lambda e: e.tensor_copy(rbc[:].rearrange("p i h -> p (i h)"), self.B[2][:, 0:128]), reads=["B2"], writes=["rbc"])
                P.barrier()
            with ExitStack() as ph:
                wq2 = self.T(ph, "wq2", [128, 8, 2048], BF16)
                wout = self.T(ph, "wout", [128, 8, D], BF16)
                cwb = self.T(ph, "cwb", [128, 3, 512], BF16)
                shm = self.T(ph, "shm", [128, 4, 128], BF16)
                caus = self.T(ph, "caus", [128, 128], BF16)
                uw = [self.T(ph, "uw%d" % k, [128, 3, 512], BF16) for k in range(2)]
                ccs = sq[:, 0:512]
                cbs = sq[:, 512:1024]
                ucur = self.ntmp[:, 0:512]
                Obf = self.hbf
                P.dma(wq2[:], w_in[:, 0:2048].rearrange("(c p) n -> p c n", p=128), writes=["wq2"], q="pool")
                P.dma(wout[:], I["even_w_out"][li].rearrange("(c p) n -> p c n", p=128), writes=["wout"], q="pool")
                P.dma(cwb[:].rearrange("p k c -> p (k c)"), I["even_conv_w"][li:li + 1, :].broadcast_to([128, 1536]), writes=["cwb"], q="pool")
                P.dma(shm[:], I["k_shm"], writes=["shm"], q="pool")
                P.dma(caus[:], I["k_caus"], writes=["caus"], q="pool")
                for i in range(NT):
                    self.norm_hT(i)
                    for k in range(4):
                        self.proj(1 + k, 512, wq2, "wq2", 512 * k)
                    P.op("act", lambda e: e.copy(ccs, self.B[2][:, :]), reads=["B2"], writes=["sq"])
                    P.op("act", lambda e: e.copy(cbs, self.B[1][:, :]), reads=["B1"], writes=["sq"])
                    P.op("dve", lambda e: e.tensor_tensor(ucur, ccs, self.B[3][:, :], ALU.mult), reads=["sq", "B3"], writes=["ntmp"])
                    uwc, uwp = uw[i % 2], uw[(i + 1) % 2]
                    kc_, kp_ = "uw%d" % (i % 2), "uw%d" % ((i + 1) % 2)
                    P.op("pool", lambda e: e.tensor_tensor(uwc[:], ucur.unsqueeze(1).broadcast_to([128, 3, 512]), cwb[:], ALU.mult),
                         reads=["ntmp", "cwb"], writes=[kc_])
                    mms = [(self.identb[:], uwc[:, 2, :], [kc_]), (shm[:, 0, :], uwc[:, 1, :], [kc_, "shm"]), (shm[:, 1, :], uwc[:, 0, :], [kc_, "shm"])]
                    if i > 0:
                        mms += [(shm[:, 2, :], uwp[:, 1, :], [kp_, "shm"]), (shm[:, 3, :], uwp[:, 0, :], [kp_, "shm"])]
                    for k, (lt, rh, rd) in enumerate(mms):
                        self.mm(self.B[2][:, :], lt, rh, k == 0, k == len(mms) - 1, rd + ["identb"], ["B2"])
                    P.op("dve", lambda e: e.tensor_tensor(Obf[:, 0:512], cbs, self.B[2][:, :], ALU.mult), reads=["sq", "B2"], writes=["hbf"])
                    self.head_rmsnorm(self.B[4][:, :], "B4", 8, qgb[:], "qgb", kn[:].rearrange("p (h d) -> p h d", d=64), "kn", sq, rs)
                    b1 = self.bank_bf(1)
                    for c in range(4):
                        self.tr(b1[:, c * 128:(c + 1) * 128], kn[:, c * 128:(c + 1) * 128], self.identb[:], ["kn", "identb"], [("B1", c)])
                    P.op("act", lambda e: e.copy(qT[:], b1[:, 0:512].rearrange("p (c t) -> p c t", c=4)), reads=["B1"], writes=["qT"])
                    for h in range(8):
                        base = (h % 2) * 64
                        pr = h // 2
                        P.op("dve", lambda e: e.tensor_scalar(bcol[:, h, 0:i + 1], cTT[:, 0:i + 1, h], rbc[:, i, h:h + 1], -1.0, ALU.subtract, ALU.mult),
                             reads=["cTT", "rbc"], writes=[("bcol", h)])
                        tasks = []
                        oi = self.B[7][:, (h % 4) * 65:(h % 4) * 65 + 65]
                        oik = ("B7", h % 4)
                        for j0 in range(0, i + 1, 4):
                            js = list(range(j0, min(j0 + 4, i + 1)))
                            groups = []
                            exps = []
                            pv = []
                            for jj, j in enumerate(js):
                                mm_ = [(kT[base:base + 64, pr, j * 128:(j + 1) * 128], qT[base:base + 64, pr, :], ["kT", "qT"])]
                                if j == i:
                                    mm_.append((self.identb[:], caus[:], ["identb", "caus"]))
                                groups.append((jj * 128, 128, 1, mm_))
                                exps.append((jj * 128, 128, bcol[:, h, j:j + 1], [("bcol", h)]))
                                pv.append((jj * 128, 128, Vp[:, j, h, :], oi, oik, j == 0, j == i, ["Vp"]))
                            tasks.append(dict(nrow=128, width=len(js) * 128, groups=groups, exps=exps, pv=pv))
                        self.run_attn(tasks)
                        P.op("dve", lambda e: e.reciprocal(self.rd[:, h:h + 1], oi[:, 64:65]), reads=[oik], writes=[("rd", h)])
                        P.op("dve", lambda e: e.tensor_scalar(Obf[:, 512 + h * 64:512 + (h + 1) * 64], oi[:, 0:64], self.rd[:, h:h + 1], None, ALU.mult),
                             reads=[oik, ("rd", h)], writes=["hbf"])
                    self.out_proj_residual(i, wout)
                P.barrier()

    def rel_tables(self):
        P = self.P
        I = self.I
        with ExitStack() as st:
            tab = self.T(st, "tab", [32, 16], F32)
            ohb = self.T(st, "ohb", [32, 2048], F32)
            fvr = self.T(st, "fvr", [16, 4096], BF16)
            P.dma(tab[:], I["rel_table"], writes=["tab"])
            P.dma(ohb[:], I["k_ohb"], writes=["ohb"])
            P.op("pool", lambda e: e.memset(fvr[:], -BIG), writes=["fvr"])
            for q in range(4):
                self.mm(self.B[1][0:16, :], tab[:], ohb[:, q * 512:(q + 1) * 512], True, True, ["tab", "ohb"], ["B1"])
                P.op("dve", lambda e: e.tensor_scalar(fvr[:, 2048 + q * 512:2048 + (q + 1) * 512], self.B[1][0:16, :], 8.0, None, ALU.mult),
                     reads=["B1"], writes=["fvr"])
            P.dma(self.FV, fvr[:], reads=["fvr"], writes=["FV"])
            P.op("pool", lambda e: e.memset(fvr[:, 2048 + 512:4096], -BIG), reads=["fvr"], writes=["fvr"])
            P.dma(self.FW, fvr[:], reads=["fvr"], writes=["FW"])
            P.barrier()

    def odd_layer(self, l):
        P = self.P
        I = self.I
        li = l // 2
        w_in = I["odd_w_in"][li]
        rd = self.rd
        self.load_mods(l, 0)
        with ExitStack() as lay:
            ksT = self.T(lay, "ksT", [128, 2, S], BF16)
            kwT = self.T(lay, "kwT", [128, 2, S], BF16)
            Vs = self.T(lay, "Vs", [128, NT, 4, 65], BF16)
            Vw = self.T(lay, "Vw", [128, NT, 4, 65], BF16)
            KcT = self.T(lay, "KcT", [128, 2, 128], BF16)
            VcX = self.T(lay, "VcX", [128, 4, 97], BF16)
            gbs = self.T(lay, "gbs", [128, 4, 64], F32)
            sq = self.T(lay, "sq", [128, 1024], F32)
            rs = self.T(lay, "rs", [128, 64], F32)
            D0 = self.T(lay, "D0", [128, 16, 128], BF16)
            D1 = self.T(lay, "D1", [128, 16, 128], BF16)
            D4 = self.T(lay, "D4", [128, 16, 128], BF16)
            CROW = self.T(lay, "CROW", [1, 16, 128], BF16)
            for (t, nm, src, k) in ((D0, "D0", self.FV, 0), (D1, "D1", self.FV, 1), (D4, "D4", self.FW, 4)):
                ap = bass.AP(tensor=src.tensor, offset=2048 + 128 * k - 127, ap=[[1, 128], [4096, 16], [1, 128]])
                P.dma(t[:], ap, writes=[nm])
            ap = bass.AP(tensor=self.FV.tensor, offset=2048 + 1000, ap=[[0, 1], [4096, 16], [1, 128]])
            P.dma(CROW[:], ap, writes=["CROW"])
            P.dma(gbs[:, 0, :], I["odd_q_g"][li:li + 1, :].broadcast_to([128, 64]), writes=[("gbs", 0)])
            P.dma(gbs[:, 1:4, :].rearrange("p k d -> p (k d)"), I["odd_k_g"][li:li + 1, :].broadcast_to([128, 192]), writes=[("gbs", 1)])
            P.op("pool", lambda e: e.memset(Vs[:], 1.0), writes=["Vs"])
            P.op("pool", lambda e: e.memset(Vw[:], 1.0), writes=["Vw"])
            P.op("pool", lambda e: e.memset(VcX[:], 1.0), writes=["VcX"])
            with ExitStack() as ph:
                wkv = self.T(ph, "wkv", [128, 8, 1536], BF16)
                kcT = self.T(ph, "kcT", [128, 2, S], BF16)
                vcT = self.T(ph, "vcT", [128, 2, S], BF16)
                kvb = self.T(ph, "kvb", [128, 4, 256], BF16)
                w1b = [self.T(ph, "w1b%d" % a, [128, 32, 64], BF16) for a in range(2)]
                w2b = [self.T(ph, "w2b%d" % a, [64, 64], BF16) for a in range(2)]
                pos = self.T(ph, "pos", [32, 2, 64], F32)
                posT = self.T(ph, "posT", [64, 2, 32], BF16)
                cst = self.T(ph, "cst", [64, 2], F32)
                HT = self.T(ph, "HT", [64, 128], BF16)
                KcN = self.T(ph, "KcN", [128, 4, 64], BF16)
                ovl = self.T(ph, "ovl", [127, 32], F32)
                P.dma(wkv[:], w_in[:, 1024:2560].rearrange("(c p) n -> p c n", p=128), writes=["wkv"], q="pool")
                for a in range(2):
                    for hf in range(2):
                        P.dma(w1b[a][hf * 64:(hf + 1) * 64, :, :], I["odd_cmp_w1"][li, a].rearrange("(l d) o -> d l o", d=64),
                              writes=[("w1b%d" % a, hf)], q="pool")
                    P.dma(w2b[a][:], I["odd_cmp_w2"][li, a], writes=["w2b%d" % a], q="pool")
                    P.dma(pos[:, a, :], I["odd_cmp_pos"][li, a], writes=[("pos", a)])
                P.dma(ovl[:], I["k_ovl"], writes=["ovl"])
                P.op("dve", lambda e: e.tensor_copy(VcX[0:127, :, 65:97], ovl[:].unsqueeze(1).broadcast_to([127, 4, 32])),
                     reads=["ovl", "VcX"], writes=["VcX"])
                for tb in range(NT):
                    self.norm_hT(tb)
                    self.proj(1, 512, wkv, "wkv", 0)
                    self.proj(2, 512, wkv, "wkv", 512)
                    self.proj(3, 512, wkv, "wkv", 1024)
                    P.op("act", lambda e: e.copy(kvb[:, 0:2, :], self.B[1][:, :].rearrange("p (a n) -> p a n", a=2)), reads=["B1"], writes=[("kvb", 0)])
                    self.head_rmsnorm(self.B[2][:, 0:256], "B2", 4, gbs[:, 2, :], ("gbs", 1), kvb[:, 2, :].rearrange("p (h d) -> p h d", d=64), ("kvb", 2), sq, rs)
                    self.head_rmsnorm(self.B[3][:, 0:256], "B3", 4, gbs[:, 3, :], ("gbs", 1), kvb[:, 3, :].rearrange("p (h d) -> p h d", d=64), ("kvb", 3), sq, rs)
                    P.op("act", lambda e: e.copy(Vs[:, tb, :, 0:64], self.B[2][:, 256:512].rearrange("p (h d) -> p h d", d=64)), reads=["B2"], writes=[("Vs", tb)])
                    P.op("act", lambda e: e.copy(Vw[:, tb, :, 0:64], self.B[3][:, 256:512].rearrange("p (h d) -> p h d", d=64)), reads=["B3"], writes=[("Vw", tb)])
                    b4 = self.bank_bf(4)
                    for a in range(4):
                        for c in range(2):
                            self.tr(b4[:, (a * 2 + c) * 128:(a * 2 + c + 1) * 128], kvb[:, a, c * 128:(c + 1) * 128], self.identb[:],
                                    ["kvb", "identb"], [("B4", a * 2 + c)])
                    for a, (dst, nm) in enumerate(((kcT, "kcT"), (vcT, "vcT"), (ksT, "ksT"), (kwT, "kwT"))):
                        P.op("act", lambda e: e.copy(dst[:, :, tb * 128:(tb + 1) * 128], b4[:, a * 256:(a + 1) * 256].rearrange("p (c t) -> p c t", c=2)),
                             reads=["B4"], writes=[(nm, tb)])
                for a in range(2):
                    self.tr(self.B[1][0:64, 0:32], pos[:, a, :], self.identf[0:32, 0:32], ["pos", "identf"], ["B1"])
                    P.op("act", lambda e: e.copy(posT[:, a, :], self.B[1][0:64, 0:32]), reads=["B1"], writes=[("posT", a)])
                    for lq in range(32):
                        self.mm(self.B[2][0:64, 0:1], w1b[a][0:64, lq, :], posT[:, a, lq:lq + 1], lq == 0, lq == 31, ["w1b%d" % a, ("posT", a)], ["B2"])
                    P.op("dve", lambda e: e.tensor_copy(cst[:, a:a + 1], self.B[2][0:64, 0:1]), reads=["B2"], writes=[("cst", a)])
                for a, srcT in enumerate((kcT, vcT)):
                    for gq in range(4):
                        base = (gq % 2) * 64
                        ch = gq // 2
                        for lq in range(32):
                            rhs = srcT[base:base + 64, ch, lq:lq + 16 * 126 + 1:16]
                            self.mm(self.B[5][0:64, 0:127], w1b[a][base:base + 64, lq, :], rhs, lq == 0, lq == 31,
                                    ["w1b%d" % a, "kcT", "vcT"], ["B5"])
                        P.op("act", lambda e: e.activation(HT[:, 0:127], self.B[5][0:64, 0:127], AF.Gelu_apprx_tanh, bias=cst[:, a:a + 1], scale=1.0),
                             reads=["B5", ("cst", a)], writes=["HT"])
                        self.mm(self.B[6][0:127, 0:64], HT[:, 0:127], w2b[a][:], True, True, ["HT", "w2b%d" % a], ["B6"])
                        if a == 0:
                            self.head_rmsnorm(self.B[6][0:127, 0:64], "B6", 1, gbs[0:127, 1, :], ("gbs", 1), KcN[0:127, gq:gq + 1, :], ("KcN", gq),
                                              sq[0:127], rs[0:127], npart=127)
                        else:
                            P.op("act", lambda e: e.copy(VcX[0:127, gq, 0:64], self.B[6][0:127, 0:64]), reads=["B6"], writes=[("VcX", gq)])
                b4 = self.bank_bf(4)
                for c in range(2):
                    self.tr(b4[:, c * 128:c * 128 + 127], KcN[0:127, 2 * c:2 * c + 2, :].rearrange("p g d -> p (g d)"), self.identb[0:127, 0:127],
                            ["KcN", "identb"], [("B4", c)])
                    P.op("act", lambda e: e.copy(KcT[:, c, 0:127], b4[:, c * 128:c * 128 + 127]), reads=[("B4", c)], writes=[("KcT", c)])
                P.barrier()
            with ExitStack() as ph:
                wq = self.T(ph, "wq", [128, 8, 1072], BF16)
                wout = self.T(ph, "wout", [128, 8, D], BF16)
                bgb = self.T(ph, "bgb", [128, 48], F32)
                gates = self.T(ph, "gates", [128, 48], F32)
                a1 = self.T(ph, "a1", [128, 16, 32], BF16)
                a0 = self.T(ph, "a0", [128, 16, 32], BF16)
                ej = self.T(ph, "ej", [32, 16, 128], BF16)
                qn = self.T(ph, "qn", [128, D], BF16)
                qT = self.T(ph, "qT", [128, 8, 128], BF16)
                Of = self.ntmp
                imp = self.T(ph, "imp", [128, 4, 32], F32)
                impw = self.T(ph, "impw", [128, 4, 32], F32)
                nmk = self.T(ph, "nmk", [128, 4, 32], BF16)
                NMT = self.T(ph, "NMT", [32, 4, 128], BF16)
                bci = self.T(ph, "bci", [127, 16, 128], BF16)
                self.OT = self.T(ph, "OT", [128, 8, 128], BF16)
                self.PT = [self.T(ph, "PT%d" % k, [128, 512], BF16) for k in range(3)]
                P.dma(wq[:, :, 0:1024], w_in[:, 0:1024].rearrange("(c p) n -> p c n", p=128), writes=[("wq", 0)], q="pool")
                P.dma(wq[:, :, 1024:1072], w_in[:, 2560:2608].rearrange("(c p) n -> p c n", p=128), writes=[("wq", 1)], q="pool")
                P.dma(wout[:], I["odd_w_out"][li].rearrange("(c p) n -> p c n", p=128), writes=["wout"], q="pool")
                P.dma(bgb[:], I["odd_b_gate"][li:li + 1, :].broadcast_to([128, 48]), writes=["bgb"])
                P.dma(a1[:], I["k_a1"], writes=["a1"], q="pool")
                P.dma(a0[:], I["k_a0"], writes=["a0"], q="pool")
                P.dma(ej[:], I["k_ej"], writes=["ej"], q="pool")
                for i in range(NT):
                    self.norm_hT(i)
                    self.proj(1, 512, wq, "wq", 0)
                    self.proj(2, 512, wq, "wq", 512)
                    self.proj(3, 48, wq, "wq", 1024)
                    P.op("dve", lambda e: e.tensor_tensor(gates[:], self.B[3][:, 0:48], bgb[:], ALU.add), reads=["B3", "bgb"], writes=["gates"])
                    P.op("act", lambda e: e.activation(gates[:], gates[:], AF.Sigmoid), reads=["gates"], writes=["gates"])
                    for hb in range(2):
                        src = self.B[1 + hb][:, :]
                        sk = "B%d" % (1 + hb)
                        P.op("act", lambda e: e.activation(sq[:, 0:512], src, AF.Square), reads=[sk], writes=["sq"])
                        P.op("dve", lambda e: e.tensor_reduce(rs[:, 0:8], sq[:, 0:512].rearrange("p (h d) -> p h d", d=64), AX.X, ALU.add), reads=["sq"], writes=[("rs", 0)])
                        P.op("dve", lambda e: e.tensor_scalar(rs[:, 16:24], rs[:, 0:8], 1.0 / 64, 1e-6, ALU.mult, ALU.add), reads=[("rs", 0)], writes=[("rs", 1)])
                        P.op("act", lambda e: e.activation(rs[:, 32:40], rs[:, 16:24], AF.Sqrt), reads=[("rs", 1)], writes=[("rs", 2)])
                        P.op("dve", lambda e: e.reciprocal(rs[:, 48:56], rs[:, 32:40]), reads=[("rs", 2)], writes=[("rs", 3)])
                        P.op("dve", lambda e: e.tensor_tensor(sq[:, 0:512].rearrange("p (h d) -> p h d", d=64), src.rearrange("p (h d) -> p h d", d=64),
                                                              rs[:, 48:56].unsqueeze(2).broadcast_to([128, 8, 64]), ALU.mult),
                             reads=[sk, ("rs", 3), "sq"], writes=["sq"])
                        for gh in range(2):
                            dst = qn[:, hb * 512:(hb + 1) * 512].rearrange("p (j gh d) -> p gh j d", j=4, gh=2)[:, gh, :, :]
                            srcv = sq[:, gh * 256:(gh + 1) * 256].rearrange("p (j d) -> p j d", j=4)
                            P.op("pool", lambda e: e.tensor_tensor(dst, srcv, gbs[:, 0, :].unsqueeze(1).broadcast_to([128, 4, 64]), ALU.mult),
                                 reads=["sq", ("gbs", 0)], writes=[("qn", hb, gh)])
                    b4 = self.bank_bf(4)
                    for c in range(8):
                        self.tr(b4[:, c * 128:(c + 1) * 128], qn[:, c * 128:(c + 1) * 128], self.identb[:], ["qn", "identb"], [("B4", c)])
                    P.op("act", lambda e: e.copy(qT[:], b4[:, 0:1024].rearrange("p (c t) -> p c t", c=8)), reads=["B4"], writes=["qT"])
                    ap = bass.AP(tensor=self.FV.tensor, offset=2048 + 128 * i - 2047, ap=[[16, 127], [4096, 16], [1, 128]])
                    P.dma(bci[:], ap, writes=["bci"])
                    oi3 = self.B[7][:, 0:388].rearrange("p (a b) -> p a b", a=4)
                    for gq in range(4):
                        base = (gq % 2) * 64
                        cq0 = (gq // 2) * 4
                        mms = [(KcT[base:base + 64, gq // 2, 0:127], qT[base:base + 64, cq0:cq0 + 4, :], ["KcT", "qT"]),
                               (self.anti127[0:127, 0:127], bci[:, 4 * gq:4 * gq + 4, :], ["anti127", "bci"])]
                        pv = [(jh * 128, 128, VcX[0:127, gq, :], oi3[:, jh, :], "B7", True, True, ["VcX"]) for jh in range(4)]
                        self.run_attn([dict(nrow=127, width=512, groups=[(0, 512, 4, mms)], pv=pv)])
                        P.op("dve", lambda e: e.tensor_scalar(rd[:, 0:4], oi3[:, :, 64], 1e-30, None, ALU.max), reads=["B7"], writes=[("rd", "den")])
                        P.op("dve", lambda e: e.reciprocal(rd[:, 4:8], rd[:, 0:4]), reads=[("rd", "den")], writes=[("rd", "rden")])
                        gsl = gates[:, 12 * gq:12 * gq + 12].rearrange("p (j b) -> p j b", b=3)
                        P.op("dve", lambda e: e.tensor_tensor(rd[:, 8:12], rd[:, 4:8], gsl[:, :, 0], ALU.mult), reads=[("rd", "rden"), "gates"], writes=[("rd", "gr")])
                        P.op("dve", lambda e: e.tensor_tensor(Of[:, gq * 256:(gq + 1) * 256].rearrange("p (j d) -> p j d", j=4), oi3[:, :, 0:64],
                                                              rd[:, 8:12].unsqueeze(2).broadcast_to([128, 4, 64]), ALU.mult),
                             reads=["B7", ("rd", "gr")], writes=["ntmp"])
                        P.op("dve", lambda e: e.tensor_tensor(impw[:], oi3[:, :, 65:97], rd[:, 4:8].unsqueeze(2).broadcast_to([128, 4, 32]), ALU.mult),
                             reads=["B7", ("rd", "rden")], writes=["impw"])
                        P.op("dve", lambda e: e.tensor_reduce(imp[:, gq, :], impw[:].rearrange("p j m -> p m j"), AX.X, ALU.add), reads=["impw"], writes=[("imp", gq)])
                    P.op("dve", lambda e: e.tensor_tensor(imp[:], imp[:], a1[:, i:i + 1, :].broadcast_to([128, 4, 32]), ALU.mult), reads=["imp", "a1"], writes=["imp"])
                    P.op("dve", lambda e: e.tensor_tensor(imp[:], imp[:], a0[:, i:i + 1, :].broadcast_to([128, 4, 32]), ALU.add), reads=["imp", "a0"], writes=["imp"])
                    for gq in range(4):
                        P.op("dve", lambda e: e.max(rd[:, 12:20], imp[:, gq, :]), reads=["imp"], writes=[("rd", "m8a")])
                        P.op("dve", lambda e: e.match_replace(impw[:, gq, :], rd[:, 12:20], imp[:, gq, :], -1e30), reads=["imp", ("rd", "m8a")], writes=["impw"])
                        P.op("dve", lambda e: e.max(rd[:, 20:28], impw[:, gq, :]), reads=["impw"], writes=[("rd", "m8b")])
                        P.op("dve", lambda e: e.tensor_scalar(rd[:, 28:29], rd[:, 27:28], 0.0, None, ALU.max), reads=[("rd", "m8b")], writes=[("rd", "thr")])
                        P.op("dve", lambda e: e.tensor_scalar(impw[:, gq, :], imp[:, gq, :], rd[:, 28:29], 1.0, ALU.is_ge, ALU.subtract),
                             reads=["imp", ("rd", "thr"), "impw"], writes=["impw"])
                        P.op("dve", lambda e: e.tensor_scalar(nmk[:, gq, :], impw[:, gq, :], BIG, None, ALU.mult), reads=["impw"], writes=[("nmk", gq)])
                    b3 = self.bank_bf(3)
                    for gq in range(4):
                        self.tr(b3[0:32, 512 + gq * 128:512 + (gq + 1) * 128], nmk[:, gq, :], self.identb[:], ["nmk", "identb"], [("B3", gq)])
                    P.op("act", lambda e: e.copy(NMT[:], b3[0:32, 512:1024].rearrange("p (g t) -> p g t", g=4)), reads=["B3"], writes=["NMT"])
                    oi4 = self.B[7][:, 0:260].rearrange("p (a b) -> p a b", a=4)
                    for br, (kTt, knm, Vt, vnm, jlo) in enumerate(((ksT, "ksT", Vs, "Vs", 0), (kwT, "kwT", Vw, "Vw", max(0, i - 4)))):
                        for gq in range(4):
                            base = (gq % 2) * 64
                            cq0 = (gq // 2) * 4
                            tasks = []
                            for j in range(jlo, i + 1):
                                dl = i - j
                                mms = [(kTt[base:base + 64, gq // 2, j * 128:(j + 1) * 128], qT[base:base + 64, cq0:cq0 + 4, :], [knm, "qT"])]
                                if br == 0:
                                    mms.append((ej[:, j, :], NMT[:, gq:gq + 1, :].broadcast_to([32, 4, 128]), ["ej", "NMT"]))
                                if dl == 0:
                                    mms.append((self.antib[:], D0[:, 4 * gq:4 * gq + 4, :], ["antib", "D0"]))
                                elif dl == 1:
                                    mms.append((self.antib[:], D1[:, 4 * gq:4 * gq + 4, :], ["antib", "D1"]))
                                elif dl == 4 and br == 1:
                                    mms.append((self.antib[:], D4[:, 4 * gq:4 * gq + 4, :], ["antib", "D4"]))
                                else:
                                    mms.append((self.ones_row[:], CROW[0:1, 4 * gq:4 * gq + 4, :], ["ones_row", "CROW"]))
                                pv = [(jh * 128, 128, Vt[:, j, gq, :], oi4[:, jh, :], "B7", (j == jlo and jh == 0), j == i, [vnm]) for jh in range(4)]
                                tasks.append(dict(nrow=128, width=512, groups=[(0, 512, 4, mms)], pv=pv))
                            self.run_attn(tasks)
                            gsl = gates[:, 12 * gq:12 * gq + 12].rearrange("p (j b) -> p j b", b=3)
                            P.op("dve", lambda e: e.reciprocal(rd[:, 4:8], oi4[:, :, 64]), reads=["B7"], writes=[("rd", "rden")])
                            P.op("dve", lambda e: e.tensor_tensor(rd[:, 8:12], rd[:, 4:8], gsl[:, :, 1 + br], ALU.mult), reads=[("rd", "rden"), "gates"], writes=[("rd", "gr")])
                            P.op("dve", lambda e: e.tensor_tensor(sq[:, 0:256].rearrange("p (j d) -> p j d", j=4), oi4[:, :, 0:64],
                                                                  rd[:, 8:12].unsqueeze(2).broadcast_to([128, 4, 64]), ALU.mult),
                                 reads=["B7", ("rd", "gr")], writes=["sq"])
                            P.op("pool", lambda e: e.tensor_tensor(Of[:, gq * 256:(gq + 1) * 256], Of[:, gq * 256:(gq + 1) * 256], sq[:, 0:256], ALU.add),
                                 reads=["sq", "ntmp"], writes=["ntmp"])
                    P.op("act", lambda e: e.copy(self.hbf[:], Of[:]), reads=["ntmp"], writes=["hbf"])
                    self.out_proj_residual(i, wout)
                P.barrier()

    def peer_layer(self, l):
        P = self.P
        I = self.I
        self.load_mods(l, 1)
        for et in range(32):
            P.dma(self.UB[et], I["peer_ut"][l, :, et * 512:(et + 1) * 512].rearrange("(c p) e -> p c e", p=128), writes=[("UB", et)], q="pool")
            P.dma(self.VB[et], I["peer_v"][l, et * 512:(et + 1) * 512, :].rearrange("(c p) d -> p c d", p=128), writes=[("VB", et)], q="pool")
        with ExitStack() as ph:
            wq = self.T(ph, "wq", [128, 8, D], BF16)
            kin = self.T(ph, "kin", [128, 2, 128], F32)
            kbd = self.T(ph, "kbd", [128, 256], BF16)
            qTp = self.T(ph, "qTp", [128, 8, 128], BF16)
            sc = self.T(ph, "sc", [128, 256], F32)
            scr = self.T(ph, "scr", [128, 256], F32)
            tk = self.T(ph, "tk", [128, 8, 64], F32)
            e01 = self.T(ph, "e01", [128, 8, 256], F32)
            W = self.T(ph, "W", [128, 16384], BF16)
            prod = [self.T(ph, "prod%d" % k, [128, 1024], F32) for k in range(2)]
            tmp2 = [self.T(ph, "tmp2%d" % k, [128, 1024], BF16) for k in range(2)]
            ub = [self.T(ph, "ub%d" % k, [128, 8, 512], BF16) for k in range(2)]
            vb = [self.T(ph, "vb%d" % k, [128, 4, D], BF16) for k in range(2)]
            gA = [self.T(ph, "gA%d" % k, [128, 512], BF16) for k in range(2)]
            G = [self.T(ph, "G%d" % k, [128, 512], BF16) for k in range(2)]
            GT = [self.T(ph, "GT%d" % k, [128, 4, 128], BF16) for k in range(2)]
            P.dma(wq[:], I["peer_w_q"][l].rearrange("(c p) n -> p c n", p=128), writes=["wq"], q="pool")
            P.op("pool", lambda e: e.memset(kin[:], 0.0), writes=["kin"])
            P.dma(kin[:, 0, 0:64], I["peer_keys"][l, 0], reads=["kin"], writes=[("kin", 0)])
            P.dma(kin[:, 1, 64:128], I["peer_keys"][l, 1], reads=["kin"], writes=[("kin", 1)])
            for pq in range(2):
                self.tr(self.B[1][:, pq * 128:(pq + 1) * 128], kin[:, pq, :], self.identf[:], ["kin", "identf"], [("B1", pq)])
            P.op("act", lambda e: e.copy(kbd[:], self.B[1][:, 0:256]), reads=["B1"], writes=["kbd"])
            n_et = 0
            for tb in range(NT):
                self.norm_hT(tb)
                for h in range(8):
                    bk = 1 + h // 4
                    for dc in range(8):
                        self.mm(self.B[bk][:, (h % 4) * 128:(h % 4 + 1) * 128], wq[:, dc, h * 128:(h + 1) * 128], self.hT[:, dc, :], dc == 0, dc == 7,
                                ["wq", "hT"], [("B%d" % bk, h % 4)])
                for bk in (1, 2):
                    P.op("act", lambda e: e.copy(qTp[:, (bk - 1) * 4:(bk - 1) * 4 + 4, :], self.B[bk][:, :].rearrange("p (h t) -> p h t", h=4)),
                         reads=["B%d" % bk], writes=[("qTp", bk)])
                for h in range(8):
                    self.mm(self.B[3][:, (h % 2) * 256:(h % 2 + 1) * 256], qTp[:, h, :], kbd[:], True, True, ["qTp", "kbd"], [("B3", h % 2)])
                    P.op("act", lambda e: e.copy(sc[:], self.B[3][:, (h % 2) * 256:(h % 2 + 1) * 256]), reads=[("B3", h % 2)], writes=["sc"])
                    for pq in range(2):
                        s_ = sc[:, pq * 128:(pq + 1) * 128]
                        o = pq * 16
                        P.op("dve", lambda e: e.max(tk[:, h, o:o + 8], s_), reads=["sc"], writes=[("tk", h, pq)])
                        P.op("dve", lambda e: e.match_replace(scr[:, 0:128], tk[:, h, o:o + 8], s_, -1e30), reads=["sc", ("tk", h, pq)], writes=["scr"])
                        P.op("dve", lambda e: e.max(tk[:, h, o + 8:o + 16], scr[:, 0:128]), reads=["scr"], writes=[("tk", h, pq)])
                    cand = scr[:, 0:256].rearrange("p (a b) -> p a b", a=16)
                    P.op("dve", lambda e: e.tensor_tensor(cand, tk[:, h, 0:16].unsqueeze(2).broadcast_to([128, 16, 16]),
                                                          tk[:, h, 16:32].unsqueeze(1).broadcast_to([128, 16, 16]), ALU.add),
                         reads=[("tk", h)], writes=["scr"])
                    P.op("dve", lambda e: e.max(tk[:, h, 32:40], scr[:, 0:256]), reads=["scr"], writes=[("tk", h, 2)])
                    P.op("dve", lambda e: e.match_replace(scr[:, 0:256], tk[:, h, 32:40], scr[:, 0:256], -1e30), reads=["scr", ("tk", h, 2)], writes=["scr"])
                    P.op("dve", lambda e: e.max(tk[:, h, 40:48], scr[:, 0:256]), reads=["scr"], writes=[("tk", h, 2)])
                    P.op("dve", lambda e: e.tensor_scalar(tk[:, h, 48:49], tk[:, h, 32:33], -1.0, None, ALU.mult), reads=[("tk", h, 2)], writes=[("tk", h, 3)])
                    P.op("act", lambda e: e.activation(scr[:, 0:16], tk[:, h, 32:48], AF.Exp, bias=tk[:, h, 48:49], scale=1.0, accum_out=tk[:, h, 49:50]),
                         reads=[("tk", h, 2), ("tk", h, 3), "scr"], writes=["scr", ("tk", h, 4)])
                    P.op("dve", lambda e: e.reciprocal(tk[:, h, 50:51], tk[:, h, 49:50]), reads=[("tk", h, 4)], writes=[("tk", h, 5)])
                    P.op("dve", lambda e: e.tensor_scalar(tk[:, h, 51:52], tk[:, h, 0:1], -1.0, None, ALU.mult), reads=[("tk", h, 0)], writes=[("tk", h, 6)])
                    P.op("dve", lambda e: e.tensor_scalar(tk[:, h, 52:53], tk[:, h, 16:17], -1.0, None, ALU.mult), reads=[("tk", h, 1)], writes=[("tk", h, 7)])
                    P.op("act", lambda e: e.activation(tk[:, h, 54:55], tk[:, h, 47:48], AF.Exp, bias=tk[:, h, 48:49], scale=1.0),
                         reads=[("tk", h, 2), ("tk", h, 3)], writes=[("tk", h, 8)])
                    P.op("dve", lambda e: e.tensor_scalar(tk[:, h, 54:55], tk[:, h, 54:55], tk[:, h, 50:51], 0.9995, ALU.mult, ALU.mult),
                         reads=[("tk", h, 8), ("tk", h, 5)], writes=[("tk", h, 8)])
                    P.op("act", lambda e: e.activation(e01[:, h, 0:128], sc[:, 0:128], AF.Exp, bias=tk[:, h, 51:52], scale=1.0),
                         reads=["sc", ("tk", h, 6)], writes=[("e01", h, 0)])
                    P.op("dve", lambda e: e.tensor_scalar(e01[:, h, 0:128], e01[:, h, 0:128], tk[:, h, 50:51], None, ALU.mult),
                         reads=[("e01", h, 0), ("tk", h, 5)], writes=[("e01", h, 0)])
                    P.op("act", lambda e: e.activation(e01[:, h, 128:256], sc[:, 128:256], AF.Exp, bias=tk[:, h, 52:53], scale=1.0),
                         reads=["sc", ("tk", h, 7)], writes=[("e01", h, 1)])
                n_pr = 0
                for q8 in range(16):
                    wsl = W[:, q8 * 1024:(q8 + 1) * 1024]
                    for h in range(8):
                        pr = prod[n_pr % 2]
                        t2 = tmp2[n_pr % 2]
                        pk = "prod%d" % (n_pr % 2)
                        tk2 = "tmp2%d" % (n_pr % 2)
                        n_pr += 1
                        P.op("pool", lambda e: e.tensor_tensor(pr[:].rearrange("p (a b) -> p a b", a=8),
                                                               e01[:, h, q8 * 8:(q8 + 1) * 8].unsqueeze(2).broadcast_to([128, 8, 128]),
                                                               e01[:, h, 128:256].unsqueeze(1).broadcast_to([128, 8, 128]), ALU.mult),
                             reads=[("e01", h)], writes=[pk])
                        if h == 0:
                            P.op("dve", lambda e: e.scalar_tensor_tensor(wsl, pr[:], tk[:, h, 54:55], pr[:], ALU.is_ge, ALU.mult),
                                 reads=[pk, ("tk", h, 8)], writes=[("W", q8)])
                        else:
                            P.op("dve", lambda e: e.scalar_tensor_tensor(t2[:], pr[:], tk[:, h, 54:55], pr[:], ALU.is_ge, ALU.mult),
                                 reads=[pk, ("tk", h, 8)], writes=[tk2])
                            P.op("dve", lambda e: e.tensor_tensor(wsl, wsl, t2[:], ALU.add), reads=[tk2, ("W", q8)], writes=[("W", q8)])
                for et in range(32):
                    k2 = n_et % 2
                    n_et += 1
                    P.dma(ub[k2][:], self.UB[et], reads=[("UB", et)], writes=["ub%d" % k2])
                    P.dma(vb[k2][:], self.VB[et], reads=[("VB", et)], writes=["vb%d" % k2])
                    ab = 4 + k2
                    for dc in range(8):
                        self.mm(self.B[ab][:, :], self.hT[:, dc, :], ub[k2][:, dc, :], dc == 0, dc == 7, ["hT", "ub%d" % k2], ["B%d" % ab])
                    P.op("act", lambda e: e.activation(gA[k2][:], self.B[ab][:, :], AF.Gelu_apprx_tanh), reads=["B%d" % ab], writes=["gA%d" % k2])
                    P.op("dve", lambda e: e.tensor_tensor(G[k2][:], gA[k2][:], W[:, et * 512:(et + 1) * 512], ALU.mult),
                         reads=["gA%d" % k2, ("W", et // 2)], writes=["G%d" % k2])
                    gb_ = self.bank_bf(1 + k2)
                    gk = "B%d" % (1 + k2)
                    for c in range(4):
                        self.tr(gb_[:, c * 128:(c + 1) * 128], G[k2][:, c * 128:(c + 1) * 128], self.identb[:], ["G%d" % k2, "identb"], [gk])
                    P.op("act", lambda e: e.copy(GT[k2][:], gb_[:, 0:512].rearrange("p (c t) -> p c t", c=4)), reads=[gk], writes=["GT%d" % k2])
                    for c in range(4):
                        for half in range(2):
                            self.mm(self.B[6 + half][:, :], GT[k2][:, c, :], vb[k2][:, c, half * 512:(half + 1) * 512],
                                    et == 0 and c == 0, et == 31 and c == 3, ["GT%d" % k2, "vb%d" % k2], ["B%d" % (6 + half)])
                for half in range(2):
                    P.op("dve", lambda e: e.tensor_tensor(self.ntmp[:, half * 512:(half + 1) * 512], self.B[6 + half][:, :],
                                                          self.GTB[:, half * 512:(half + 1) * 512], ALU.mult),
                         reads=["B%d" % (6 + half), "GTB"], writes=[("ntmp", half)])
                    P.op("pool", lambda e: e.tensor_tensor(self.X[:, tb, half * 512:(half + 1) * 512], self.X[:, tb, half * 512:(half + 1) * 512],
                                                           self.ntmp[:, half * 512:(half + 1) * 512], ALU.add),
                         reads=[("ntmp", half), ("X", tb)], writes=[("X", tb)])
            P.barrier()


_CACHE = {}


def make_in_maps(inputs):
    consts = host_consts()
    shared = {}
    f = lambda a: np.ascontiguousarray(np.asarray(a, dtype=np.float32))
    shared["ada_w"] = f(inputs["ada_w"])
    shared["ada_b"] = f(inputs["ada_b"]).reshape(1, -1)
    shared["norm_g"] = f(inputs["norm_g"]).reshape(1, -1)
    for k in ("even_w_in", "even_b_f", "even_q_g", "even_k_g", "even_w_out", "odd_w_in", "odd_b_gate", "odd_q_g",
              "odd_cmp_pos", "odd_cmp_w1", "odd_cmp_w2", "odd_w_out", "rel_table", "peer_w_q", "peer_keys", "peer_v"):
        shared[k] = f(inputs[k])
    shared["even_conv_w"] = f(inputs["even_conv_w"]).reshape(2, -1)
    shared["odd_k_g"] = f(inputs["odd_k_g"]).reshape(2, -1)
    shared["peer_ut"] = np.ascontiguousarray(np.transpose(f(inputs["peer_u"]), (0, 2, 1)))
    shared.update(consts)
    x = f(inputs["x"])
    c = f(inputs["c"])
    maps = []
    for b in range(8):
        m = dict(shared)
        m["x"] = x[b]
        m["c"] = c[b:b + 1]
        maps.append(m)
    return maps


def kernel(**inputs):
    if "nc" not in _CACHE:
        _CACHE["nc"] = Builder().build()
    nc = _CACHE["nc"]
    maps = make_in_maps(inputs)
    res = run_bass_kernel_spmd(nc, maps, core_ids=list(range(8)))
    return np.stack([np.asarray(r["y"], dtype=np.float32) for r in res.results], axis=0)
```

```python
import math
from contextlib import ExitStack
import numpy as np
import concourse.bass as bass
import concourse.mybir as mybir
from concourse.bass_utils import run_bass_kernel_spmd

F32 = mybir.dt.float32
BF16 = mybir.dt.bfloat16
AF = mybir.ActivationFunctionType
ALU = mybir.AluOpType
AX = mybir.AxisListType

S = 2048
D = 1024
NT = 16
BIG = 240000.0
SEM_ROT = 12000


def _conflict(a, b):
    n = min(len(a), len(b))
    return a[:n] == b[:n]


class _Eng:
    def __init__(self, P, name, eng, same_wait):
        self.P = P
        self.name = name
        self.eng = eng
        self.same_wait = same_wait
        self.sem = None
        self.count = 0
        self.waited = {}

    def new_sem(self):
        self.sem = self.P.alloc_sem(self.name)
        self.count = 0


class Prog:
    def __init__(self, nc, stack, n_dma_sems=4):
        self.nc = nc
        self.stack = stack
        self.nsem = 0
        self.engs = {}
        for name, eng, sw in (("pe", nc.tensor, False), ("dve", nc.vector, True),
                              ("act", nc.scalar, True), ("pool", nc.gpsimd, True),
                              ("sp", nc.sync, False)):
            e = _Eng(self, name, eng, sw)
            e.new_sem()
            self.engs[name] = e
        self.dq = {}
        for q in ("sp", "pool"):
            self.dq[q] = {"sems": [self.alloc_sem("d" + q) for _ in range(n_dma_sems)],
                          "vals": [0] * n_dma_sems, "n": 0}
        self.state = {}
        self.out_events = []
        self.ninst = 0

    def alloc_sem(self, name):
        self.nsem += 1
        return self.stack.enter_context(self.nc.semaphore("s%s%d" % (name, self.nsem)))

    def _deps(self, reads, writes):
        deps = []
        for k in reads:
            for k2, st in self.state.get(k[0], {}).items():
                if st[0] is not None and _conflict(k, k2):
                    deps.append(st[0])
        for k in writes:
            for k2, st in self.state.get(k[0], {}).items():
                if _conflict(k, k2):
                    if st[0] is not None:
                        deps.append(st[0])
                    deps.extend(st[1])
        return deps

    def _record(self, ev, reads, writes):
        for k in reads:
            d = self.state.setdefault(k[0], {})
            st = d.setdefault(k, [None, []])
            st[1] = [e for e in st[1] if e[0] is not ev[0]] + [ev]
        for k in writes:
            d = self.state.setdefault(k[0], {})
            for k2 in [k2 for k2 in d if len(k2) > len(k) and k2[:len(k)] == k]:
                del d[k2]
            d[k] = [ev, []]

    def _wait(self, E, deps):
        need = {}
        for (sem, val, owner) in deps:
            if owner is E and not E.same_wait:
                continue
            if E.waited.get(id(sem), 0) >= val:
                continue
            if need.get(id(sem), (None, 0))[1] < val:
                need[id(sem)] = (sem, val)
        for sem, val in need.values():
            E.eng.wait_ge(sem, val)
            E.waited[id(sem)] = val

    @staticmethod
    def _keys(ks):
        return [k if isinstance(k, tuple) else (k,) for k in ks]

    def op(self, engname, fn, reads=(), writes=()):
        E = self.engs[engname]
        reads = self._keys(reads)
        writes = self._keys(writes)
        self._wait(E, self._deps(reads, writes))
        if E.count >= SEM_ROT:
            E.new_sem()
        inst = fn(E.eng)
        inst.then_inc(E.sem, 1)
        E.count += 1
        self.ninst += 1
        ev = (E.sem, E.count, E)
        self._record(ev, reads, writes)
        return ev

    def dma(self, out, in_, reads=(), writes=(), q="sp", is_output=False, **kw):
        E = self.engs[q]
        Q = self.dq[q]
        reads = self._keys(reads)
        writes = self._keys(writes)
        i = Q["n"] % len(Q["sems"])
        Q["n"] += 1
        sem = Q["sems"][i]
        deps = self._deps(reads, writes)
        if Q["vals"][i] > 0:
            deps.append((sem, Q["vals"][i], None))
        self._wait(E, deps)
        inst = E.eng.dma_start(out=out, in_=in_, **kw)
        Q["vals"][i] += 16
        inst.then_inc(sem, 16)
        self.ninst += 1
        ev = (sem, Q["vals"][i], None)
        self._record(ev, reads, writes)
        if is_output:
            self.out_events.append(ev)
        return ev

    def _all_events(self):
        evs = []
        for e in self.engs.values():
            if e.count > 0:
                evs.append((e.sem, e.count, e))
        for Q in self.dq.values():
            for sem, v in zip(Q["sems"], Q["vals"]):
                if v > 0:
                    evs.append((sem, v, None))
        return evs

    def barrier(self):
        evs = self._all_events()
        for E in self.engs.values():
            self._wait(E, [ev for ev in evs if ev[2] is not E])
        self.state = {}

    def finish(self):
        E = self.engs["sp"]
        self._wait(E, self.out_events + [ev for ev in self._all_events() if ev[2] is not E])


def host_consts():
    c = {}
    dist = np.arange(2048)
    nf = np.maximum(dist, 1).astype(np.float32)
    large = 16 + (np.log(nf / np.float32(16)) / np.float32(math.log(8.0)) * np.float32(16)).astype(np.int32)
    large = np.minimum(large, 31)
    bucket = np.where(dist < 16, dist, large)
    ohb = np.zeros((32, 2048), np.float32)
    ohb[bucket, dist] = 1.0
    c["k_ohb"] = ohb
    p = np.arange(128)[:, None, None]
    i = np.arange(16)[None, :, None]
    m = np.arange(32)[None, None, :]
    t = 128 * i + p
    cur = t // 64
    forced = (m == 0) | (m == cur) | (m == cur - 1)
    allowed = (64 * m <= t)
    c["k_a1"] = (allowed & ~forced).astype(np.float32)
    c["k_a0"] = np.where(forced, 1e6, np.where(allowed, 0.0, -1.0)).astype(np.float32)
    mm = np.arange(32)[:, None, None]
    jj = np.arange(16)[None, :, None]
    sp = np.arange(128)[None, None, :]
    c["k_ej"] = (mm == 2 * jj + sp // 64).astype(np.float32)
    starts = (np.arange(127) * 16)[:, None]
    bstart = (np.arange(32) * 64)[None, :]
    c["k_ovl"] = ((starts < bstart + 64) & (starts + 32 > bstart)).astype(np.float32)
    s_ = np.arange(128)[:, None]
    t_ = np.arange(128)[None, :]
    c["k_caus"] = np.where(s_ > t_, -BIG, 0.0).astype(np.float32)
    sel8 = np.zeros((8, 8, 128), np.float32)
    for h in range(8):
        sel8[h, h, :] = 1.0
    c["k_sel8"] = sel8
    shm = np.zeros((128, 4, 128), np.float32)
    shm[:, 0, :] = (s_ == t_ - 1)
    shm[:, 1, :] = (s_ == t_ - 2)
    shm[127, 2, 0] = 1.0
    shm[126, 3, 0] = 1.0
    shm[127, 3, 1] = 1.0
    c["k_shm"] = shm
    return c


CONST_SHAPES = {"k_ohb": [32, 2048], "k_a1": [128, 16, 32], "k_a0": [128, 16, 32], "k_ej": [32, 16, 128],
                "k_ovl": [127, 32], "k_caus": [128, 128], "k_sel8": [8, 8, 128], "k_shm": [128, 4, 128]}

IN_SHAPES = {
    "x": [S, D], "c": [1, D], "ada_w": [4, D, 6 * D], "ada_b": [1, 4 * 6 * D], "norm_g": [1, 4 * 2 * D],
    "even_w_in": [2, D, 3080], "even_b_f": [2, 8], "even_conv_w": [2, 3 * 512], "even_q_g": [2, 64],
    "even_k_g": [2, 64], "even_w_out": [2, D, D], "odd_w_in": [2, D, 2608], "odd_b_gate": [2, 48],
    "odd_q_g": [2, 64], "odd_k_g": [2, 3 * 64], "odd_cmp_pos": [2, 2, 32, 64], "odd_cmp_w1": [2, 2, 2048, 64],
    "odd_cmp_w2": [2, 2, 64, 64], "odd_w_out": [2, D, D], "rel_table": [32, 16], "peer_w_q": [4, D, D],
    "peer_keys": [4, 2, 128, 64], "peer_ut": [4, D, 16384], "peer_v": [4, 16384, D],
}


class Builder:
    def __init__(self, n_layers=4, stop=None, peer=True, snaps=False):
        self.snaps = snaps
        self.snap_names = []
        self.n_layers = n_layers
        self.stop = stop
        self.do_peer = peer
        nc = self.nc = bass.Bass("TRN2", target_bir_lowering=False)
        self.I = {}
        for k, shp in list(IN_SHAPES.items()) + list(CONST_SHAPES.items()):
            self.I[k] = nc.dram_tensor(k, list(shp), F32, kind="ExternalInput").ap()
        self.y_out = nc.dram_tensor("y", [S, D], F32, kind="ExternalOutput").ap()
        self.MODS = nc.dram_tensor("mods_s", [4, 6, D], F32, kind="Internal").ap()
        self.FV = nc.dram_tensor("fv_s", [16, 4096], BF16, kind="Internal").ap()
        self.FW = nc.dram_tensor("fw_s", [16, 4096], BF16, kind="Internal").ap()
        self.UB = nc.dram_tensor("ub_s", [32, 128, 8, 512], BF16, kind="Internal").ap()
        self.VB = nc.dram_tensor("vb_s", [32, 128, 4, 1024], BF16, kind="Internal").ap()

    def T(self, st, name, shape, dt):
        self._tn = getattr(self, "_tn", 0) + 1
        return st.enter_context(self.nc.sbuf_tensor("%s_%d" % (name, self._tn), list(shape), dt))

    def mm(self, out, lhsT, rhs, start, stop, reads, writes, skip=False):
        self.P.op("pe", lambda e: e.matmul(out, lhsT, rhs, start=start, stop=stop, skip_group_check=skip),
                  reads=reads, writes=writes)

    def tr(self, out, in_, ident, reads, writes):
        self.P.op("pe", lambda e: e.transpose(out, in_, ident), reads=reads, writes=writes)

    def bank_bf(self, b):
        return self.B[b][:].bitcast(BF16)

    def build(self):
        nc = self.nc
        with ExitStack() as g:
            P = self.P = Prog(nc, g)
            self.B = [g.enter_context(nc.psum_tensor("B%d" % i, [128, 512], F32)) for i in range(8)]
            self.X = self.T(g, "X", [128, NT, D], F32)
            self.identf = self.T(g, "identf", [128, 128], F32)
            self.identb = self.T(g, "identb", [128, 128], BF16)
            self.antib = self.T(g, "antib", [128, 128], BF16)
            self.anti127 = self.T(g, "anti127", [128, 128], BF16)
            self.ones_row = self.T(g, "ones_row", [1, 128], BF16)
            self.one11 = self.T(g, "one11", [1, 1], F32)
            self.GB = self.T(g, "GB", [128, D], BF16)
            self.SHB = self.T(g, "SHB", [128, D], BF16)
            self.GTB = self.T(g, "GTB", [128, D], BF16)
            self.onec = self.T(g, "onec", [128, 1], F32)
            self.onesf = self.T(g, "onesf", [8, 128], F32)
            self.rd = self.T(g, "rd", [128, 32], F32)
            self.ntmp = self.T(g, "ntmp", [128, D], F32)
            self.hbf = self.T(g, "hbf", [128, D], BF16)
            self.hT = self.T(g, "hT", [128, 8, 128], BF16)
            self.sm = self.T(g, "sm", [128, 64], F32)
            tmpi = self.T(g, "tmpi", [128, 128], F32)

            P.op("pool", lambda e: e.iota(tmpi[:], [[1, 128]], base=0, channel_multiplier=-1,
                                          allow_small_or_imprecise_dtypes=True), writes=["tmpi"])
            P.op("dve", lambda e: e.tensor_scalar(self.identf[:], tmpi[:], 0.0, None, ALU.is_equal), reads=["tmpi"], writes=["identf"])
            P.op("dve", lambda e: e.tensor_scalar(self.identb[:], tmpi[:], 0.0, None, ALU.is_equal), reads=["tmpi"], writes=["identb"])
            P.op("pool", lambda e: e.iota(tmpi[:], [[1, 128]], base=-127, channel_multiplier=1,
                                          allow_small_or_imprecise_dtypes=True), reads=["identb", "identf"], writes=["tmpi"])
            P.op("dve", lambda e: e.tensor_scalar(self.antib[:], tmpi[:], 0.0, None, ALU.is_equal), reads=["tmpi"], writes=["antib"])
            P.op("dve", lambda e: e.tensor_scalar(self.anti127[:], tmpi[:], -1.0, None, ALU.is_equal), reads=["tmpi"], writes=["anti127"])
            P.op("pool", lambda e: e.memset(self.ones_row[:], 1.0), writes=["ones_row"])
            P.op("pool", lambda e: e.memset(self.one11[:], 1.0), writes=["one11"])
            P.op("pool", lambda e: e.memset(self.onec[:], 1.0), writes=["onec"])
            P.op("pool", lambda e: e.memset(self.onesf[:], 1.0), writes=["onesf"])

            for tb in range(NT):
                P.dma(self.X[:, tb, :], self.I["x"][tb * 128:(tb + 1) * 128, :], writes=[("X", tb)])

            self.adaln()
            P.barrier()
            done = False
            tables_ready = False
            for l in range(self.n_layers):
                if l % 2 == 0:
                    self.even_layer(l)
                else:
                    if not tables_ready:
                        self.rel_tables()
                        tables_ready = True
                    self.odd_layer(l)
                self.snapshot("xm_%d" % l)
                if self.stop == "L%dmix" % l:
                    break
                if self.do_peer:
                    self.peer_layer(l)
                    self.snapshot("x_%d" % l)
                if self.stop == "L%d" % l:
                    break
            P.barrier()
            for tb in range(NT):
                P.dma(self.y_out[tb * 128:(tb + 1) * 128, :], self.X[:, tb, :], reads=[("X", tb)], is_output=True)
            P.finish()
        return nc

    def snapshot(self, name):
        if not self.snaps:
            return
        t = self.nc.dram_tensor("snap_" + name, [S, D], F32, kind="ExternalOutput").ap()
        self.snap_names.append(name)
        for tb in range(NT):
            self.P.dma(t[tb * 128:(tb + 1) * 128, :], self.X[:, tb, :], reads=[("X", tb)], is_output=True)

    def adaln(self):
        P = self.P
        I = self.I
        with ExitStack() as st:
            crow = self.T(st, "crow", [1, D], F32)
            srow = self.T(st, "srow", [1, D], F32)
            scol = self.T(st, "scol", [128, 8], F32)
            brow = self.T(st, "brow", [1, 6 * D], F32)
            grow = self.T(st, "grow", [1, 2 * D], F32)
            mrow = self.T(st, "mrow", [1, 6 * D], F32)
            wts = [self.T(st, "adw%d" % k, [128, 8, 512], F32) for k in range(2)]
            P.dma(crow[:], I["c"], writes=["crow"])
            P.op("act", lambda e: e.activation(srow[:], crow[:], AF.Silu), reads=["crow"], writes=["srow"])
            ps = self.B[0]
            for dc in range(8):
                self.mm(ps[:, dc:dc + 1], srow[0:1, dc * 128:(dc + 1) * 128], self.one11[:], True, True,
                        ["srow", "one11"], [("B0", dc)])
            P.op("dve", lambda e: e.tensor_copy(scol[:], ps[:, 0:8]), reads=["B0"], writes=["scol"])
            n = 0
            for l in range(self.n_layers):
                P.dma(brow[:], I["ada_b"][0:1, l * 6144:(l + 1) * 6144], writes=["brow"])
                P.dma(grow[:], I["norm_g"][0:1, l * 2048:(l + 1) * 2048], writes=["grow"])
                for nt in range(12):
                    wt = wts[n % 2]
                    wk = "adw%d" % (n % 2)
                    P.dma(wt[:], I["ada_w"][l, :, nt * 512:(nt + 1) * 512].rearrange("(c p) n -> p c n", p=128), writes=[wk])
                    pb = self.B[1 + n % 2]
                    pk = "B%d" % (1 + n % 2)
                    for dc in range(8):
                        self.mm(pb[0:1, :], scol[:, dc:dc + 1], wt[:, dc, :], dc == 0, dc == 7, ["scol", wk], [pk])
                    P.op("dve", lambda e: e.tensor_tensor(mrow[0:1, nt * 512:(nt + 1) * 512], pb[0:1, :],
                                                          brow[0:1, nt * 512:(nt + 1) * 512], ALU.add),
                         reads=[pk, "brow"], writes=[("mrow", nt)])
                    n += 1
                for k in range(2):
                    sc = mrow[0:1, (3 * k + 1) * D:(3 * k + 2) * D]
                    ng = grow[0:1, k * D:(k + 1) * D]
                    P.op("dve", lambda e: e.scalar_tensor_tensor(sc, sc, 1.0, ng, ALU.add, ALU.mult), reads=["mrow", "grow"], writes=["mrow"])
                P.dma(self.MODS[l].rearrange("k d -> (k d)").unsqueeze(0), mrow[:], reads=["mrow"], writes=[("MODS", l)])

    def load_mods(self, l, k):
        P = self.P
        for j, (t, nm) in zip((1, 0, 2), ((self.GB, "GB"), (self.SHB, "SHB"), (self.GTB, "GTB"))):
            P.dma(t[:], self.MODS[l, 3 * k + j:3 * k + j + 1, :].broadcast_to([128, D]), reads=[("MODS", l)], writes=[nm], q="pool")

    def norm_hT(self, tb):
        P = self.P
        xt = self.X[:, tb, :]
        sm = self.sm
        P.op("act", lambda e: e.activation(self.ntmp[:], xt, AF.Square, accum_out=sm[:, 0:1]), reads=[("X", tb)], writes=["ntmp", ("sm", 0)])
        P.op("dve", lambda e: e.tensor_scalar(sm[:, 1:2], sm[:, 0:1], 1.0 / D, 1e-6, ALU.mult, ALU.add), reads=[("sm", 0)], writes=[("sm", 1)])
        P.op("act", lambda e: e.activation(sm[:, 2:3], sm[:, 1:2], AF.Sqrt), reads=[("sm", 1)], writes=[("sm", 2)])
        P.op("dve", lambda e: e.reciprocal(sm[:, 3:4], sm[:, 2:3]), reads=[("sm", 2)], writes=[("sm", 3)])
        P.op("dve", lambda e: e.scalar_tensor_tensor(self.ntmp[:], xt, sm[:, 3:4], self.GB[:], ALU.mult, ALU.mult),
             reads=[("X", tb), ("sm", 3), "GB"], writes=["ntmp"])
        P.op("pool", lambda e: e.tensor_tensor(self.hbf[:], self.ntmp[:], self.SHB[:], ALU.add), reads=["ntmp", "SHB"], writes=["hbf"])
        bt = self.bank_bf(0)
        for c in range(8):
            self.tr(bt[:, c * 128:(c + 1) * 128], self.hbf[:, c * 128:(c + 1) * 128], self.identb[:], ["hbf", "identb"], [("B0", c)])
        P.op("act", lambda e: e.copy(self.hT[:], bt[:, 0:1024].rearrange("p (c t) -> p c t", c=8)), reads=["B0"], writes=["hT"])

    def proj(self, bank, ncols, w, wkey, c0):
        for dc in range(8):
            self.mm(self.B[bank][:, 0:ncols], self.hT[:, dc, :], w[:, dc, c0:c0 + ncols], dc == 0, dc == 7,
                    ["hT", wkey], ["B%d" % bank])

    def head_rmsnorm(self, src, srckey, nh, gb, gbkey, out_ap, outkey, sq, rs, npart=128):
        P = self.P
        n = nh * 64
        P.op("act", lambda e: e.activation(sq[:, 0:n], src, AF.Square), reads=[srckey], writes=["sq"])
        P.op("dve", lambda e: e.tensor_reduce(rs[:, 0:nh], sq[:, 0:n].rearrange("p (h d) -> p h d", d=64), AX.X, ALU.add), reads=["sq"], writes=[("rs", 0)])
        P.op("dve", lambda e: e.tensor_scalar(rs[:, 16:16 + nh], rs[:, 0:nh], 1.0 / 64, 1e-6, ALU.mult, ALU.add), reads=[("rs", 0)], writes=[("rs", 1)])
        P.op("act", lambda e: e.activation(rs[:, 32:32 + nh], rs[:, 16:16 + nh], AF.Sqrt), reads=[("rs", 1)], writes=[("rs", 2)])
        P.op("dve", lambda e: e.reciprocal(rs[:, 48:48 + nh], rs[:, 32:32 + nh]), reads=[("rs", 2)], writes=[("rs", 3)])
        P.op("dve", lambda e: e.tensor_tensor(sq[:, 0:n].rearrange("p (h d) -> p h d", d=64), src.rearrange("p (h d) -> p h d", d=64),
                                              rs[:, 48:48 + nh].unsqueeze(2).broadcast_to([npart, nh, 64]), ALU.mult),
             reads=[srckey, ("rs", 3), "sq"], writes=["sq"])
        P.op("pool", lambda e: e.tensor_tensor(out_ap, sq[:, 0:n].rearrange("p (h d) -> p h d", d=64),
                                               gb.unsqueeze(1).broadcast_to([npart, nh, 64]), ALU.mult),
             reads=["sq", gbkey], writes=[outkey])

    def run_attn(self, tasks):
        P = self.P

        def emit_qk(n, t):
            bi = 5 + n % 2
            sb = self.B[bi]
            key = "B%d" % bi
            for (c0, w, nsub, mms) in t["groups"]:
                out = sb[0:t["nrow"], c0:c0 + w]
                if nsub > 1:
                    out = out.rearrange("p (a b) -> p a b", a=nsub)
                for k, (lhsT, rhs, rd) in enumerate(mms):
                    self.mm(out, lhsT, rhs, k == 0, k == len(mms) - 1, rd, [key])

        def emit_rest(n, t):
            bi = 5 + n % 2
            sb = self.B[bi]
            key = "B%d" % bi
            pt = self.PT[n % 3]
            pk = ("PT", n % 3)
            nr, wd = t["nrow"], t["width"]
            exps = t.get("exps") or [(0, wd, None, [])]
            for (c0, w, bias, rd) in exps:
                if bias is None:
                    P.op("act", lambda e: e.activation(pt[0:nr, c0:c0 + w], sb[0:nr, c0:c0 + w], AF.Exp, scale=0.125), reads=[key], writes=[pk])
                else:
                    P.op("act", lambda e: e.activation(pt[0:nr, c0:c0 + w], sb[0:nr, c0:c0 + w], AF.Exp, bias=bias, scale=0.125),
                         reads=[key] + rd, writes=[pk])
            for (pc0, pw, rhs, out, outkey, start, stop, rd) in t["pv"]:
                self.mm(out, pt[0:nr, pc0:pc0 + pw], rhs, start, stop, [pk] + rd, [outkey], skip=True)

        if not tasks:
            return
        emit_qk(0, tasks[0])
        for n, t in enumerate(tasks):
            if n + 1 < len(tasks):
                emit_qk(n + 1, tasks[n + 1])
            emit_rest(n, t)

    def out_proj_residual(self, i, wout):
        P = self.P
        Obf = self.hbf
        bt = self.bank_bf(0)
        for c in range(8):
            self.tr(bt[:, c * 128:(c + 1) * 128], Obf[:, c * 128:(c + 1) * 128], self.identb[:], ["hbf", "identb"], [("B0", c)])
        P.op("act", lambda e: e.copy(self.OT[:], bt[:, 0:1024].rearrange("p (c t) -> p c t", c=8)), reads=["B0"], writes=["OT"])
        for half in range(2):
            bk = 3 + half
            for fc in range(8):
                self.mm(self.B[bk][:, :], self.OT[:, fc, :], wout[:, fc, half * 512:(half + 1) * 512], fc == 0, fc == 7,
                        ["OT", "wout"], ["B%d" % bk])
            P.op("dve", lambda e: e.tensor_tensor(self.ntmp[:, half * 512:(half + 1) * 512], self.B[bk][:, :],
                                                  self.GTB[:, half * 512:(half + 1) * 512], ALU.mult),
                 reads=["B%d" % bk, "GTB"], writes=[("ntmp", half)])
            P.op("pool", lambda e: e.tensor_tensor(self.X[:, i, half * 512:(half + 1) * 512], self.X[:, i, half * 512:(half + 1) * 512],
                                                   self.ntmp[:, half * 512:(half + 1) * 512], ALU.add),
                 reads=[("ntmp", half), ("X", i)], writes=[("X", i)])

    def even_layer(self, l):
        P = self.P
        I = self.I
        li = l // 2
        w_in = I["even_w_in"][li]
        self.load_mods(l, 0)
        with ExitStack() as lay:
            kT = self.T(lay, "kT", [128, 4, S], BF16)
            Vp = self.T(lay, "Vp", [128, NT, 8, 65], BF16)
            cTT = self.T(lay, "cTT", [128, NT, 8], F32)
            rbc = self.T(lay, "rbc", [128, NT, 8], F32)
            bcol = self.T(lay, "bcol", [128, 8, NT], F32)
            kgb = self.T(lay, "kgb", [128, 64], F32)
            qgb = self.T(lay, "qgb", [128, 64], F32)
            sq = self.T(lay, "sq", [128, 1024], F32)
            rs = self.T(lay, "rs", [128, 64], F32)
            kn = self.T(lay, "kn", [128, 512], BF16)
            qT = self.T(lay, "qT", [128, 4, 128], BF16)
            self.OT = self.T(lay, "OT", [128, 8, 128], BF16)
            self.PT = [self.T(lay, "PT%d" % k, [128, 512], BF16) for k in range(3)]
            P.dma(kgb[:], I["even_k_g"][li:li + 1, :].broadcast_to([128, 64]), writes=["kgb"])
            P.dma(qgb[:], I["even_q_g"][li:li + 1, :].broadcast_to([128, 64]), writes=["qgb"])
            P.op("pool", lambda e: e.memset(Vp[:], 1.0), writes=["Vp"])
            with ExitStack() as ph:
                wkv = self.T(ph, "wkv", [128, 8, 1032], BF16)
                fT = self.T(ph, "fT", [8, S], F32)
                cT = self.T(ph, "cT", [8, S], F32)
                rsel = self.T(ph, "rsel", [8, NT, 8], F32)
                fcol = self.T(ph, "fcol", [128, 8], F32)
                negb = self.T(ph, "negb", [8, 1], F32)
                P.dma(wkv[:], w_in[:, 2048:3080].rearrange("(c p) n -> p c n", p=128), writes=["wkv"], q="pool")
                P.dma(negb[:], I["even_b_f"][li].rearrange("(h o) -> h o", o=1), writes=["negb"])
                P.op("dve", lambda e: e.tensor_scalar(negb[:], negb[:], -1.0, None, ALU.mult), reads=["negb"], writes=["negb"])
                for tb in range(NT):
                    self.norm_hT(tb)
                    self.proj(1, 512, wkv, "wkv", 0)
                    self.proj(2, 512, wkv, "wkv", 512)
                    self.proj(3, 8, wkv, "wkv", 1024)
                    self.head_rmsnorm(self.B[1][:, :], "B1", 8, kgb[:], "kgb", kn[:].rearrange("p (h d) -> p h d", d=64), "kn", sq, rs)
                    b4 = self.bank_bf(4)
                    for c in range(4):
                        self.tr(b4[:, c * 128:(c + 1) * 128], kn[:, c * 128:(c + 1) * 128], self.identb[:], ["kn", "identb"], [("B4", c)])
                    P.op("act", lambda e: e.copy(kT[:, :, tb * 128:(tb + 1) * 128], b4[:, 0:512].rearrange("p (c t) -> p c t", c=4)),
                         reads=["B4"], writes=[("kT", tb)])
                    P.op("act", lambda e: e.copy(Vp[:, tb, :, 0:64], self.B[2][:, :].rearrange("p (h d) -> p h d", d=64)),
                         reads=["B2"], writes=[("Vp", tb)])
                    P.op("dve", lambda e: e.tensor_copy(fcol[:], self.B[3][:, 0:8]), reads=["B3"], writes=["fcol"])
                    self.tr(self.B[7][0:8, 0:128], fcol[:], self.identf[:], ["fcol", "identf"], ["B7"])
                    P.op("act", lambda e: e.copy(fT[:, tb * 128:(tb + 1) * 128], self.B[7][0:8, 0:128]), reads=["B7"], writes=[("fT", tb)])
                P.op("act", lambda e: e.activation(fT[:], fT[:], AF.Exp, bias=negb[:], scale=-1.0), reads=["fT", "negb"], writes=["fT"])
                P.op("act", lambda e: e.activation(fT[:], fT[:], AF.Ln, bias=1.0, scale=1.0), reads=["fT"], writes=["fT"])
                P.op("dve", lambda e: e.tensor_scalar(fT[:], fT[:], -1.0, None, ALU.mult), reads=["fT"], writes=["fT"])
                P.op("dve", lambda e: e.tensor_tensor_scan(cT[:], self.onec[0:8, 0:1].broadcast_to([8, S]), fT[:], 0.0, ALU.mult, ALU.add),
                     reads=["onec", "fT"], writes=["cT"])
                for j in range(NT):
                    self.tr(self.B[1][:, j * 8:(j + 1) * 8], cT[:, j * 128:(j + 1) * 128], self.identf[0:8, 0:8], ["cT", "identf"], [("B1", j)])
                P.op("dve", lambda e: e.tensor_copy(cTT[:].rearrange("p j h -> p (j h)"), self.B[1][:, 0:128]), reads=["B1"], writes=["cTT"])
                P.op("dve", lambda e: e.tensor_tensor(rsel[:], cT[:, 64:64 + 128 * 15 + 1:128].unsqueeze(2).broadcast_to([8, NT, 8]),
                                                      self.identf[0:8, 0:8].unsqueeze(1).broadcast_to([8, NT, 8]), ALU.mult),
                     reads=["cT", "identf"], writes=["rsel"])
                self.mm(self.B[2][:, 0:128], self.onesf[:], rsel[:].rearrange("p i h -> p (i h)"), True, True, ["onesf", "rsel"], ["B2"])
                P.op("dve", lambda e: e.tensor_copy(rbc[:].rearrange("p i h -> p (i h)"), self.B[2][:, 0:128]), reads=["B2"], writes=["rbc"])
                P.barrier()
            with ExitStack() as ph:
                wq2 = self.T(ph, "wq2", [128, 8, 2048], BF16)
                wout = self.T(ph, "wout", [128, 8, D], BF16)
                cwb = self.T(ph, "cwb", [128, 3, 512], BF16)
                shm = self.T(ph, "shm", [128, 4, 128], BF16)
                caus = self.T(ph, "caus", [128, 128], BF16)
                uw = [self.T(ph, "uw%d" % k, [128, 3, 512], BF16) for k in range(2)]
                ccs = sq[:, 0:512]
                cbs = sq[:, 512:1024]
                ucur = self.ntmp[:, 0:512]
                Obf = self.hbf
                P.dma(wq2[:], w_in[:, 0:2048].rearrange("(c p) n -> p c n", p=128), writes=["wq2"], q="pool")
                P.dma(wout[:], I["even_w_out"][li].rearrange("(c p) n -> p c n", p=128), writes=["wout"], q="pool")
                P.dma(cwb[:].rearrange("p k c -> p (k c)"), I["even_conv_w"][li:li + 1, :].broadcast_to([128, 1536]), writes=["cwb"], q="pool")
                P.dma(shm[:], I["k_shm"], writes=["shm"], q="pool")
                P.dma(caus[:], I["k_caus"], writes=["caus"], q="pool")
                for i in range(NT):
                    self.norm_hT(i)
                    for k in range(4):
                        self.proj(1 + k, 512, wq2, "wq2", 512 * k)
                    P.op("act", lambda e: e.copy(ccs, self.B[2][:, :]), reads=["B2"], writes=["sq"])
                    P.op("act", lambda e: e.copy(cbs, self.B[1][:, :]), reads=["B1"], writes=["sq"])
                    P.op("dve", lambda e: e.tensor_tensor(ucur, ccs, self.B[3][:, :], ALU.mult), reads=["sq", "B3"], writes=["ntmp"])
                    uwc, uwp = uw[i % 2], uw[(i + 1) % 2]
                    kc_, kp_ = "uw%d" % (i % 2), "uw%d" % ((i + 1) % 2)
                    P.op("pool", lambda e: e.tensor_tensor(uwc[:], ucur.unsqueeze(1).broadcast_to([128, 3, 512]), cwb[:], ALU.mult),
                         reads=["ntmp", "cwb"], writes=[kc_])
                    mms = [(self.identb[:], uwc[:, 2, :], [kc_]), (shm[:, 0, :], uwc[:, 1, :], [kc_, "shm"]), (shm[:, 1, :], uwc[:, 0, :], [kc_, "shm"])]
                    if i > 0:
                        mms += [(shm[:, 2, :], uwp[:, 1, :], [kp_, "shm"]), (shm[:, 3, :], uwp[:, 0, :], [kp_, "shm"])]
                    for k, (lt, rh, rd) in enumerate(mms):
                        self.mm(self.B[2][:, :], lt, rh, k == 0, k == len(mms) - 1, rd + ["identb"], ["B2"])
                    P.op("dve", lambda e: e.tensor_tensor(Obf[:, 0:512], cbs, self.B[2][:, :], ALU.mult), reads=["sq", "B2"], writes=["hbf"])
                    self.head_rmsnorm(self.B[4][:, :], "B4", 8, qgb[:], "qgb", kn[:].rearrange("p (h d) -> p h d", d=64), "kn", sq, rs)
                    b1 = self.bank_bf(1)
                    for c in range(4):
                        self.tr(b1[:, c * 128:(c + 1) * 128], kn[:, c * 128:(c + 1) * 128], self.identb[:], ["kn", "identb"], [("B1", c)])
                    P.op("act", lambda e: e.copy(qT[:], b1[:, 0:512].rearrange("p (c t) -> p c t", c=4)), reads=["B1"], writes=["qT"])
                    for h in range(8):
                        base = (h % 2) * 64
                        pr = h // 2
                        P.op("dve", lambda e: e.tensor_scalar(bcol[:, h, 0:i + 1], cTT[:, 0:i + 1, h], rbc[:, i, h:h + 1], -1.0, ALU.subtract, ALU.mult),
                             reads=["cTT", "rbc"], writes=[("bcol", h)])
                        tasks = []
                        oi = self.B[7][:, (h % 4) * 65:(h % 4) * 65 + 65]
                        oik = ("B7", h % 4)
                        for j0 in range(0, i + 1, 4):
                            js = list(range(j0, min(j0 + 4, i + 1)))
                            groups = []
                            exps = []
                            pv = []
                            for jj, j in enumerate(js):
                                mm_ = [(kT[base:base + 64, pr, j * 128:(j + 1) * 128], qT[base:base + 64, pr, :], ["kT", "qT"])]
                                if j == i:
                                    mm_.append((self.identb[:], caus[:], ["identb", "caus"]))
                                groups.append((jj * 128, 128, 1, mm_))
                                exps.append((jj * 128, 128, bcol[:, h, j:j + 1], [("bcol", h)]))
                                pv.append((jj * 128, 128, Vp[:, j, h, :], oi, oik, j == 0, j == i, ["Vp"]))
                            tasks.append(dict(nrow=128, width=len(js) * 128, groups=groups, exps=exps, pv=pv))
                        self.run_attn(tasks)
                        P.op("dve", lambda e: e.reciprocal(self.rd[:, h:h + 1], oi[:, 64:65]), reads=[oik], writes=[("rd", h)])
                        P.op("dve", lambda e: e.tensor_scalar(Obf[:, 512 + h * 64:512 + (h + 1) * 64], oi[:, 0:64], self.rd[:, h:h + 1], None, ALU.mult),
                             reads=[oik, ("rd", h)], writes=["hbf"])
                    self.out_proj_residual(i, wout)
                P.barrier()

    def rel_tables(self):
        P = self.P
        I = self.I
        with ExitStack() as st:
            tab = self.T(st, "tab", [32, 16], F32)
            ohb = self.T(st, "ohb", [32, 2048], F32)
            fvr = self.T(st, "fvr", [16, 4096], BF16)
            P.dma(tab[:], I["rel_table"], writes=["tab"])
            P.dma(ohb[:], I["k_ohb"], writes=["ohb"])
            P.op("pool", lambda e: e.memset(fvr[:], -BIG), writes=["fvr"])
            for q in range(4):
                self.mm(self.B[1][0:16, :], tab[:], ohb[:, q * 512:(q + 1) * 512], True, True, ["tab", "ohb"], ["B1"])
                P.op("dve", lambda e: e.tensor_scalar(fvr[:, 2048 + q * 512:2048 + (q + 1) * 512], self.B[1][0:16, :], 8.0, None, ALU.mult),
                     reads=["B1"], writes=["fvr"])
            P.dma(self.FV, fvr[:], reads=["fvr"], writes=["FV"])
            P.op("pool", lambda e: e.memset(fvr[:, 2048 + 512:4096], -BIG), reads=["fvr"], writes=["fvr"])
            P.dma(self.FW, fvr[:], reads=["fvr"], writes=["FW"])
            P.barrier()

    def odd_layer(self, l):
        P = self.P
        I = self.I
        li = l // 2
        w_in = I["odd_w_in"][li]
        rd = self.rd
        self.load_mods(l, 0)
        with ExitStack() as lay:
            ksT = self.T(lay, "ksT", [128, 2, S], BF16)
            kwT = self.T(lay, "kwT", [128, 2, S], BF16)
            Vs = self.T(lay, "Vs", [128, NT, 4, 65], BF16)
            Vw = self.T(lay, "Vw", [128, NT, 4, 65], BF16)
            KcT = self.T(lay, "KcT", [128, 2, 128], BF16)
            VcX = self.T(lay, "VcX", [128, 4, 97], BF16)
            gbs = self.T(lay, "gbs", [128, 4, 64], F32)
            sq = self.T(lay, "sq", [128, 1024], F32)
            rs = self.T(lay, "rs", [128, 64], F32)
            D0 = self.T(lay, "D0", [128, 16, 128], BF16)
            D1 = self.T(lay, "D1", [128, 16, 128], BF16)
            D4 = self.T(lay, "D4", [128, 16, 128], BF16)
            CROW = self.T(lay, "CROW", [1, 16, 128], BF16)
            for (t, nm, src, k) in ((D0, "D0", self.FV, 0), (D1, "D1", self.FV, 1), (D4, "D4", self.FW, 4)):
                ap = bass.AP(tensor=src.tensor, offset=2048 + 128 * k - 127, ap=[[1, 128], [4096, 16], [1, 128]])
                P.dma(t[:], ap, writes=[nm])
            ap = bass.AP(tensor=self.FV.tensor, offset=2048 + 1000, ap=[[0, 1], [4096, 16], [1, 128]])
            P.dma(CROW[:], ap, writes=["CROW"])
            P.dma(gbs[:, 0, :], I["odd_q_g"][li:li + 1, :].broadcast_to([128, 64]), writes=[("gbs", 0)])
            P.dma(gbs[:, 1:4, :].rearrange("p k d -> p (k d)"), I["odd_k_g"][li:li + 1, :].broadcast_to([128, 192]), writes=[("gbs", 1)])
            P.op("pool", lambda e: e.memset(Vs[:], 1.0), writes=["Vs"])
            P.op("pool", lambda e: e.memset(Vw[:], 1.0), writes=["Vw"])
            P.op("pool", lambda e: e.memset(VcX[:], 1.0), writes=["VcX"])
            with ExitStack() as ph:
                wkv = self.T(ph, "wkv", [128, 8, 1536], BF16)
                kcT = self.T(ph, "kcT", [128, 2, S], BF16)
                vcT = self.T(ph, "vcT", [128, 2, S], BF16)
                kvb = self.T(ph, "kvb", [128, 4, 256], BF16)
                w1b = [self.T(ph, "w1b%d" % a, [128, 32, 64], BF16) for a in range(2)]
                w2b = [self.T(ph, "w2b%d" % a, [64, 64], BF16) for a in range(2)]
                pos = self.T(ph, "pos", [32, 2, 64], F32)
                posT = self.T(ph, "posT", [64, 2, 32], BF16)
                cst = self.T(ph, "cst", [64, 2], F32)
                HT = self.T(ph, "HT", [64, 128], BF16)
                KcN = self.T(ph, "KcN", [128, 4, 64], BF16)
                ovl = self.T(ph, "ovl", [127, 32], F32)
                P.dma(wkv[:], w_in[:, 1024:2560].rearrange("(c p) n -> p c n", p=128), writes=["wkv"], q="pool")
                for a in range(2):
                    for hf in range(2):
                        P.dma(w1b[a][hf * 64:(hf + 1) * 64, :, :], I["odd_cmp_w1"][li, a].rearrange("(l d) o -> d l o", d=64),
                              writes=[("w1b%d" % a, hf)], q="pool")
                    P.dma(w2b[a][:], I["odd_cmp_w2"][li, a], writes=["w2b%d" % a], q="pool")
                    P.dma(pos[:, a, :], I["odd_cmp_pos"][li, a], writes=[("pos", a)])
                P.dma(ovl[:], I["k_ovl"], writes=["ovl"])
                P.op("dve", lambda e: e.tensor_copy(VcX[0:127, :, 65:97], ovl[:].unsqueeze(1).broadcast_to([127, 4, 32])),
                     reads=["ovl", "VcX"], writes=["VcX"])
                for tb in range(NT):
                    self.norm_hT(tb)
                    self.proj(1, 512, wkv, "wkv", 0)
                    self.proj(2, 512, wkv, "wkv", 512)
                    self.proj(3, 512, wkv, "wkv", 1024)
                    P.op("act", lambda e: e.copy(kvb[:, 0:2, :], self.B[1][:, :].rearrange("p (a n) -> p a n", a=2)), reads=["B1"], writes=[("kvb", 0)])
                    self.head_rmsnorm(self.B[2][:, 0:256], "B2", 4, gbs[:, 2, :], ("gbs", 1), kvb[:, 2, :].rearrange("p (h d) -> p h d", d=64), ("kvb", 2), sq, rs)
                    self.head_rmsnorm(self.B[3][:, 0:256], "B3", 4, gbs[:, 3, :], ("gbs", 1), kvb[:, 3, :].rearrange("p (h d) -> p h d", d=64), ("kvb", 3), sq, rs)
                    P.op("act", lambda e: e.copy(Vs[:, tb, :, 0:64], self.B[2][:, 256:512].rearrange("p (h d) -> p h d", d=64)), reads=["B2"], writes=[("Vs", tb)])
                    P.op("act", lambda e: e.copy(Vw[:, tb, :, 0:64], self.B[3][:, 256:512].rearrange("p (h d) -> p h d", d=64)), reads=["B3"], writes=[("Vw", tb)])
                    b4 = self.bank_bf(4)
                    for a in range(4):
                        for c in range(2):
                            self.tr(b4[:, (a * 2 + c) * 128:(a * 2 + c + 1) * 128], kvb[:, a, c * 128:(c + 1) * 128], self.identb[:],
                                    ["kvb", "identb"], [("B4", a * 2 + c)])
                    for a, (dst, nm) in enumerate(((kcT, "kcT"), (vcT, "vcT"), (ksT, "ksT"), (kwT, "kwT"))):
                        P.op("act", lambda e: e.copy(dst[:, :, tb * 128:(tb + 1) * 128], b4[:, a * 256:(a + 1) * 256].rearrange("p (c t) -> p c t", c=2)),
                             reads=["B4"], writes=[(nm, tb)])
                for a in range(2):
                    self.tr(self.B[1][0:64, 0:32], pos[:, a, :], self.identf[0:32, 0:32], ["pos", "identf"], ["B1"])
                    P.op("act", lambda e: e.copy(posT[:, a, :], self.B[1][0:64, 0:32]), reads=["B1"], writes=[("posT", a)])
                    for lq in range(32):
                        self.mm(self.B[2][0:64, 0:1], w1b[a][0:64, lq, :], posT[:, a, lq:lq + 1], lq == 0, lq == 31, ["w1b%d" % a, ("posT", a)], ["B2"])
                    P.op("dve", lambda e: e.tensor_copy(cst[:, a:a + 1], self.B[2][0:64, 0:1]), reads=["B2"], writes=[("cst", a)])
                for a, srcT in enumerate((kcT, vcT)):
                    for gq in range(4):
                        base = (gq % 2) * 64
                        ch = gq // 2
                        for lq in range(32):
                            rhs = srcT[base:base + 64, ch, lq:lq + 16 * 126 + 1:16]
                            self.mm(self.B[5][0:64, 0:127], w1b[a][base:base + 64, lq, :], rhs, lq == 0, lq == 31,
                                    ["w1b%d" % a, "kcT", "vcT"], ["B5"])
                        P.op("act", lambda e: e.activation(HT[:, 0:127], self.B[5][0:64, 0:127], AF.Gelu_apprx_tanh, bias=cst[:, a:a + 1], scale=1.0),
                             reads=["B5", ("cst", a)], writes=["HT"])
                        self.mm(self.B[6][0:127, 0:64], HT[:, 0:127], w2b[a][:], True, True, ["HT", "w2b%d" % a], ["B6"])
                        if a == 0:
                            self.head_rmsnorm(self.B[6][0:127, 0:64], "B6", 1, gbs[0:127, 1, :], ("gbs", 1), KcN[0:127, gq:gq + 1, :], ("KcN", gq),
                                              sq[0:127], rs[0:127], npart=127)
                        else:
                            P.op("act", lambda e: e.copy(VcX[0:127, gq, 0:64], self.B[6][0:127, 0:64]), reads=["B6"], writes=[("VcX", gq)])
                b4 = self.bank_bf(4)
                for c in range(2):
                    self.tr(b4[:, c * 128:c * 128 + 127], KcN[0:127, 2 * c:2 * c + 2, :].rearrange("p g d -> p (g d)"), self.identb[0:127, 0:127],
                            ["KcN", "identb"], [("B4", c)])
                    P.op("act", lambda e: e.copy(KcT[:, c, 0:127], b4[:, c * 128:c * 128 + 127]), reads=[("B4", c)], writes=[("KcT", c)])
                P.barrier()
            with ExitStack() as ph:
                wq = self.T(ph, "wq", [128, 8, 1072], BF16)
                wout = self.T(ph, "wout", [128, 8, D], BF16)
                bgb = self.T(ph, "bgb", [128, 48], F32)
                gates = self.T(ph, "gates", [128, 48], F32)
                a1 = self.T(ph, "a1", [128, 16, 32], BF16)
                a0 = self.T(ph, "a0", [128, 16, 32], BF16)
                ej = self.T(ph, "ej", [32, 16, 128], BF16)
                qn = self.T(ph, "qn", [128, D], BF16)
                qT = self.T(ph, "qT", [128, 8, 128], BF16)
                Of = self.ntmp
                imp = self.T(ph, "imp", [128, 4, 32], F32)
                impw = self.T(ph, "impw", [128, 4, 32], F32)
                nmk = self.T(ph, "nmk", [128, 4, 32], BF16)
                NMT = self.T(ph, "NMT", [32, 4, 128], BF16)
                bci = self.T(ph, "bci", [127, 16, 128], BF16)
                self.OT = self.T(ph, "OT", [128, 8, 128], BF16)
                self.PT = [self.T(ph, "PT%d" % k, [128, 512], BF16) for k in range(3)]
                P.dma(wq[:, :, 0:1024], w_in[:, 0:1024].rearrange("(c p) n -> p c n", p=128), writes=[("wq", 0)], q="pool")
                P.dma(wq[:, :, 1024:1072], w_in[:, 2560:2608].rearrange("(c p) n -> p c n", p=128), writes=[("wq", 1)], q="pool")
                P.dma(wout[:], I["odd_w_out"][li].rearrange("(c p) n -> p c n", p=128), writes=["wout"], q="pool")
                P.dma(bgb[:], I["odd_b_gate"][li:li + 1, :].broadcast_to([128, 48]), writes=["bgb"])
                P.dma(a1[:], I["k_a1"], writes=["a1"], q="pool")
                P.dma(a0[:], I["k_a0"], writes=["a0"], q="pool")
                P.dma(ej[:], I["k_ej"], writes=["ej"], q="pool")
                for i in range(NT):
                    self.norm_hT(i)
                    self.proj(1, 512, wq, "wq", 0)
                    self.proj(2, 512, wq, "wq", 512)
                    self.proj(3, 48, wq, "wq", 1024)
                    P.op("dve", lambda e: e.tensor_tensor(gates[:], self.B[3][:, 0:48], bgb[:], ALU.add), reads=["B3", "bgb"], writes=["gates"])
                    P.op("act", lambda e: e.activation(gates[:], gates[:], AF.Sigmoid), reads=["gates"], writes=["gates"])
                    for hb in range(2):
                        src = self.B[1 + hb][:, :]
                        sk = "B%d" % (1 + hb)
                        P.op("act", lambda e: e.activation(sq[:, 0:512], src, AF.Square), reads=[sk], writes=["sq"])
                        P.op("dve", lambda e: e.tensor_reduce(rs[:, 0:8], sq[:, 0:512].rearrange("p (h d) -> p h d", d=64), AX.X, ALU.add), reads=["sq"], writes=[("rs", 0)])
                        P.op("dve", lambda e: e.tensor_scalar(rs[:, 16:24], rs[:, 0:8], 1.0 / 64, 1e-6, ALU.mult, ALU.add), reads=[("rs", 0)], writes=[("rs", 1)])
                        P.op("act", lambda e: e.activation(rs[:, 32:40], rs[:, 16:24], AF.Sqrt), reads=[("rs", 1)], writes=[("rs", 2)])
                        P.op("dve", lambda e: e.reciprocal(rs[:, 48:56], rs[:, 32:40]), reads=[("rs", 2)], writes=[("rs", 3)])
                        P.op("dve", lambda e: e.tensor_tensor(sq[:, 0:512].rearrange("p (h d) -> p h d", d=64), src.rearrange("p (h d) -> p h d", d=64),
                                                              rs[:, 48:56].unsqueeze(2).broadcast_to([128, 8, 64]), ALU.mult),
                             reads=[sk, ("rs", 3), "sq"], writes=["sq"])
                        for gh in range(2):
                            dst = qn[:, hb * 512:(hb + 1) * 512].rearrange("p (j gh d) -> p gh j d", j=4, gh=2)[:, gh, :, :]
                            srcv = sq[:, gh * 256:(gh + 1) * 256].rearrange("p (j d) -> p j d", j=4)
                            P.op("pool", lambda e: e.tensor_tensor(dst, srcv, gbs[:, 0, :].unsqueeze(1).broadcast_to([128, 4, 64]), ALU.mult),
                                 reads=["sq", ("gbs", 0)], writes=[("qn", hb, gh)])
                    b4 = self.bank_bf(4)
                    for c in range(8):
                        self.tr(b4[:, c * 128:(c + 1) * 128], qn[:, c * 128:(c + 1) * 128], self.identb[:], ["qn", "identb"], [("B4", c)])
                    P.op("act", lambda e: e.copy(qT[:], b4[:, 0:1024].rearrange("p (c t) -> p c t", c=8)), reads=["B4"], writes=["qT"])
                    ap = bass.AP(tensor=self.FV.tensor, offset=2048 + 128 * i - 2047, ap=[[16, 127], [4096, 16], [1, 128]])
                    P.dma(bci[:], ap, writes=["bci"])
                    oi3 = self.B[7][:, 0:388].rearrange("p (a b) -> p a b", a=4)
                    for gq in range(4):
                        base = (gq % 2) * 64
                        cq0 = (gq // 2) * 4
                        mms = [(KcT[base:base + 64, gq // 2, 0:127], qT[base:base + 64, cq0:cq0 + 4, :], ["KcT", "qT"]),
                               (self.anti127[0:127, 0:127], bci[:, 4 * gq:4 * gq + 4, :], ["anti127", "bci"])]
                        pv = [(jh * 128, 128, VcX[0:127, gq, :], oi3[:, jh, :], "B7", True, True, ["VcX"]) for jh in range(4)]
                        self.run_attn([dict(nrow=127, width=512, groups=[(0, 512, 4, mms)], pv=pv)])
                        P.op("dve", lambda e: e.tensor_scalar(rd[:, 0:4], oi3[:, :, 64], 1e-30, None, ALU.max), reads=["B7"], writes=[("rd", "den")])
                        P.op("dve", lambda e: e.reciprocal(rd[:, 4:8], rd[:, 0:4]), reads=[("rd", "den")], writes=[("rd", "rden")])
                        gsl = gates[:, 12 * gq:12 * gq + 12].rearrange("p (j b) -> p j b", b=3)
                        P.op("dve", lambda e: e.tensor_tensor(rd[:, 8:12], rd[:, 4:8], gsl[:, :, 0], ALU.mult), reads=[("rd", "rden"), "gates"], writes=[("rd", "gr")])
                        P.op("dve", lambda e: e.tensor_tensor(Of[:, gq * 256:(gq + 1) * 256].rearrange("p (j d) -> p j d", j=4), oi3[:, :, 0:64],
                                                              rd[:, 8:12].unsqueeze(2).broadcast_to([128, 4, 64]), ALU.mult),
                             reads=["B7", ("rd", "gr")], writes=["ntmp"])
                        P.op("dve", lambda e: e.tensor_tensor(impw[:], oi3[:, :, 65:97], rd[:, 4:8].unsqueeze(2).broadcast_to([128, 4, 32]), ALU.mult),
                             reads=["B7", ("rd", "rden")], writes=["impw"])
                        P.op("dve", lambda e: e.tensor_reduce(imp[:, gq, :], impw[:].rearrange("p j m -> p m j"), AX.X, ALU.add), reads=["impw"], writes=[("imp", gq)])
                    P.op("dve", lambda e: e.tensor_tensor(imp[:], imp[:], a1[:, i:i + 1, :].broadcast_to([128, 4, 32]), ALU.mult), reads=["imp", "a1"], writes=["imp"])
                    P.op("dve", lambda e: e.tensor_tensor(imp[:], imp[:], a0[:, i:i + 1, :].broadcast_to([128, 4, 32]), ALU.add), reads=["imp", "a0"], writes=["imp"])
                    for gq in range(4):
                        P.op("dve", lambda e: e.max(rd[:, 12:20], imp[:, gq, :]), reads=["imp"], writes=[("rd", "m8a")])
                        P.op("dve", lambda e: e.match_replace(impw[:, gq, :], rd[:, 12:20], imp[:, gq, :], -1e30), reads=["imp", ("rd", "m8a")], writes=["impw"])
                        P.op("dve", lambda e: e.max(rd[:, 20:28], impw[:, gq, :]), reads=["impw"], writes=[("rd", "m8b")])
                        P.op("dve", lambda e: e.tensor_scalar(rd[:, 28:29], rd[:, 27:28], 0.0, None, ALU.max), reads=[("rd", "m8b")], writes=[("rd", "thr")])
                        P.op("dve", lambda e: e.tensor_scalar(impw[:, gq, :], imp[:, gq, :], rd[:, 28:29], 1.0, ALU.is_ge, ALU.subtract),
                             reads=["imp", ("rd", "thr"), "impw"], writes=["impw"])
                        P.op("dve", lambda e: e.tensor_scalar(nmk[:, gq, :], impw[:, gq, :], BIG, None, ALU.mult), reads=["impw"], writes=[("nmk", gq)])
                    b3 = self.bank_bf(3)
                    for gq in range(4):
                        self.tr(b3[0:32, 512 + gq * 128:512 + (gq + 1) * 128], nmk[:, gq, :], self.identb[:], ["nmk", "identb"], [("B3", gq)])
                    P.op("act", lambda e: e.copy(NMT[:], b3[0:32, 512:1024].rearrange("p (g t) -> p g t", g=4)), reads=["B3"], writes=["NMT"])
                    oi4 = self.B[7][:, 0:260].rearrange("p (a b) -> p a b", a=4)
                    for br, (kTt, knm, Vt, vnm, jlo) in enumerate(((ksT, "ksT", Vs, "Vs", 0), (kwT, "kwT", Vw, "Vw", max(0, i - 4)))):
                        for gq in range(4):
                            base = (gq % 2) * 64
                            cq0 = (gq // 2) * 4
                            tasks = []
                            for j in range(jlo, i + 1):
                                dl = i - j
                                mms = [(kTt[base:base + 64, gq // 2, j * 128:(j + 1) * 128], qT[base:base + 64, cq0:cq0 + 4, :], [knm, "qT"])]
                                if br == 0:
                                    mms.append((ej[:, j, :], NMT[:, gq:gq + 1, :].broadcast_to([32, 4, 128]), ["ej", "NMT"]))
                                if dl == 0:
                                    mms.append((self.antib[:], D0[:, 4 * gq:4 * gq + 4, :], ["antib", "D0"]))
                                elif dl == 1:
                                    mms.append((self.antib[:], D1[:, 4 * gq:4 * gq + 4, :], ["antib", "D1"]))
                                elif dl == 4 and br == 1:
                                    mms.append((self.antib[:], D4[:, 4 * gq:4 * gq + 4, :], ["antib", "D4"]))
                                else:
                                    mms.append((self.ones_row[:], CROW[0:1, 4 * gq:4 * gq + 4, :], ["ones_row", "CROW"]))
                                pv = [(jh * 128, 128, Vt[:, j, gq, :], oi4[:, jh, :], "B7", (j == jlo and jh == 0), j == i, [vnm]) for jh in range(4)]
                                tasks.append(dict(nrow=128, width=512, groups=[(0, 512, 4, mms)], pv=pv))
                            self.run_attn(tasks)
                            gsl = gates[:, 12 * gq:12 * gq + 12].rearrange("p (j b) -> p j b", b=3)
                            P.op("dve", lambda e: e.reciprocal(rd[:, 4:8], oi4[:, :, 64]), reads=["B7"], writes=[("rd", "rden")])
                            P.op("dve", lambda e: e.tensor_tensor(rd[:, 8:12], rd[:, 4:8], gsl[:, :, 1 + br], ALU.mult), reads=[("rd", "rden"), "gates"], writes=[("rd", "gr")])
                            P.op("dve", lambda e: e.tensor_tensor(sq[:, 0:256].rearrange("p (j d) -> p j d", j=4), oi4[:, :, 0:64],
                                                                  rd[:, 8:12].unsqueeze(2).broadcast_to([128, 4, 64]), ALU.mult),
                                 reads=["B7", ("rd", "gr")], writes=["sq"])
                            P.op("pool", lambda e: e.tensor_tensor(Of[:, gq * 256:(gq + 1) * 256], Of[:, gq * 256:(gq + 1) * 256], sq[:, 0:256], ALU.add),
                                 reads=["sq", "ntmp"], writes=["ntmp"])
                    P.op("act", lambda e: e.copy(self.hbf[:], Of[:]), reads=["ntmp"], writes=["hbf"])
                    self.out_proj_residual(i, wout)
                P.barrier()

    def peer_layer(self, l):
        P = self.P
        I = self.I
        self.load_mods(l, 1)
        for et in range(32):
            P.dma(self.UB[et], I["peer_ut"][l, :, et * 512:(et + 1) * 512].rearrange("(c p) e -> p c e", p=128), writes=[("UB", et)], q="pool")
            P.dma(self.VB[et], I["peer_v"][l, et * 512:(et + 1) * 512, :].rearrange("(c p) d -> p c d", p=128), writes=[("VB", et)], q="pool")
        with ExitStack() as ph:
            wq = self.T(ph, "wq", [128, 8, D], BF16)
            kin = self.T(ph, "kin", [128, 2, 128], F32)
            kbd = self.T(ph, "kbd", [128, 256], BF16)
            qTp = self.T(ph, "qTp", [128, 8, 128], BF16)
            sc = self.T(ph, "sc", [128, 256], F32)
            scr = self.T(ph, "scr", [128, 256], F32)
            tk = self.T(ph, "tk", [128, 8, 64], F32)
            e01 = self.T(ph, "e01", [128, 8, 256], F32)
            prod = [self.T(ph, "prod%d" % k, [128, 512], F32) for k in range(3)]
            tmp2 = [self.T(ph, "tmp2%d" % k, [128, 512], BF16) for k in range(16)]
            ub = [self.T(ph, "ub%d" % k, [128, 8, 512], BF16) for k in range(2)]
            vb = [self.T(ph, "vb%d" % k, [128, 4, D], BF16) for k in range(2)]
            gA = [self.T(ph, "gA%d" % k, [128, 512], BF16) for k in range(2)]
            G = [self.T(ph, "G%d" % k, [128, 512], BF16) for k in range(2)]
            GT = [self.T(ph, "GT%d" % k, [128, 4, 128], BF16) for k in range(2)]
            P.dma(wq[:], I["peer_w_q"][l].rearrange("(c p) n -> p c n", p=128), writes=["wq"], q="pool")
            P.op("pool", lambda e: e.memset(kin[:], 0.0), writes=["kin"])
            P.dma(kin[:, 0, 0:64], I["peer_keys"][l, 0], reads=["kin"], writes=[("kin", 0)])
            P.dma(kin[:, 1, 64:128], I["peer_keys"][l, 1], reads=["kin"], writes=[("kin", 1)])
            for pq in range(2):
                self.tr(self.B[1][:, pq * 128:(pq + 1) * 128], kin[:, pq, :], self.identf[:], ["kin", "identf"], [("B1", pq)])
            P.op("act", lambda e: e.copy(kbd[:], self.B[1][:, 0:256]), reads=["B1"], writes=["kbd"])
            n_et = 0
            for tb in range(NT):
                self.norm_hT(tb)
                for h in range(8):
                    bk = 1 + h // 4
                    for dc in range(8):
                        self.mm(self.B[bk][:, (h % 4) * 128:(h % 4 + 1) * 128], wq[:, dc, h * 128:(h + 1) * 128], self.hT[:, dc, :], dc == 0, dc == 7,
                                ["wq", "hT"], [("B%d" % bk, h % 4)])
                for bk in (1, 2):
                    P.op("act", lambda e: e.copy(qTp[:, (bk - 1) * 4:(bk - 1) * 4 + 4, :], self.B[bk][:, :].rearrange("p (h t) -> p h t", h=4)),
                         reads=["B%d" % bk], writes=[("qTp", bk)])
                for h in range(8):
                    self.mm(self.B[3][:, (h % 2) * 256:(h % 2 + 1) * 256], qTp[:, h, :], kbd[:], True, True, ["qTp", "kbd"], [("B3", h % 2)])
                    P.op("act", lambda e: e.copy(sc[:], self.B[3][:, (h % 2) * 256:(h % 2 + 1) * 256]), reads=[("B3", h % 2)], writes=["sc"])
                    for pq in range(2):
                        s_ = sc[:, pq * 128:(pq + 1) * 128]
                        o = pq * 16
                        P.op("dve", lambda e: e.max(tk[:, h, o:o + 8], s_), reads=["sc"], writes=[("tk", h, pq)])
                        P.op("dve", lambda e: e.match_replace(scr[:, 0:128], tk[:, h, o:o + 8], s_, -1e30), reads=["sc", ("tk", h, pq)], writes=["scr"])
                        P.op("dve", lambda e: e.max(tk[:, h, o + 8:o + 16], scr[:, 0:128]), reads=["scr"], writes=[("tk", h, pq)])
                    cand = scr[:, 0:256].rearrange("p (a b) -> p a b", a=16)
                    P.op("dve", lambda e: e.tensor_tensor(cand, tk[:, h, 0:16].unsqueeze(2).broadcast_to([128, 16, 16]),
                                                          tk[:, h, 16:32].unsqueeze(1).broadcast_to([128, 16, 16]), ALU.add),
                         reads=[("tk", h)], writes=["scr"])
                    P.op("dve", lambda e: e.max(tk[:, h, 32:40], scr[:, 0:256]), reads=["scr"], writes=[("tk", h, 2)])
                    P.op("dve", lambda e: e.match_replace(scr[:, 0:256], tk[:, h, 32:40], scr[:, 0:256], -1e30), reads=["scr", ("tk", h, 2)], writes=["scr"])
                    P.op("dve", lambda e: e.max(tk[:, h, 40:48], scr[:, 0:256]), reads=["scr"], writes=[("tk", h, 2)])
                    P.op("dve", lambda e: e.tensor_scalar(tk[:, h, 48:49], tk[:, h, 32:33], -1.0, None, ALU.mult), reads=[("tk", h, 2)], writes=[("tk", h, 3)])
                    P.op("act", lambda e: e.activation(scr[:, 0:16], tk[:, h, 32:48], AF.Exp, bias=tk[:, h, 48:49], scale=1.0, accum_out=tk[:, h, 49:50]),
                         reads=[("tk", h, 2), ("tk", h, 3), "scr"], writes=["scr", ("tk", h, 4)])
                    P.op("dve", lambda e: e.reciprocal(tk[:, h, 50:51], tk[:, h, 49:50]), reads=[("tk", h, 4)], writes=[("tk", h, 5)])
                    P.op("dve", lambda e: e.tensor_scalar(tk[:, h, 51:52], tk[:, h, 0:1], -1.0, None, ALU.mult), reads=[("tk", h, 0)], writes=[("tk", h, 6)])
                    P.op("dve", lambda e: e.tensor_scalar(tk[:, h, 52:53], tk[:, h, 16:17], -1.0, None, ALU.mult), reads=[("tk", h, 1)], writes=[("tk", h, 7)])
                    P.op("act", lambda e: e.activation(tk[:, h, 54:55], tk[:, h, 47:48], AF.Exp, bias=tk[:, h, 48:49], scale=1.0),
                         reads=[("tk", h, 2), ("tk", h, 3)], writes=[("tk", h, 8)])
                    P.op("dve", lambda e: e.tensor_scalar(tk[:, h, 54:55], tk[:, h, 54:55], tk[:, h, 50:51], 0.9995, ALU.mult, ALU.mult),
                         reads=[("tk", h, 8), ("tk", h, 5)], writes=[("tk", h, 8)])
                    P.op("act", lambda e: e.activation(e01[:, h, 0:128], sc[:, 0:128], AF.Exp, bias=tk[:, h, 51:52], scale=1.0),
                         reads=["sc", ("tk", h, 6)], writes=[("e01", h, 0)])
                    P.op("dve", lambda e: e.tensor_scalar(e01[:, h, 0:128], e01[:, h, 0:128], tk[:, h, 50:51], None, ALU.mult),
                         reads=[("e01", h, 0), ("tk", h, 5)], writes=[("e01", h, 0)])
                    P.op("act", lambda e: e.activation(e01[:, h, 128:256], sc[:, 128:256], AF.Exp, bias=tk[:, h, 52:53], scale=1.0),
                         reads=["sc", ("tk", h, 7)], writes=[("e01", h, 1)])
                def grid(et):
                    for h in range(8):
                        pr = prod[self._npr % 3]
                        pk = "prod%d" % (self._npr % 3)
                        self._npr += 1
                        slot = (et % 2) * 8 + h
                        t2 = tmp2[slot]
                        tk2 = "tmp2%d" % slot
                        if h < 2:
                            for ii in range(4):
                                P.op("act", lambda e: e.activation(pr[:, ii * 128:(ii + 1) * 128], e01[:, h, 128:256], AF.Identity,
                                                                   scale=e01[:, h, et * 4 + ii:et * 4 + ii + 1]),
                                     reads=[("e01", h)], writes=[(pk, ii)])
                        else:
                            P.op("pool", lambda e: e.tensor_tensor(pr[:].rearrange("p (a b) -> p a b", a=4),
                                                                   e01[:, h, et * 4:(et + 1) * 4].unsqueeze(2).broadcast_to([128, 4, 128]),
                                                                   e01[:, h, 128:256].unsqueeze(1).broadcast_to([128, 4, 128]), ALU.mult),
                                 reads=[("e01", h)], writes=[pk])
                        P.op("dve", lambda e: e.scalar_tensor_tensor(t2[:], pr[:], tk[:, h, 54:55], pr[:], ALU.is_ge, ALU.mult),
                             reads=[pk, ("tk", h, 8)], writes=[tk2])

                def amm(et):
                    k2 = et % 2
                    P.dma(ub[k2][:], self.UB[et], reads=[("UB", et)], writes=["ub%d" % k2])
                    P.dma(vb[k2][:], self.VB[et], reads=[("VB", et)], writes=["vb%d" % k2])
                    ab = 4 + k2
                    for dc in range(8):
                        self.mm(self.B[ab][:, :], self.hT[:, dc, :], ub[k2][:, dc, :], dc == 0, dc == 7, ["hT", "ub%d" % k2], ["B%d" % ab])
                    P.op("act", lambda e: e.activation(gA[k2][:], self.B[ab][:, :], AF.Gelu_apprx_tanh), reads=["B%d" % ab], writes=["gA%d" % k2])
                    wb = 1 + k2
                    for h in range(8):
                        slot = k2 * 8 + h
                        self.mm(self.B[wb][:, :], self.identb[:], tmp2[slot][:], h == 0, h == 7, ["identb", "tmp2%d" % slot], ["B%d" % wb])

                def gmul(et):
                    k2 = et % 2
                    P.op("dve", lambda e: e.tensor_tensor(G[k2][:], gA[k2][:], self.B[1 + k2][:, :], ALU.mult),
                         reads=["gA%d" % k2, "B%d" % (1 + k2)], writes=["G%d" % k2])

                def ymm(et):
                    k2 = et % 2
                    b0 = self.bank_bf(0)
                    for c in range(4):
                        self.tr(b0[:, k2 * 512 + c * 128:k2 * 512 + (c + 1) * 128], G[k2][:, c * 128:(c + 1) * 128], self.identb[:],
                                ["G%d" % k2, "identb"], [("B0", k2 * 4 + c)])
                    P.op("act", lambda e: e.copy(GT[k2][:], b0[:, k2 * 512:(k2 + 1) * 512].rearrange("p (c t) -> p c t", c=4)),
                         reads=[("B0", k2 * 4), ("B0", k2 * 4 + 1), ("B0", k2 * 4 + 2), ("B0", k2 * 4 + 3)], writes=["GT%d" % k2])
                    for c in range(4):
                        for half in range(2):
                            self.mm(self.B[6 + half][:, :], GT[k2][:, c, :], vb[k2][:, c, half * 512:(half + 1) * 512],
                                    et == 0 and c == 0, et == 31 and c == 3, ["GT%d" % k2, "vb%d" % k2], ["B%d" % (6 + half)])

                self._npr = getattr(self, "_npr", 0)
                for k in range(-2, 32):
                    if k >= 0:
                        gmul(k)
                    if k + 2 <= 31:
                        grid(k + 2)
                    if 0 <= k + 1 <= 31:
                        amm(k + 1)
                    if k >= 0:
                        ymm(k)
                for half in range(2):
                    P.op("dve", lambda e: e.tensor_tensor(self.ntmp[:, half * 512:(half + 1) * 512], self.B[6 + half][:, :],
                                                          self.GTB[:, half * 512:(half + 1) * 512], ALU.mult),
                         reads=["B%d" % (6 + half), "GTB"], writes=[("ntmp", half)])
                    P.op("pool", lambda e: e.tensor_tensor(self.X[:, tb, half * 512:(half + 1) * 512], self.X[:, tb, half * 512:(half + 1) * 512],
                                                           self.ntmp[:, half * 512:(half + 1) * 512], ALU.add),
                         reads=[("ntmp", half), ("X", tb)], writes=[("X", tb)])
            P.barrier()


_CACHE = {}


def make_in_maps(inputs):
    consts = host_consts()
    shared = {}
    f = lambda a: np.ascontiguousarray(np.asarray(a, dtype=np.float32))
    shared["ada_w"] = f(inputs["ada_w"])
    shared["ada_b"] = f(inputs["ada_b"]).reshape(1, -1)
    shared["norm_g"] = f(inputs["norm_g"]).reshape(1, -1)
    for k in ("even_w_in", "even_b_f", "even_q_g", "even_k_g", "even_w_out", "odd_w_in", "odd_b_gate", "odd_q_g",
              "odd_cmp_pos", "odd_cmp_w1", "odd_cmp_w2", "odd_w_out", "rel_table", "peer_w_q", "peer_keys", "peer_v"):
        shared[k] = f(inputs[k])
    shared["even_conv_w"] = f(inputs["even_conv_w"]).reshape(2, -1)
    shared["odd_k_g"] = f(inputs["odd_k_g"]).reshape(2, -1)
    shared["peer_ut"] = np.ascontiguousarray(np.transpose(f(inputs["peer_u"]), (0, 2, 1)))
    shared.update(consts)
    x = f(inputs["x"])
    c = f(inputs["c"])
    maps = []
    for b in range(8):
        m = dict(shared)
        m["x"] = x[b]
        m["c"] = c[b:b + 1]
        maps.append(m)
    return maps


def kernel(**inputs):
    if "nc" not in _CACHE:
        _CACHE["nc"] = Builder().build()
    nc = _CACHE["nc"]
    maps = make_in_maps(inputs)
    res = run_bass_kernel_spmd(nc, maps, core_ids=list(range(8)))
    return np.stack([np.asarray(r["y"], dtype=np.float32) for r in res.results], axis=0)
```

```python
import math
from contextlib import ExitStack
import numpy as np
import concourse.bass as bass
import concourse.mybir as mybir
from concourse.bass_utils import run_bass_kernel_spmd

F32 = mybir.dt.float32
BF16 = mybir.dt.bfloat16
AF = mybir.ActivationFunctionType
ALU = mybir.AluOpType
AX = mybir.AxisListType

S = 2048
D = 1024
NT = 16
BIG = 240000.0
SEM_ROT = 15000


def _conflict(a, b):
    n = min(len(a), len(b))
    return a[:n] == b[:n]


class _Eng:
    def __init__(self, P, name, eng, same_wait):
        self.P = P
        self.name = name
        self.eng = eng
        self.same_wait = same_wait
        self.sem = None
        self.count = 0
        self.waited = {}

    def new_sem(self):
        self.sem = self.P.alloc_sem(self.name)
        self.count = 0


class Prog:
    def __init__(self, nc, stack, n_dma_sems=4):
        self.nc = nc
        self.stack = stack
        self.nsem = 0
        self.engs = {}
        for name, eng, sw in (("pe", nc.tensor, False), ("dve", nc.vector, True),
                              ("act", nc.scalar, True), ("pool", nc.gpsimd, True),
                              ("sp", nc.sync, False)):
            e = _Eng(self, name, eng, sw)
            e.new_sem()
            self.engs[name] = e
        self.dq = {}
        for q, eng_name, ns in (("sp", "sp", 4), ("pool", "pool", 2), ("conv", "pool", 4)):
            self.dq[q] = {"sems": [self.alloc_sem("d" + q) for _ in range(ns)],
                          "vals": [0] * ns, "n": 0, "eng": eng_name}
        self.state = {}
        self.out_events = []
        self.ninst = 0

    def alloc_sem(self, name):
        self.nsem += 1
        return self.stack.enter_context(self.nc.semaphore("s%s%d" % (name, self.nsem)))

    def _deps(self, reads, writes):
        deps = []
        for k in reads:
            for k2, st in self.state.get(k[0], {}).items():
                if st[0] is not None and _conflict(k, k2):
                    deps.append(st[0])
        for k in writes:
            for k2, st in self.state.get(k[0], {}).items():
                if _conflict(k, k2):
                    if st[0] is not None:
                        deps.append(st[0])
                    deps.extend(st[1])
        return deps

    def _record(self, ev, reads, writes):
        for k in reads:
            d = self.state.setdefault(k[0], {})
            st = d.setdefault(k, [None, []])
            st[1] = [e for e in st[1] if e[0] is not ev[0]] + [ev]
        for k in writes:
            d = self.state.setdefault(k[0], {})
            for k2 in [k2 for k2 in d if len(k2) > len(k) and k2[:len(k)] == k]:
                del d[k2]
            d[k] = [ev, []]

    def _wait(self, E, deps):
        need = {}
        for (sem, val, owner) in deps:
            if owner is E and not E.same_wait:
                continue
            if E.waited.get(id(sem), 0) >= val:
                continue
            if need.get(id(sem), (None, 0))[1] < val:
                need[id(sem)] = (sem, val)
        for sem, val in need.values():
            E.eng.wait_ge(sem, val)
            E.waited[id(sem)] = val

    @staticmethod
    def _keys(ks):
        return [k if isinstance(k, tuple) else (k,) for k in ks]

    def op(self, engname, fn, reads=(), writes=()):
        E = self.engs[engname]
        reads = self._keys(reads)
        writes = self._keys(writes)
        self._wait(E, self._deps(reads, writes))
        if E.count >= SEM_ROT:
            E.new_sem()
        inst = fn(E.eng)
        inst.then_inc(E.sem, 1)
        E.count += 1
        self.ninst += 1
        ev = (E.sem, E.count, E)
        self._record(ev, reads, writes)
        return ev

    def dma(self, out, in_, reads=(), writes=(), q="sp", is_output=False, **kw):
        Q = self.dq[q]
        E = self.engs[Q["eng"]]
        reads = self._keys(reads)
        writes = self._keys(writes)
        i = Q["n"] % len(Q["sems"])
        Q["n"] += 1
        sem = Q["sems"][i]
        deps = self._deps(reads, writes)
        if Q["vals"][i] > 0:
            deps.append((sem, Q["vals"][i], None))
        self._wait(E, deps)
        inst = E.eng.dma_start(out=out, in_=in_, **kw)
        Q["vals"][i] += 16
        inst.then_inc(sem, 16)
        self.ninst += 1
        ev = (sem, Q["vals"][i], None)
        self._record(ev, reads, writes)
        if is_output:
            self.out_events.append(ev)
        return ev

    def _all_events(self):
        evs = []
        for e in self.engs.values():
            if e.count > 0:
                evs.append((e.sem, e.count, e))
        for Q in self.dq.values():
            for sem, v in zip(Q["sems"], Q["vals"]):
                if v > 0:
                    evs.append((sem, v, None))
        return evs

    def barrier(self):
        evs = self._all_events()
        for E in self.engs.values():
            self._wait(E, [ev for ev in evs if ev[2] is not E])
        self.state = {}

    def finish(self):
        E = self.engs["sp"]
        self._wait(E, self.out_events + [ev for ev in self._all_events() if ev[2] is not E])


def host_consts():
    c = {}
    dist = np.arange(2048)
    nf = np.maximum(dist, 1).astype(np.float32)
    large = 16 + (np.log(nf / np.float32(16)) / np.float32(math.log(8.0)) * np.float32(16)).astype(np.int32)
    large = np.minimum(large, 31)
    bucket = np.where(dist < 16, dist, large)
    ohb = np.zeros((32, 2048), np.float32)
    ohb[bucket, dist] = 1.0
    c["k_ohb"] = ohb
    p = np.arange(128)[:, None, None]
    i = np.arange(16)[None, :, None]
    m = np.arange(32)[None, None, :]
    t = 128 * i + p
    cur = t // 64
    forced = (m == 0) | (m == cur) | (m == cur - 1)
    allowed = (64 * m <= t)
    c["k_a1"] = (allowed & ~forced).astype(np.float32)
    c["k_a0"] = np.where(forced, 1e6, np.where(allowed, 0.0, -1.0)).astype(np.float32)
    mm = np.arange(32)[:, None, None]
    jj = np.arange(16)[None, :, None]
    sp = np.arange(128)[None, None, :]
    c["k_ej"] = (mm == 2 * jj + sp // 64).astype(np.float32)
    starts = (np.arange(127) * 16)[:, None]
    bstart = (np.arange(32) * 64)[None, :]
    c["k_ovl"] = ((starts < bstart + 64) & (starts + 32 > bstart)).astype(np.float32)
    s_ = np.arange(128)[:, None]
    t_ = np.arange(128)[None, :]
    c["k_caus"] = np.where(s_ > t_, -BIG, 0.0).astype(np.float32)
    sel8 = np.zeros((8, 8, 128), np.float32)
    for h in range(8):
        sel8[h, h, :] = 1.0
    c["k_sel8"] = sel8
    shm = np.zeros((128, 4, 128), np.float32)
    shm[:, 0, :] = (s_ == t_ - 1)
    shm[:, 1, :] = (s_ == t_ - 2)
    shm[127, 2, 0] = 1.0
    shm[126, 3, 0] = 1.0
    shm[127, 3, 1] = 1.0
    c["k_shm"] = shm
    return c


CONST_SHAPES = {"k_ohb": [32, 2048], "k_a1": [128, 16, 32], "k_a0": [128, 16, 32], "k_ej": [32, 16, 128],
                "k_ovl": [127, 32], "k_caus": [128, 128], "k_sel8": [8, 8, 128], "k_shm": [128, 4, 128]}

IN_SHAPES = {
    "x": [S, D], "c": [1, D], "ada_w": [4, D, 6 * D], "ada_b": [1, 4 * 6 * D], "norm_g": [1, 4 * 2 * D],
    "even_w_in": [2, D, 3080], "even_b_f": [2, 8], "even_conv_w": [2, 3 * 512], "even_q_g": [2, 64],
    "even_k_g": [2, 64], "even_w_out": [2, D, D], "odd_w_in": [2, D, 2608], "odd_b_gate": [2, 48],
    "odd_q_g": [2, 64], "odd_k_g": [2, 3 * 64], "odd_cmp_pos": [2, 2, 32, 64], "odd_cmp_w1": [2, 2, 2048, 64],
    "odd_cmp_w2": [2, 2, 64, 64], "odd_w_out": [2, D, D], "rel_table": [32, 16], "peer_w_q": [4, D, D],
    "peer_keys": [4, 2, 128, 64], "peer_ut": [4, D, 16384], "peer_v": [4, 16384, D],
}


class Builder:
    def __init__(self, n_layers=4, stop=None, peer=True, snaps=False):
        self.snaps = snaps
        self.snap_names = []
        self.n_layers = n_layers
        self.stop = stop
        self.do_peer = peer
        nc = self.nc = bass.Bass("TRN2", target_bir_lowering=False)
        self.I = {}
        for k, shp in list(IN_SHAPES.items()) + list(CONST_SHAPES.items()):
            self.I[k] = nc.dram_tensor(k, list(shp), F32, kind="ExternalInput").ap()
        self.y_out = nc.dram_tensor("y", [S, D], F32, kind="ExternalOutput").ap()
        self.MODS = nc.dram_tensor("mods_s", [4, 6, D], F32, kind="Internal").ap()
        self.FV = nc.dram_tensor("fv_s", [16, 4096], BF16, kind="Internal").ap()
        self.FW = nc.dram_tensor("fw_s", [16, 4096], BF16, kind="Internal").ap()
        self.UB = nc.dram_tensor("ub_s", [32, 128, 8, 512], BF16, kind="Internal").ap()
        self.VB = nc.dram_tensor("vb_s", [32, 128, 4, 1024], BF16, kind="Internal").ap()

    def T(self, st, name, shape, dt):
        self._tn = getattr(self, "_tn", 0) + 1
        return st.enter_context(self.nc.sbuf_tensor("%s_%d" % (name, self._tn), list(shape), dt))

    def mm(self, out, lhsT, rhs, start, stop, reads, writes, skip=False):
        self.P.op("pe", lambda e: e.matmul(out, lhsT, rhs, start=start, stop=stop, skip_group_check=skip),
                  reads=reads, writes=writes)

    def tr(self, out, in_, ident, reads, writes):
        self.P.op("pe", lambda e: e.transpose(out, in_, ident), reads=reads, writes=writes)

    def bank_bf(self, b):
        return self.B[b][:].bitcast(BF16)

    def build(self):
        nc = self.nc
        with ExitStack() as g:
            P = self.P = Prog(nc, g)
            self.B = [g.enter_context(nc.psum_tensor("B%d" % i, [128, 512], F32)) for i in range(8)]
            self.X = self.T(g, "X", [128, NT, D], F32)
            self.identf = self.T(g, "identf", [128, 128], F32)
            self.identb = self.T(g, "identb", [128, 128], BF16)
            self.antib = self.T(g, "antib", [128, 128], BF16)
            self.anti127 = self.T(g, "anti127", [128, 128], BF16)
            self.ones_row = self.T(g, "ones_row", [1, 128], BF16)
            self.one11 = self.T(g, "one11", [1, 1], F32)
            self.GB = self.T(g, "GB", [128, D], BF16)
            self.SHB = self.T(g, "SHB", [128, D], BF16)
            self.GTB = self.T(g, "GTB", [128, D], BF16)
            self.onec = self.T(g, "onec", [128, 1], F32)
            self.onesf = self.T(g, "onesf", [8, 128], F32)
            self.rd = self.T(g, "rd", [128, 32], F32)
            self.ntmp = self.T(g, "ntmp", [128, D], F32)
            self.hbf = self.T(g, "hbf", [128, D], BF16)
            self.hT = self.T(g, "hT", [128, 8, 128], BF16)
            self.sm = self.T(g, "sm", [128, 64], F32)
            tmpi = self.T(g, "tmpi", [128, 128], F32)

            P.op("pool", lambda e: e.iota(tmpi[:], [[1, 128]], base=0, channel_multiplier=-1,
                                          allow_small_or_imprecise_dtypes=True), writes=["tmpi"])
            P.op("dve", lambda e: e.tensor_scalar(self.identf[:], tmpi[:], 0.0, None, ALU.is_equal), reads=["tmpi"], writes=["identf"])
            P.op("dve", lambda e: e.tensor_scalar(self.identb[:], tmpi[:], 0.0, None, ALU.is_equal), reads=["tmpi"], writes=["identb"])
            P.op("pool", lambda e: e.iota(tmpi[:], [[1, 128]], base=-127, channel_multiplier=1,
                                          allow_small_or_imprecise_dtypes=True), reads=["identb", "identf"], writes=["tmpi"])
            P.op("dve", lambda e: e.tensor_scalar(self.antib[:], tmpi[:], 0.0, None, ALU.is_equal), reads=["tmpi"], writes=["antib"])
            P.op("dve", lambda e: e.tensor_scalar(self.anti127[:], tmpi[:], -1.0, None, ALU.is_equal), reads=["tmpi"], writes=["anti127"])
            P.op("pool", lambda e: e.memset(self.ones_row[:], 1.0), writes=["ones_row"])
            P.op("pool", lambda e: e.memset(self.one11[:], 1.0), writes=["one11"])
            P.op("pool", lambda e: e.memset(self.onec[:], 1.0), writes=["onec"])
            P.op("pool", lambda e: e.memset(self.onesf[:], 1.0), writes=["onesf"])

            for tb in range(NT):
                P.dma(self.X[:, tb, :], self.I["x"][tb * 128:(tb + 1) * 128, :], writes=[("X", tb)])

            self.adaln()
            P.barrier()
            done = False
            tables_ready = False
            for l in range(self.n_layers):
                if l % 2 == 0:
                    self.even_layer(l)
                else:
                    if not tables_ready:
                        self.rel_tables()
                        tables_ready = True
                    self.odd_layer(l)
                self.snapshot("xm_%d" % l)
                if self.stop == "L%dmix" % l:
                    break
                if self.do_peer:
                    self.peer_layer(l)
                    self.snapshot("x_%d" % l)
                if self.stop == "L%d" % l:
                    break
            P.barrier()
            for tb in range(NT):
                P.dma(self.y_out[tb * 128:(tb + 1) * 128, :], self.X[:, tb, :], reads=[("X", tb)], is_output=True)
            P.finish()
        return nc

    def snapshot(self, name):
        if not self.snaps:
            return
        t = self.nc.dram_tensor("snap_" + name, [S, D], F32, kind="ExternalOutput").ap()
        self.snap_names.append(name)
        for tb in range(NT):
            self.P.dma(t[tb * 128:(tb + 1) * 128, :], self.X[:, tb, :], reads=[("X", tb)], is_output=True)

    def adaln(self):
        P = self.P
        I = self.I
        with ExitStack() as st:
            crow = self.T(st, "crow", [1, D], F32)
            srow = self.T(st, "srow", [1, D], F32)
            scol = self.T(st, "scol", [128, 8], F32)
            brow = self.T(st, "brow", [1, 6 * D], F32)
            grow = self.T(st, "grow", [1, 2 * D], F32)
            mrow = self.T(st, "mrow", [1, 6 * D], F32)
            wts = [self.T(st, "adw%d" % k, [128, 8, 512], F32) for k in range(2)]
            P.dma(crow[:], I["c"], writes=["crow"])
            P.op("act", lambda e: e.activation(srow[:], crow[:], AF.Silu), reads=["crow"], writes=["srow"])
            ps = self.B[0]
            for dc in range(8):
                self.mm(ps[:, dc:dc + 1], srow[0:1, dc * 128:(dc + 1) * 128], self.one11[:], True, True,
                        ["srow", "one11"], [("B0", dc)])
            P.op("dve", lambda e: e.tensor_copy(scol[:], ps[:, 0:8]), reads=["B0"], writes=["scol"])
            n = 0
            for l in range(self.n_layers):
                P.dma(brow[:], I["ada_b"][0:1, l * 6144:(l + 1) * 6144], writes=["brow"])
                P.dma(grow[:], I["norm_g"][0:1, l * 2048:(l + 1) * 2048], writes=["grow"])
                for nt in range(12):
                    wt = wts[n % 2]
                    wk = "adw%d" % (n % 2)
                    P.dma(wt[:], I["ada_w"][l, :, nt * 512:(nt + 1) * 512].rearrange("(c p) n -> p c n", p=128), writes=[wk])
                    pb = self.B[1 + n % 2]
                    pk = "B%d" % (1 + n % 2)
                    for dc in range(8):
                        self.mm(pb[0:1, :], scol[:, dc:dc + 1], wt[:, dc, :], dc == 0, dc == 7, ["scol", wk], [pk])
                    P.op("dve", lambda e: e.tensor_tensor(mrow[0:1, nt * 512:(nt + 1) * 512], pb[0:1, :],
                                                          brow[0:1, nt * 512:(nt + 1) * 512], ALU.add),
                         reads=[pk, "brow"], writes=[("mrow", nt)])
                    n += 1
                for k in range(2):
                    sc = mrow[0:1, (3 * k + 1) * D:(3 * k + 2) * D]
                    ng = grow[0:1, k * D:(k + 1) * D]
                    P.op("dve", lambda e: e.scalar_tensor_tensor(sc, sc, 1.0, ng, ALU.add, ALU.mult), reads=["mrow", "grow"], writes=["mrow"])
                P.dma(self.MODS[l].rearrange("k d -> (k d)").unsqueeze(0), mrow[:], reads=["mrow"], writes=[("MODS", l)])

    def load_mods(self, l, k):
        P = self.P
        for j, (t, nm) in zip((1, 0, 2), ((self.GB, "GB"), (self.SHB, "SHB"), (self.GTB, "GTB"))):
            P.dma(t[:], self.MODS[l, 3 * k + j:3 * k + j + 1, :].broadcast_to([128, D]), reads=[("MODS", l)], writes=[nm], q="pool")

    def norm_hT(self, tb):
        P = self.P
        xt = self.X[:, tb, :]
        sm = self.sm
        P.op("act", lambda e: e.activation(self.ntmp[:], xt, AF.Square, accum_out=sm[:, 0:1]), reads=[("X", tb)], writes=["ntmp", ("sm", 0)])
        P.op("dve", lambda e: e.tensor_scalar(sm[:, 1:2], sm[:, 0:1], 1.0 / D, 1e-6, ALU.mult, ALU.add), reads=[("sm", 0)], writes=[("sm", 1)])
        P.op("act", lambda e: e.activation(sm[:, 2:3], sm[:, 1:2], AF.Sqrt), reads=[("sm", 1)], writes=[("sm", 2)])
        P.op("dve", lambda e: e.reciprocal(sm[:, 3:4], sm[:, 2:3]), reads=[("sm", 2)], writes=[("sm", 3)])
        P.op("dve", lambda e: e.scalar_tensor_tensor(self.ntmp[:], xt, sm[:, 3:4], self.GB[:], ALU.mult, ALU.mult),
             reads=[("X", tb), ("sm", 3), "GB"], writes=["ntmp"])
        P.op("pool", lambda e: e.tensor_tensor(self.hbf[:], self.ntmp[:], self.SHB[:], ALU.add), reads=["ntmp", "SHB"], writes=["hbf"])
        bt = self.bank_bf(0)
        for c in range(8):
            self.tr(bt[:, c * 128:(c + 1) * 128], self.hbf[:, c * 128:(c + 1) * 128], self.identb[:], ["hbf", "identb"], [("B0", c)])
        P.op("act", lambda e: e.copy(self.hT[:], bt[:, 0:1024].rearrange("p (c t) -> p c t", c=8)), reads=["B0"], writes=["hT"])

    def proj(self, bank, ncols, w, wkey, c0):
        for dc in range(8):
            self.mm(self.B[bank][:, 0:ncols], self.hT[:, dc, :], w[:, dc, c0:c0 + ncols], dc == 0, dc == 7,
                    ["hT", wkey], ["B%d" % bank])

    def head_rmsnorm(self, src, srckey, nh, gb, gbkey, out_ap, outkey, sq, rs, npart=128):
        P = self.P
        n = nh * 64
        P.op("act", lambda e: e.activation(sq[:, 0:n], src, AF.Square), reads=[srckey], writes=["sq"])
        P.op("dve", lambda e: e.tensor_reduce(rs[:, 0:nh], sq[:, 0:n].rearrange("p (h d) -> p h d", d=64), AX.X, ALU.add), reads=["sq"], writes=[("rs", 0)])
        P.op("dve", lambda e: e.tensor_scalar(rs[:, 16:16 + nh], rs[:, 0:nh], 1.0 / 64, 1e-6, ALU.mult, ALU.add), reads=[("rs", 0)], writes=[("rs", 1)])
        P.op("act", lambda e: e.activation(rs[:, 32:32 + nh], rs[:, 16:16 + nh], AF.Sqrt), reads=[("rs", 1)], writes=[("rs", 2)])
        P.op("dve", lambda e: e.reciprocal(rs[:, 48:48 + nh], rs[:, 32:32 + nh]), reads=[("rs", 2)], writes=[("rs", 3)])
        P.op("dve", lambda e: e.tensor_tensor(sq[:, 0:n].rearrange("p (h d) -> p h d", d=64), src.rearrange("p (h d) -> p h d", d=64),
                                              rs[:, 48:48 + nh].unsqueeze(2).broadcast_to([npart, nh, 64]), ALU.mult),
             reads=[srckey, ("rs", 3), "sq"], writes=["sq"])
        P.op("pool", lambda e: e.tensor_tensor(out_ap, sq[:, 0:n].rearrange("p (h d) -> p h d", d=64),
                                               gb.unsqueeze(1).broadcast_to([npart, nh, 64]), ALU.mult),
             reads=["sq", gbkey], writes=[outkey])

    def run_attn(self, tasks):
        P = self.P

        def emit_qk(n, t):
            bi = 5 + n % 2
            sb = self.B[bi]
            key = "B%d" % bi
            for (c0, w, nsub, mms) in t["groups"]:
                out = sb[0:t["nrow"], c0:c0 + w]
                if nsub > 1:
                    out = out.rearrange("p (a b) -> p a b", a=nsub)
                for k, (lhsT, rhs, rd) in enumerate(mms):
                    self.mm(out, lhsT, rhs, k == 0, k == len(mms) - 1, rd, [key])

        def emit_rest(n, t):
            bi = 5 + n % 2
            sb = self.B[bi]
            key = "B%d" % bi
            pt = self.PT[n % 3]
            pk = ("PT", n % 3)
            nr, wd = t["nrow"], t["width"]
            exps = t.get("exps") or [(0, wd, None, [])]
            for (c0, w, bias, rd) in exps:
                if bias is None:
                    P.op("act", lambda e: e.activation(pt[0:nr, c0:c0 + w], sb[0:nr, c0:c0 + w], AF.Exp, scale=0.125), reads=[key], writes=[pk])
                else:
                    P.op("act", lambda e: e.activation(pt[0:nr, c0:c0 + w], sb[0:nr, c0:c0 + w], AF.Exp, bias=bias, scale=0.125),
                         reads=[key] + rd, writes=[pk])
            for (pc0, pw, rhs, out, outkey, start, stop, rd) in t["pv"]:
                self.mm(out, pt[0:nr, pc0:pc0 + pw], rhs, start, stop, [pk] + rd, [outkey], skip=True)

        if not tasks:
            return
        emit_qk(0, tasks[0])
        for n, t in enumerate(tasks):
            if n + 1 < len(tasks):
                emit_qk(n + 1, tasks[n + 1])
            emit_rest(n, t)

    def out_proj_residual(self, i, wout):
        P = self.P
        Obf = self.hbf
        bt = self.bank_bf(0)
        for c in range(8):
            self.tr(bt[:, c * 128:(c + 1) * 128], Obf[:, c * 128:(c + 1) * 128], self.identb[:], ["hbf", "identb"], [("B0", c)])
        P.op("act", lambda e: e.copy(self.OT[:], bt[:, 0:1024].rearrange("p (c t) -> p c t", c=8)), reads=["B0"], writes=["OT"])
        for half in range(2):
            bk = 3 + half
            for fc in range(8):
                self.mm(self.B[bk][:, :], self.OT[:, fc, :], wout[:, fc, half * 512:(half + 1) * 512], fc == 0, fc == 7,
                        ["OT", "wout"], ["B%d" % bk])
            P.op("dve", lambda e: e.tensor_tensor(self.ntmp[:, half * 512:(half + 1) * 512], self.B[bk][:, :],
                                                  self.GTB[:, half * 512:(half + 1) * 512], ALU.mult),
                 reads=["B%d" % bk, "GTB"], writes=[("ntmp", half)])
            P.op("pool", lambda e: e.tensor_tensor(self.X[:, i, half * 512:(half + 1) * 512], self.X[:, i, half * 512:(half + 1) * 512],
                                                   self.ntmp[:, half * 512:(half + 1) * 512], ALU.add),
                 reads=[("ntmp", half), ("X", i)], writes=[("X", i)])

    def even_layer(self, l):
        P = self.P
        I = self.I
        li = l // 2
        w_in = I["even_w_in"][li]
        self.load_mods(l, 0)
        with ExitStack() as lay:
            kT = self.T(lay, "kT", [128, 4, S], BF16)
            Vp = self.T(lay, "Vp", [128, NT, 8, 65], BF16)
            cTT = self.T(lay, "cTT", [128, NT, 8], F32)
            rbc = self.T(lay, "rbc", [128, NT, 8], F32)
            bcol = self.T(lay, "bcol", [128, 8, NT], F32)
            kgb = self.T(lay, "kgb", [128, 64], F32)
            qgb = self.T(lay, "qgb", [128, 64], F32)
            sq = self.T(lay, "sq", [128, 1024], F32)
            rs = self.T(lay, "rs", [128, 64], F32)
            kn = self.T(lay, "kn", [128, 512], BF16)
            qT = self.T(lay, "qT", [128, 4, 128], BF16)
            self.OT = self.T(lay, "OT", [128, 8, 128], BF16)
            self.PT = [self.T(lay, "PT%d" % k, [128, 512], BF16) for k in range(3)]
            P.dma(kgb[:], I["even_k_g"][li:li + 1, :].broadcast_to([128, 64]), writes=["kgb"])
            P.dma(qgb[:], I["even_q_g"][li:li + 1, :].broadcast_to([128, 64]), writes=["qgb"])
            P.op("pool", lambda e: e.memset(Vp[:], 1.0), writes=["Vp"])
            with ExitStack() as ph:
                wkv = self.T(ph, "wkv", [128, 8, 1032], BF16)
                fT = self.T(ph, "fT", [8, S], F32)
                cT = self.T(ph, "cT", [8, S], F32)
                rsel = self.T(ph, "rsel", [8, NT, 8], F32)
                fcol = self.T(ph, "fcol", [128, 8], F32)
                negb = self.T(ph, "negb", [8, 1], F32)
                P.dma(wkv[:], w_in[:, 2048:3080].rearrange("(c p) n -> p c n", p=128), writes=["wkv"], q="pool")
                P.dma(negb[:], I["even_b_f"][li].rearrange("(h o) -> h o", o=1), writes=["negb"])
                P.op("dve", lambda e: e.tensor_scalar(negb[:], negb[:], -1.0, None, ALU.mult), reads=["negb"], writes=["negb"])
                for tb in range(NT):
                    self.peer_convert_step(l, tb)
                    self.norm_hT(tb)
                    self.proj(1, 512, wkv, "wkv", 0)
                    self.proj(2, 512, wkv, "wkv", 512)
                    self.proj(3, 8, wkv, "wkv", 1024)
                    self.head_rmsnorm(self.B[1][:, :], "B1", 8, kgb[:], "kgb", kn[:].rearrange("p (h d) -> p h d", d=64), "kn", sq, rs)
                    b4 = self.bank_bf(4)
                    for c in range(4):
                        self.tr(b4[:, c * 128:(c + 1) * 128], kn[:, c * 128:(c + 1) * 128], self.identb[:], ["kn", "identb"], [("B4", c)])
                    P.op("act", lambda e: e.copy(kT[:, :, tb * 128:(tb + 1) * 128], b4[:, 0:512].rearrange("p (c t) -> p c t", c=4)),
                         reads=["B4"], writes=[("kT", tb)])
                    P.op("act", lambda e: e.copy(Vp[:, tb, :, 0:64], self.B[2][:, :].rearrange("p (h d) -> p h d", d=64)),
                         reads=["B2"], writes=[("Vp", tb)])
                    P.op("dve", lambda e: e.tensor_copy(fcol[:], self.B[3][:, 0:8]), reads=["B3"], writes=["fcol"])
                    self.tr(self.B[7][0:8, 0:128], fcol[:], self.identf[:], ["fcol", "identf"], ["B7"])
                    P.op("act", lambda e: e.copy(fT[:, tb * 128:(tb + 1) * 128], self.B[7][0:8, 0:128]), reads=["B7"], writes=[("fT", tb)])
                P.op("act", lambda e: e.activation(fT[:], fT[:], AF.Exp, bias=negb[:], scale=-1.0), reads=["fT", "negb"], writes=["fT"])
                P.op("act", lambda e: e.activation(fT[:], fT[:], AF.Ln, bias=1.0, scale=1.0), reads=["fT"], writes=["fT"])
                P.op("dve", lambda e: e.tensor_scalar(fT[:], fT[:], -1.0, None, ALU.mult), reads=["fT"], writes=["fT"])
                P.op("dve", lambda e: e.tensor_tensor_scan(cT[:], self.onec[0:8, 0:1].broadcast_to([8, S]), fT[:], 0.0, ALU.mult, ALU.add),
                     reads=["onec", "fT"], writes=["cT"])
                for j in range(NT):
                    self.tr(self.B[1][:, j * 8:(j + 1) * 8], cT[:, j * 128:(j + 1) * 128], self.identf[0:8, 0:8], ["cT", "identf"], [("B1", j)])
                P.op("dve", lambda e: e.tensor_copy(cTT[:].rearrange("p j h -> p (j h)"), self.B[1][:, 0:128]), reads=["B1"], writes=["cTT"])
                P.op("dve", lambda e: e.tensor_tensor(rsel[:], cT[:, 64:64 + 128 * 15 + 1:128].unsqueeze(2).broadcast_to([8, NT, 8]),
                                                      self.identf[0:8, 0:8].unsqueeze(1).broadcast_to([8, NT, 8]), ALU.mult),
                     reads=["cT", "identf"], writes=["rsel"])
                self.mm(self.B[2][:, 0:128], self.onesf[:], rsel[:].rearrange("p i h -> p (i h)"), True, True, ["onesf", "rsel"], ["B2"])
                P.op("dve", lambda e: e.tensor_copy(rbc[:].rearrange("p i h -> p (i h)"), self.B[2][:, 0:128]), reads=["B2"], writes=["rbc"])
                P.barrier()
            with ExitStack() as ph:
                wq2 = self.T(ph, "wq2", [128, 8, 2048], BF16)
                wout = self.T(ph, "wout", [128, 8, D], BF16)
                cwb = self.T(ph, "cwb", [128, 3, 512], BF16)
                shm = self.T(ph, "shm", [128, 4, 128], BF16)
                caus = self.T(ph, "caus", [128, 128], BF16)
                uw = [self.T(ph, "uw%d" % k, [128, 3, 512], BF16) for k in range(2)]
                ccs = sq[:, 0:512]
                cbs = sq[:, 512:1024]
                ucur = self.ntmp[:, 0:512]
                Obf = self.hbf
                P.dma(wq2[:], w_in[:, 0:2048].rearrange("(c p) n -> p c n", p=128), writes=["wq2"], q="pool")
                P.dma(wout[:], I["even_w_out"][li].rearrange("(c p) n -> p c n", p=128), writes=["wout"], q="pool")
                P.dma(cwb[:].rearrange("p k c -> p (k c)"), I["even_conv_w"][li:li + 1, :].broadcast_to([128, 1536]), writes=["cwb"], q="pool")
                P.dma(shm[:], I["k_shm"], writes=["shm"], q="pool")
                P.dma(caus[:], I["k_caus"], writes=["caus"], q="pool")
                for i in range(NT):
                    self.norm_hT(i)
                    for k in range(4):
                        self.proj(1 + k, 512, wq2, "wq2", 512 * k)
                    P.op("act", lambda e: e.copy(ccs, self.B[2][:, :]), reads=["B2"], writes=["sq"])
                    P.op("act", lambda e: e.copy(cbs, self.B[1][:, :]), reads=["B1"], writes=["sq"])
                    P.op("dve", lambda e: e.tensor_tensor(ucur, ccs, self.B[3][:, :], ALU.mult), reads=["sq", "B3"], writes=["ntmp"])
                    uwc, uwp = uw[i % 2], uw[(i + 1) % 2]
                    kc_, kp_ = "uw%d" % (i % 2), "uw%d" % ((i + 1) % 2)
                    P.op("pool", lambda e: e.tensor_tensor(uwc[:], ucur.unsqueeze(1).broadcast_to([128, 3, 512]), cwb[:], ALU.mult),
                         reads=["ntmp", "cwb"], writes=[kc_])
                    mms = [(self.identb[:], uwc[:, 2, :], [kc_]), (shm[:, 0, :], uwc[:, 1, :], [kc_, "shm"]), (shm[:, 1, :], uwc[:, 0, :], [kc_, "shm"])]
                    if i > 0:
                        mms += [(shm[:, 2, :], uwp[:, 1, :], [kp_, "shm"]), (shm[:, 3, :], uwp[:, 0, :], [kp_, "shm"])]
                    for k, (lt, rh, rd) in enumerate(mms):
                        self.mm(self.B[2][:, :], lt, rh, k == 0, k == len(mms) - 1, rd + ["identb"], ["B2"])
                    P.op("dve", lambda e: e.tensor_tensor(Obf[:, 0:512], cbs, self.B[2][:, :], ALU.mult), reads=["sq", "B2"], writes=["hbf"])
                    self.head_rmsnorm(self.B[4][:, :], "B4", 8, qgb[:], "qgb", kn[:].rearrange("p (h d) -> p h d", d=64), "kn", sq, rs)
                    b1 = self.bank_bf(1)
                    for c in range(4):
                        self.tr(b1[:, c * 128:(c + 1) * 128], kn[:, c * 128:(c + 1) * 128], self.identb[:], ["kn", "identb"], [("B1", c)])
                    P.op("act", lambda e: e.copy(qT[:], b1[:, 0:512].rearrange("p (c t) -> p c t", c=4)), reads=["B1"], writes=["qT"])
                    for h in range(8):
                        base = (h % 2) * 64
                        pr = h // 2
                        P.op("dve", lambda e: e.tensor_scalar(bcol[:, h, 0:i + 1], cTT[:, 0:i + 1, h], rbc[:, i, h:h + 1], -1.0, ALU.subtract, ALU.mult),
                             reads=["cTT", "rbc"], writes=[("bcol", h)])
                        tasks = []
                        oi = self.B[7][:, (h % 4) * 65:(h % 4) * 65 + 65]
                        oik = ("B7", h % 4)
                        for j0 in range(0, i + 1, 4):
                            js = list(range(j0, min(j0 + 4, i + 1)))
                            groups = []
                            exps = []
                            pv = []
                            for jj, j in enumerate(js):
                                mm_ = [(kT[base:base + 64, pr, j * 128:(j + 1) * 128], qT[base:base + 64, pr, :], ["kT", "qT"])]
                                if j == i:
                                    mm_.append((self.identb[:], caus[:], ["identb", "caus"]))
                                groups.append((jj * 128, 128, 1, mm_))
                                exps.append((jj * 128, 128, bcol[:, h, j:j + 1], [("bcol", h)]))
                                pv.append((jj * 128, 128, Vp[:, j, h, :], oi, oik, j == 0, j == i, ["Vp"]))
                            tasks.append(dict(nrow=128, width=len(js) * 128, groups=groups, exps=exps, pv=pv))
                        self.run_attn(tasks)
                        P.op("dve", lambda e: e.reciprocal(self.rd[:, h:h + 1], oi[:, 64:65]), reads=[oik], writes=[("rd", h)])
                        P.op("dve", lambda e: e.tensor_scalar(Obf[:, 512 + h * 64:512 + (h + 1) * 64], oi[:, 0:64], self.rd[:, h:h + 1], None, ALU.mult),
                             reads=[oik, ("rd", h)], writes=["hbf"])
                    self.out_proj_residual(i, wout)
                P.barrier()

    def rel_tables(self):
        P = self.P
        I = self.I
        with ExitStack() as st:
            tab = self.T(st, "tab", [32, 16], F32)
            ohb = self.T(st, "ohb", [32, 2048], F32)
            fvr = self.T(st, "fvr", [16, 4096], BF16)
            P.dma(tab[:], I["rel_table"], writes=["tab"])
            P.dma(ohb[:], I["k_ohb"], writes=["ohb"])
            P.op("pool", lambda e: e.memset(fvr[:], -BIG), writes=["fvr"])
            for q in range(4):
                self.mm(self.B[1][0:16, :], tab[:], ohb[:, q * 512:(q + 1) * 512], True, True, ["tab", "ohb"], ["B1"])
                P.op("dve", lambda e: e.tensor_scalar(fvr[:, 2048 + q * 512:2048 + (q + 1) * 512], self.B[1][0:16, :], 8.0, None, ALU.mult),
                     reads=["B1"], writes=["fvr"])
            P.dma(self.FV, fvr[:], reads=["fvr"], writes=["FV"])
            P.op("pool", lambda e: e.memset(fvr[:, 2048 + 512:4096], -BIG), reads=["fvr"], writes=["fvr"])
            P.dma(self.FW, fvr[:], reads=["fvr"], writes=["FW"])
            P.barrier()

    def odd_layer(self, l):
        P = self.P
        I = self.I
        li = l // 2
        w_in = I["odd_w_in"][li]
        rd = self.rd
        self.load_mods(l, 0)
        with ExitStack() as lay:
            ksT = self.T(lay, "ksT", [128, 2, S], BF16)
            kwT = self.T(lay, "kwT", [128, 2, S], BF16)
            Vs = self.T(lay, "Vs", [128, NT, 4, 65], BF16)
            Vw = self.T(lay, "Vw", [128, NT, 4, 65], BF16)
            KcT = self.T(lay, "KcT", [128, 2, 128], BF16)
            VcX = self.T(lay, "VcX", [128, 4, 97], BF16)
            gbs = self.T(lay, "gbs", [128, 4, 64], F32)
            sq = self.T(lay, "sq", [128, 1024], F32)
            rs = self.T(lay, "rs", [128, 64], F32)
            D0 = self.T(lay, "D0", [128, 16, 128], BF16)
            D1 = self.T(lay, "D1", [128, 16, 128], BF16)
            D4 = self.T(lay, "D4", [128, 16, 128], BF16)
            CROW = self.T(lay, "CROW", [1, 16, 128], BF16)
            for (t, nm, src, k) in ((D0, "D0", self.FV, 0), (D1, "D1", self.FV, 1), (D4, "D4", self.FW, 4)):
                ap = bass.AP(tensor=src.tensor, offset=2048 + 128 * k - 127, ap=[[1, 128], [4096, 16], [1, 128]])
                P.dma(t[:], ap, writes=[nm])
            ap = bass.AP(tensor=self.FV.tensor, offset=2048 + 1000, ap=[[0, 1], [4096, 16], [1, 128]])
            P.dma(CROW[:], ap, writes=["CROW"])
            P.dma(gbs[:, 0, :], I["odd_q_g"][li:li + 1, :].broadcast_to([128, 64]), writes=[("gbs", 0)])
            P.dma(gbs[:, 1:4, :].rearrange("p k d -> p (k d)"), I["odd_k_g"][li:li + 1, :].broadcast_to([128, 192]), writes=[("gbs", 1)])
            P.op("pool", lambda e: e.memset(Vs[:], 1.0), writes=["Vs"])
            P.op("pool", lambda e: e.memset(Vw[:], 1.0), writes=["Vw"])
            P.op("pool", lambda e: e.memset(VcX[:], 1.0), writes=["VcX"])
            with ExitStack() as ph:
                wkv = self.T(ph, "wkv", [128, 8, 1536], BF16)
                kcT = self.T(ph, "kcT", [128, 2, S], BF16)
                vcT = self.T(ph, "vcT", [128, 2, S], BF16)
                kvb = self.T(ph, "kvb", [128, 4, 256], BF16)
                w1b = [self.T(ph, "w1b%d" % a, [128, 32, 64], BF16) for a in range(2)]
                w2b = [self.T(ph, "w2b%d" % a, [64, 64], BF16) for a in range(2)]
                pos = self.T(ph, "pos", [32, 2, 64], F32)
                posT = self.T(ph, "posT", [64, 2, 32], BF16)
                cst = self.T(ph, "cst", [64, 2], F32)
                HT = self.T(ph, "HT", [64, 128], BF16)
                KcN = self.T(ph, "KcN", [128, 4, 64], BF16)
                ovl = self.T(ph, "ovl", [127, 32], F32)
                P.dma(wkv[:], w_in[:, 1024:2560].rearrange("(c p) n -> p c n", p=128), writes=["wkv"], q="pool")
                for a in range(2):
                    for hf in range(2):
                        P.dma(w1b[a][hf * 64:(hf + 1) * 64, :, :], I["odd_cmp_w1"][li, a].rearrange("(l d) o -> d l o", d=64),
                              writes=[("w1b%d" % a, hf)], q="pool")
                    P.dma(w2b[a][:], I["odd_cmp_w2"][li, a], writes=["w2b%d" % a], q="pool")
                    P.dma(pos[:, a, :], I["odd_cmp_pos"][li, a], writes=[("pos", a)])
                P.dma(ovl[:], I["k_ovl"], writes=["ovl"])
                P.op("dve", lambda e: e.tensor_copy(VcX[0:127, :, 65:97], ovl[:].unsqueeze(1).broadcast_to([127, 4, 32])),
                     reads=["ovl", "VcX"], writes=["VcX"])
                for tb in range(NT):
                    self.peer_convert_step(l, tb)
                    self.norm_hT(tb)
                    self.proj(1, 512, wkv, "wkv", 0)
                    self.proj(2, 512, wkv, "wkv", 512)
                    self.proj(3, 512, wkv, "wkv", 1024)
                    P.op("act", lambda e: e.copy(kvb[:, 0:2, :], self.B[1][:, :].rearrange("p (a n) -> p a n", a=2)), reads=["B1"], writes=[("kvb", 0)])
                    self.head_rmsnorm(self.B[2][:, 0:256], "B2", 4, gbs[:, 2, :], ("gbs", 1), kvb[:, 2, :].rearrange("p (h d) -> p h d", d=64), ("kvb", 2), sq, rs)
                    self.head_rmsnorm(self.B[3][:, 0:256], "B3", 4, gbs[:, 3, :], ("gbs", 1), kvb[:, 3, :].rearrange("p (h d) -> p h d", d=64), ("kvb", 3), sq, rs)
                    P.op("act", lambda e: e.copy(Vs[:, tb, :, 0:64], self.B[2][:, 256:512].rearrange("p (h d) -> p h d", d=64)), reads=["B2"], writes=[("Vs", tb)])
                    P.op("act", lambda e: e.copy(Vw[:, tb, :, 0:64], self.B[3][:, 256:512].rearrange("p (h d) -> p h d", d=64)), reads=["B3"], writes=[("Vw", tb)])
                    b4 = self.bank_bf(4)
                    for a in range(4):
                        for c in range(2):
                            self.tr(b4[:, (a * 2 + c) * 128:(a * 2 + c + 1) * 128], kvb[:, a, c * 128:(c + 1) * 128], self.identb[:],
                                    ["kvb", "identb"], [("B4", a * 2 + c)])
                    for a, (dst, nm) in enumerate(((kcT, "kcT"), (vcT, "vcT"), (ksT, "ksT"), (kwT, "kwT"))):
                        P.op("act", lambda e: e.copy(dst[:, :, tb * 128:(tb + 1) * 128], b4[:, a * 256:(a + 1) * 256].rearrange("p (c t) -> p c t", c=2)),
                             reads=["B4"], writes=[(nm, tb)])
                for a in range(2):
                    self.tr(self.B[1][0:64, 0:32], pos[:, a, :], self.identf[0:32, 0:32], ["pos", "identf"], ["B1"])
                    P.op("act", lambda e: e.copy(posT[:, a, :], self.B[1][0:64, 0:32]), reads=["B1"], writes=[("posT", a)])
                    for lq in range(32):
                        self.mm(self.B[2][0:64, 0:1], w1b[a][0:64, lq, :], posT[:, a, lq:lq + 1], lq == 0, lq == 31, ["w1b%d" % a, ("posT", a)], ["B2"])
                    P.op("dve", lambda e: e.tensor_copy(cst[:, a:a + 1], self.B[2][0:64, 0:1]), reads=["B2"], writes=[("cst", a)])
                for a, srcT in enumerate((kcT, vcT)):
                    for gq in range(4):
                        base = (gq % 2) * 64
                        ch = gq // 2
                        for lq in range(32):
                            rhs = srcT[base:base + 64, ch, lq:lq + 16 * 126 + 1:16]
                            self.mm(self.B[5][0:64, 0:127], w1b[a][base:base + 64, lq, :], rhs, lq == 0, lq == 31,
                                    ["w1b%d" % a, "kcT", "vcT"], ["B5"])
                        P.op("act", lambda e: e.activation(HT[:, 0:127], self.B[5][0:64, 0:127], AF.Gelu_apprx_tanh, bias=cst[:, a:a + 1], scale=1.0),
                             reads=["B5", ("cst", a)], writes=["HT"])
                        self.mm(self.B[6][0:127, 0:64], HT[:, 0:127], w2b[a][:], True, True, ["HT", "w2b%d" % a], ["B6"])
                        if a == 0:
                            self.head_rmsnorm(self.B[6][0:127, 0:64], "B6", 1, gbs[0:127, 1, :], ("gbs", 1), KcN[0:127, gq:gq + 1, :], ("KcN", gq),
                                              sq[0:127], rs[0:127], npart=127)
                        else:
                            P.op("act", lambda e: e.copy(VcX[0:127, gq, 0:64], self.B[6][0:127, 0:64]), reads=["B6"], writes=[("VcX", gq)])
                b4 = self.bank_bf(4)
                for c in range(2):
                    self.tr(b4[:, c * 128:c * 128 + 127], KcN[0:127, 2 * c:2 * c + 2, :].rearrange("p g d -> p (g d)"), self.identb[0:127, 0:127],
                            ["KcN", "identb"], [("B4", c)])
                    P.op("act", lambda e: e.copy(KcT[:, c, 0:127], b4[:, c * 128:c * 128 + 127]), reads=[("B4", c)], writes=[("KcT", c)])
                P.barrier()
            with ExitStack() as ph:
                wq = self.T(ph, "wq", [128, 8, 1072], BF16)
                wout = self.T(ph, "wout", [128, 8, D], BF16)
                bgb = self.T(ph, "bgb", [128, 48], F32)
                gates = self.T(ph, "gates", [128, 48], F32)
                a1 = self.T(ph, "a1", [128, 16, 32], BF16)
                a0 = self.T(ph, "a0", [128, 16, 32], BF16)
                ej = self.T(ph, "ej", [32, 16, 128], BF16)
                qn = self.T(ph, "qn", [128, D], BF16)
                qT = self.T(ph, "qT", [128, 8, 128], BF16)
                Of = self.ntmp
                imp = self.T(ph, "imp", [128, 4, 32], F32)
                impw = self.T(ph, "impw", [128, 4, 32], F32)
                nmk = self.T(ph, "nmk", [128, 4, 32], BF16)
                NMT = self.T(ph, "NMT", [32, 4, 128], BF16)
                bci = self.T(ph, "bci", [127, 16, 128], BF16)
                self.OT = self.T(ph, "OT", [128, 8, 128], BF16)
                self.PT = [self.T(ph, "PT%d" % k, [128, 512], BF16) for k in range(3)]
                P.dma(wq[:, :, 0:1024], w_in[:, 0:1024].rearrange("(c p) n -> p c n", p=128), writes=[("wq", 0)], q="pool")
                P.dma(wq[:, :, 1024:1072], w_in[:, 2560:2608].rearrange("(c p) n -> p c n", p=128), writes=[("wq", 1)], q="pool")
                P.dma(wout[:], I["odd_w_out"][li].rearrange("(c p) n -> p c n", p=128), writes=["wout"], q="pool")
                P.dma(bgb[:], I["odd_b_gate"][li:li + 1, :].broadcast_to([128, 48]), writes=["bgb"])
                P.dma(a1[:], I["k_a1"], writes=["a1"], q="pool")
                P.dma(a0[:], I["k_a0"], writes=["a0"], q="pool")
                P.dma(ej[:], I["k_ej"], writes=["ej"], q="pool")
                for i in range(NT):
                    self.norm_hT(i)
                    self.proj(1, 512, wq, "wq", 0)
                    self.proj(2, 512, wq, "wq", 512)
                    self.proj(3, 48, wq, "wq", 1024)
                    P.op("dve", lambda e: e.tensor_tensor(gates[:], self.B[3][:, 0:48], bgb[:], ALU.add), reads=["B3", "bgb"], writes=["gates"])
                    P.op("act", lambda e: e.activation(gates[:], gates[:], AF.Sigmoid), reads=["gates"], writes=["gates"])
                    for hb in range(2):
                        src = self.B[1 + hb][:, :]
                        sk = "B%d" % (1 + hb)
                        P.op("act", lambda e: e.activation(sq[:, 0:512], src, AF.Square), reads=[sk], writes=["sq"])
                        P.op("dve", lambda e: e.tensor_reduce(rs[:, 0:8], sq[:, 0:512].rearrange("p (h d) -> p h d", d=64), AX.X, ALU.add), reads=["sq"], writes=[("rs", 0)])
                        P.op("dve", lambda e: e.tensor_scalar(rs[:, 16:24], rs[:, 0:8], 1.0 / 64, 1e-6, ALU.mult, ALU.add), reads=[("rs", 0)], writes=[("rs", 1)])
                        P.op("act", lambda e: e.activation(rs[:, 32:40], rs[:, 16:24], AF.Sqrt), reads=[("rs", 1)], writes=[("rs", 2)])
                        P.op("dve", lambda e: e.reciprocal(rs[:, 48:56], rs[:, 32:40]), reads=[("rs", 2)], writes=[("rs", 3)])
                        P.op("dve", lambda e: e.tensor_tensor(sq[:, 0:512].rearrange("p (h d) -> p h d", d=64), src.rearrange("p (h d) -> p h d", d=64),
                                                              rs[:, 48:56].unsqueeze(2).broadcast_to([128, 8, 64]), ALU.mult),
                             reads=[sk, ("rs", 3), "sq"], writes=["sq"])
                        for gh in range(2):
                            dst = qn[:, hb * 512:(hb + 1) * 512].rearrange("p (j gh d) -> p gh j d", j=4, gh=2)[:, gh, :, :]
                            srcv = sq[:, gh * 256:(gh + 1) * 256].rearrange("p (j d) -> p j d", j=4)
                            P.op("pool", lambda e: e.tensor_tensor(dst, srcv, gbs[:, 0, :].unsqueeze(1).broadcast_to([128, 4, 64]), ALU.mult),
                                 reads=["sq", ("gbs", 0)], writes=[("qn", hb, gh)])
                    b4 = self.bank_bf(4)
                    for c in range(8):
                        self.tr(b4[:, c * 128:(c + 1) * 128], qn[:, c * 128:(c + 1) * 128], self.identb[:], ["qn", "identb"], [("B4", c)])
                    P.op("act", lambda e: e.copy(qT[:], b4[:, 0:1024].rearrange("p (c t) -> p c t", c=8)), reads=["B4"], writes=["qT"])
                    ap = bass.AP(tensor=self.FV.tensor, offset=2048 + 128 * i - 2047, ap=[[16, 127], [4096, 16], [1, 128]])
                    P.dma(bci[:], ap, writes=["bci"])
                    oi3 = self.B[7][:, 0:388].rearrange("p (a b) -> p a b", a=4)
                    for gq in range(4):
                        base = (gq % 2) * 64
                        cq0 = (gq // 2) * 4
                        mms = [(KcT[base:base + 64, gq // 2, 0:127], qT[base:base + 64, cq0:cq0 + 4, :], ["KcT", "qT"]),
                               (self.anti127[0:127, 0:127], bci[:, 4 * gq:4 * gq + 4, :], ["anti127", "bci"])]
                        pv = [(jh * 128, 128, VcX[0:127, gq, :], oi3[:, jh, :], "B7", True, True, ["VcX"]) for jh in range(4)]
                        self.run_attn([dict(nrow=127, width=512, groups=[(0, 512, 4, mms)], pv=pv)])
                        P.op("dve", lambda e: e.tensor_scalar(rd[:, 0:4], oi3[:, :, 64], 1e-30, None, ALU.max), reads=["B7"], writes=[("rd", "den")])
                        P.op("dve", lambda e: e.reciprocal(rd[:, 4:8], rd[:, 0:4]), reads=[("rd", "den")], writes=[("rd", "rden")])
                        gsl = gates[:, 12 * gq:12 * gq + 12].rearrange("p (j b) -> p j b", b=3)
                        P.op("dve", lambda e: e.tensor_tensor(rd[:, 8:12], rd[:, 4:8], gsl[:, :, 0], ALU.mult), reads=[("rd", "rden"), "gates"], writes=[("rd", "gr")])
                        P.op("dve", lambda e: e.tensor_tensor(Of[:, gq * 256:(gq + 1) * 256].rearrange("p (j d) -> p j d", j=4), oi3[:, :, 0:64],
                                                              rd[:, 8:12].unsqueeze(2).broadcast_to([128, 4, 64]), ALU.mult),
                             reads=["B7", ("rd", "gr")], writes=["ntmp"])
                        P.op("dve", lambda e: e.tensor_tensor(impw[:], oi3[:, :, 65:97], rd[:, 4:8].unsqueeze(2).broadcast_to([128, 4, 32]), ALU.mult),
                             reads=["B7", ("rd", "rden")], writes=["impw"])
                        P.op("dve", lambda e: e.tensor_reduce(imp[:, gq, :], impw[:].rearrange("p j m -> p m j"), AX.X, ALU.add), reads=["impw"], writes=[("imp", gq)])
                    P.op("dve", lambda e: e.tensor_tensor(imp[:], imp[:], a1[:, i:i + 1, :].broadcast_to([128, 4, 32]), ALU.mult), reads=["imp", "a1"], writes=["imp"])
                    P.op("dve", lambda e: e.tensor_tensor(imp[:], imp[:], a0[:, i:i + 1, :].broadcast_to([128, 4, 32]), ALU.add), reads=["imp", "a0"], writes=["imp"])
                    for gq in range(4):
                        P.op("dve", lambda e: e.max(rd[:, 12:20], imp[:, gq, :]), reads=["imp"], writes=[("rd", "m8a")])
                        P.op("dve", lambda e: e.match_replace(impw[:, gq, :], rd[:, 12:20], imp[:, gq, :], -1e30), reads=["imp", ("rd", "m8a")], writes=["impw"])
                        P.op("dve", lambda e: e.max(rd[:, 20:28], impw[:, gq, :]), reads=["impw"], writes=[("rd", "m8b")])
                        P.op("dve", lambda e: e.tensor_scalar(rd[:, 28:29], rd[:, 27:28], 0.0, None, ALU.max), reads=[("rd", "m8b")], writes=[("rd", "thr")])
                        P.op("dve", lambda e: e.tensor_scalar(impw[:, gq, :], imp[:, gq, :], rd[:, 28:29], 1.0, ALU.is_ge, ALU.subtract),
                             reads=["imp", ("rd", "thr"), "impw"], writes=["impw"])
                        P.op("dve", lambda e: e.tensor_scalar(nmk[:, gq, :], impw[:, gq, :], BIG, None, ALU.mult), reads=["impw"], writes=[("nmk", gq)])
                    b3 = self.bank_bf(3)
                    for gq in range(4):
                        self.tr(b3[0:32, 512 + gq * 128:512 + (gq + 1) * 128], nmk[:, gq, :], self.identb[:], ["nmk", "identb"], [("B3", gq)])
                    P.op("act", lambda e: e.copy(NMT[:], b3[0:32, 512:1024].rearrange("p (g t) -> p g t", g=4)), reads=["B3"], writes=["NMT"])
                    oi4 = self.B[7][:, 0:260].rearrange("p (a b) -> p a b", a=4)
                    for br, (kTt, knm, Vt, vnm, jlo) in enumerate(((ksT, "ksT", Vs, "Vs", 0), (kwT, "kwT", Vw, "Vw", max(0, i - 4)))):
                        for gq in range(4):
                            base = (gq % 2) * 64
                            cq0 = (gq // 2) * 4
                            tasks = []
                            for j in range(jlo, i + 1):
                                dl = i - j
                                mms = [(kTt[base:base + 64, gq // 2, j * 128:(j + 1) * 128], qT[base:base + 64, cq0:cq0 + 4, :], [knm, "qT"])]
                                if br == 0:
                                    mms.append((ej[:, j, :], NMT[:, gq:gq + 1, :].broadcast_to([32, 4, 128]), ["ej", "NMT"]))
                                if dl == 0:
                                    mms.append((self.antib[:], D0[:, 4 * gq:4 * gq + 4, :], ["antib", "D0"]))
                                elif dl == 1:
                                    mms.append((self.antib[:], D1[:, 4 * gq:4 * gq + 4, :], ["antib", "D1"]))
                                elif dl == 4 and br == 1:
                                    mms.append((self.antib[:], D4[:, 4 * gq:4 * gq + 4, :], ["antib", "D4"]))
                                else:
                                    mms.append((self.ones_row[:], CROW[0:1, 4 * gq:4 * gq + 4, :], ["ones_row", "CROW"]))
                                pv = [(jh * 128, 128, Vt[:, j, gq, :], oi4[:, jh, :], "B7", (j == jlo and jh == 0), j == i, [vnm]) for jh in range(4)]
                                tasks.append(dict(nrow=128, width=512, groups=[(0, 512, 4, mms)], pv=pv))
                            self.run_attn(tasks)
                            gsl = gates[:, 12 * gq:12 * gq + 12].rearrange("p (j b) -> p j b", b=3)
                            P.op("dve", lambda e: e.reciprocal(rd[:, 4:8], oi4[:, :, 64]), reads=["B7"], writes=[("rd", "rden")])
                            P.op("dve", lambda e: e.tensor_tensor(rd[:, 8:12], rd[:, 4:8], gsl[:, :, 1 + br], ALU.mult), reads=[("rd", "rden"), "gates"], writes=[("rd", "gr")])
                            P.op("dve", lambda e: e.tensor_tensor(sq[:, 0:256].rearrange("p (j d) -> p j d", j=4), oi4[:, :, 0:64],
                                                                  rd[:, 8:12].unsqueeze(2).broadcast_to([128, 4, 64]), ALU.mult),
                                 reads=["B7", ("rd", "gr")], writes=["sq"])
                            P.op("pool", lambda e: e.tensor_tensor(Of[:, gq * 256:(gq + 1) * 256], Of[:, gq * 256:(gq + 1) * 256], sq[:, 0:256], ALU.add),
                                 reads=["sq", "ntmp"], writes=["ntmp"])
                    P.op("act", lambda e: e.copy(self.hbf[:], Of[:]), reads=["ntmp"], writes=["hbf"])
                    self.out_proj_residual(i, wout)
                P.barrier()

    def peer_convert_step(self, l, k):
        if not self.do_peer:
            return
        P = self.P
        I = self.I
        for et in (2 * k, 2 * k + 1):
            P.dma(self.UB[et], I["peer_ut"][l, :, et * 512:(et + 1) * 512].rearrange("(c p) e -> p c e", p=128), writes=[("UB", et)], q="conv")
            P.dma(self.VB[et], I["peer_v"][l, et * 512:(et + 1) * 512, :].rearrange("(c p) d -> p c d", p=128), writes=[("VB", et)], q="conv")

    def peer_layer(self, l):
        P = self.P
        I = self.I
        self.load_mods(l, 1)
        with ExitStack() as ph:
            wq = self.T(ph, "wq", [128, 8, D], BF16)
            kin = self.T(ph, "kin", [128, 2, 128], F32)
            kbd = self.T(ph, "kbd", [128, 256], BF16)
            qTp = self.T(ph, "qTp", [128, 8, 128], BF16)
            sc = self.T(ph, "sc", [128, 256], F32)
            scr = self.T(ph, "scr", [128, 256], F32)
            tk = self.T(ph, "tk", [128, 8, 64], F32)
            e01 = self.T(ph, "e01", [128, 8, 256], F32)
            prod = [self.T(ph, "prod%d" % k, [128, 512], F32) for k in range(3)]
            tmp2 = [self.T(ph, "tmp2%d" % k, [128, 512], BF16) for k in range(16)]
            ub = [self.T(ph, "ub%d" % k, [128, 8, 512], BF16) for k in range(2)]
            vb = [self.T(ph, "vb%d" % k, [128, 4, D], BF16) for k in range(2)]
            gA = [self.T(ph, "gA%d" % k, [128, 512], BF16) for k in range(2)]
            G = [self.T(ph, "G%d" % k, [128, 512], BF16) for k in range(2)]
            GT = [self.T(ph, "GT%d" % k, [128, 4, 128], BF16) for k in range(2)]
            P.dma(wq[:], I["peer_w_q"][l].rearrange("(c p) n -> p c n", p=128), writes=["wq"], q="pool")
            P.op("pool", lambda e: e.memset(kin[:], 0.0), writes=["kin"])
            P.dma(kin[:, 0, 0:64], I["peer_keys"][l, 0], reads=["kin"], writes=[("kin", 0)])
            P.dma(kin[:, 1, 64:128], I["peer_keys"][l, 1], reads=["kin"], writes=[("kin", 1)])
            for pq in range(2):
                self.tr(self.B[1][:, pq * 128:(pq + 1) * 128], kin[:, pq, :], self.identf[:], ["kin", "identf"], [("B1", pq)])
            P.op("act", lambda e: e.copy(kbd[:], self.B[1][:, 0:256]), reads=["B1"], writes=["kbd"])
            n_et = 0
            for tb in range(NT):
                self.norm_hT(tb)
                for h in range(8):
                    bk = 1 + h // 4
                    for dc in range(8):
                        self.mm(self.B[bk][:, (h % 4) * 128:(h % 4 + 1) * 128], wq[:, dc, h * 128:(h + 1) * 128], self.hT[:, dc, :], dc == 0, dc == 7,
                                ["wq", "hT"], [("B%d" % bk, h % 4)])
                for bk in (1, 2):
                    P.op("act", lambda e: e.copy(qTp[:, (bk - 1) * 4:(bk - 1) * 4 + 4, :], self.B[bk][:, :].rearrange("p (h t) -> p h t", h=4)),
                         reads=["B%d" % bk], writes=[("qTp", bk)])
                for h in range(8):
                    self.mm(self.B[3][:, (h % 2) * 256:(h % 2 + 1) * 256], qTp[:, h, :], kbd[:], True, True, ["qTp", "kbd"], [("B3", h % 2)])
                    P.op("act", lambda e: e.copy(sc[:], self.B[3][:, (h % 2) * 256:(h % 2 + 1) * 256]), reads=[("B3", h % 2)], writes=["sc"])
                    for pq in range(2):
                        s_ = sc[:, pq * 128:(pq + 1) * 128]
                        o = pq * 16
                        P.op("dve", lambda e: e.max(tk[:, h, o:o + 8], s_), reads=["sc"], writes=[("tk", h, pq)])
                        P.op("dve", lambda e: e.match_replace(scr[:, 0:128], tk[:, h, o:o + 8], s_, -1e30), reads=["sc", ("tk", h, pq)], writes=["scr"])
                        P.op("dve", lambda e: e.max(tk[:, h, o + 8:o + 16], scr[:, 0:128]), reads=["scr"], writes=[("tk", h, pq)])
                    cand = scr[:, 0:256].rearrange("p (a b) -> p a b", a=16)
                    P.op("dve", lambda e: e.tensor_tensor(cand, tk[:, h, 0:16].unsqueeze(2).broadcast_to([128, 16, 16]),
                                                          tk[:, h, 16:32].unsqueeze(1).broadcast_to([128, 16, 16]), ALU.add),
                         reads=[("tk", h)], writes=["scr"])
                    P.op("dve", lambda e: e.max(tk[:, h, 32:40], scr[:, 0:256]), reads=["scr"], writes=[("tk", h, 2)])
                    P.op("dve", lambda e: e.match_replace(scr[:, 0:256], tk[:, h, 32:40], scr[:, 0:256], -1e30), reads=["scr", ("tk", h, 2)], writes=["scr"])
                    P.op("dve", lambda e: e.max(tk[:, h, 40:48], scr[:, 0:256]), reads=["scr"], writes=[("tk", h, 2)])
                    P.op("dve", lambda e: e.tensor_scalar(tk[:, h, 48:49], tk[:, h, 32:33], -1.0, None, ALU.mult), reads=[("tk", h, 2)], writes=[("tk", h, 3)])
                    P.op("act", lambda e: e.activation(scr[:, 0:16], tk[:, h, 32:48], AF.Exp, bias=tk[:, h, 48:49], scale=1.0, accum_out=tk[:, h, 49:50]),
                         reads=[("tk", h, 2), ("tk", h, 3), "scr"], writes=["scr", ("tk", h, 4)])
                    P.op("dve", lambda e: e.reciprocal(tk[:, h, 50:51], tk[:, h, 49:50]), reads=[("tk", h, 4)], writes=[("tk", h, 5)])
                    P.op("dve", lambda e: e.tensor_scalar(tk[:, h, 51:52], tk[:, h, 0:1], -1.0, None, ALU.mult), reads=[("tk", h, 0)], writes=[("tk", h, 6)])
                    P.op("dve", lambda e: e.tensor_scalar(tk[:, h, 52:53], tk[:, h, 16:17], -1.0, None, ALU.mult), reads=[("tk", h, 1)], writes=[("tk", h, 7)])
                    P.op("act", lambda e: e.activation(tk[:, h, 54:55], tk[:, h, 47:48], AF.Exp, bias=tk[:, h, 48:49], scale=1.0),
                         reads=[("tk", h, 2), ("tk", h, 3)], writes=[("tk", h, 8)])
                    P.op("dve", lambda e: e.tensor_scalar(tk[:, h, 54:55], tk[:, h, 54:55], tk[:, h, 50:51], 0.9995, ALU.mult, ALU.mult),
                         reads=[("tk", h, 8), ("tk", h, 5)], writes=[("tk", h, 8)])
                    P.op("act", lambda e: e.activation(e01[:, h, 0:128], sc[:, 0:128], AF.Exp, bias=tk[:, h, 51:52], scale=1.0),
                         reads=["sc", ("tk", h, 6)], writes=[("e01", h, 0)])
                    P.op("dve", lambda e: e.tensor_scalar(e01[:, h, 0:128], e01[:, h, 0:128], tk[:, h, 50:51], None, ALU.mult),
                         reads=[("e01", h, 0), ("tk", h, 5)], writes=[("e01", h, 0)])
                    P.op("act", lambda e: e.activation(e01[:, h, 128:256], sc[:, 128:256], AF.Exp, bias=tk[:, h, 52:53], scale=1.0),
                         reads=["sc", ("tk", h, 7)], writes=[("e01", h, 1)])
                def grid(et):
                    for h in range(8):
                        pr = prod[self._npr % 3]
                        pk = "prod%d" % (self._npr % 3)
                        self._npr += 1
                        slot = (et % 2) * 8 + h
                        t2 = tmp2[slot]
                        tk2 = "tmp2%d" % slot
                        if h < 4:
                            for ii in range(4):
                                P.op("act", lambda e: e.activation(pr[:, ii * 128:(ii + 1) * 128], e01[:, h, 128:256], AF.Identity,
                                                                   scale=e01[:, h, et * 4 + ii:et * 4 + ii + 1]),
                                     reads=[("e01", h)], writes=[(pk, ii)])
                        else:
                            P.op("pool", lambda e: e.tensor_tensor(pr[:].rearrange("p (a b) -> p a b", a=4),
                                                                   e01[:, h, et * 4:(et + 1) * 4].unsqueeze(2).broadcast_to([128, 4, 128]),
                                                                   e01[:, h, 128:256].unsqueeze(1).broadcast_to([128, 4, 128]), ALU.mult),
                                 reads=[("e01", h)], writes=[pk])
                        P.op("dve", lambda e: e.scalar_tensor_tensor(t2[:], pr[:], tk[:, h, 54:55], pr[:], ALU.is_ge, ALU.mult),
                             reads=[pk, ("tk", h, 8)], writes=[tk2])

                def amm(et):
                    k2 = et % 2
                    P.dma(ub[k2][:], self.UB[et], reads=[("UB", et)], writes=["ub%d" % k2])
                    P.dma(vb[k2][:], self.VB[et], reads=[("VB", et)], writes=["vb%d" % k2])
                    ab = 4 + k2
                    for dc in range(8):
                        self.mm(self.B[ab][:, :], self.hT[:, dc, :], ub[k2][:, dc, :], dc == 0, dc == 7, ["hT", "ub%d" % k2], ["B%d" % ab])
                    P.op("act", lambda e: e.activation(gA[k2][:], self.B[ab][:, :], AF.Gelu_apprx_tanh), reads=["B%d" % ab], writes=["gA%d" % k2])
                    wb = 1 + k2
                    for h in range(8):
                        slot = k2 * 8 + h
                        self.mm(self.B[wb][:, :], self.identb[:], tmp2[slot][:], h == 0, h == 7, ["identb", "tmp2%d" % slot], ["B%d" % wb])

                def gmul(et):
                    k2 = et % 2
                    P.op("dve", lambda e: e.tensor_tensor(G[k2][:], gA[k2][:], self.B[1 + k2][:, :], ALU.mult),
                         reads=["gA%d" % k2, "B%d" % (1 + k2)], writes=["G%d" % k2])

                def ymm(et):
                    k2 = et % 2
                    b0 = self.bank_bf(0)
                    for c in range(4):
                        self.tr(b0[:, k2 * 512 + c * 128:k2 * 512 + (c + 1) * 128], G[k2][:, c * 128:(c + 1) * 128], self.identb[:],
                                ["G%d" % k2, "identb"], [("B0", k2 * 4 + c)])
                    P.op("act", lambda e: e.copy(GT[k2][:], b0[:, k2 * 512:(k2 + 1) * 512].rearrange("p (c t) -> p c t", c=4)),
                         reads=[("B0", k2 * 4), ("B0", k2 * 4 + 1), ("B0", k2 * 4 + 2), ("B0", k2 * 4 + 3)], writes=["GT%d" % k2])
                    for c in range(4):
                        for half in range(2):
                            self.mm(self.B[6 + half][:, :], GT[k2][:, c, :], vb[k2][:, c, half * 512:(half + 1) * 512],
                                    et == 0 and c == 0, et == 31 and c == 3, ["GT%d" % k2, "vb%d" % k2], ["B%d" % (6 + half)])

                self._npr = getattr(self, "_npr", 0)
                for k in range(-2, 32):
                    if k >= 0:
                        gmul(k)
                    if k + 2 <= 31:
                        grid(k + 2)
                    if 0 <= k + 1 <= 31:
                        amm(k + 1)
                    if k >= 0:
                        ymm(k)
                for half in range(2):
                    P.op("dve", lambda e: e.tensor_tensor(self.ntmp[:, half * 512:(half + 1) * 512], self.B[6 + half][:, :],
                                                          self.GTB[:, half * 512:(half + 1) * 512], ALU.mult),
                         reads=["B%d" % (6 + half), "GTB"], writes=[("ntmp", half)])
                    P.op("pool", lambda e: e.tensor_tensor(self.X[:, tb, half * 512:(half + 1) * 512], self.X[:, tb, half * 512:(half + 1) * 512],
                                                           self.ntmp[:, half * 512:(half + 1) * 512], ALU.add),
                         reads=[("ntmp", half), ("X", tb)], writes=[("X", tb)])
            P.barrier()


_CACHE = {}


def make_in_maps(inputs):
    consts = host_consts()
    shared = {}
    f = lambda a: np.ascontiguousarray(np.asarray(a, dtype=np.float32))
    shared["ada_w"] = f(inputs["ada_w"])
    shared["ada_b"] = f(inputs["ada_b"]).reshape(1, -1)
    shared["norm_g"] = f(inputs["norm_g"]).reshape(1, -1)
    for k in ("even_w_in", "even_b_f", "even_q_g", "even_k_g", "even_w_out", "odd_w_in", "odd_b_gate", "odd_q_g",
              "odd_cmp_pos", "odd_cmp_w1", "odd_cmp_w2", "odd_w_out", "rel_table", "peer_w_q", "peer_keys", "peer_v"):
        shared[k] = f(inputs[k])
    shared["even_conv_w"] = f(inputs["even_conv_w"]).reshape(2, -1)
    shared["odd_k_g"] = f(inputs["odd_k_g"]).reshape(2, -1)
    shared["peer_ut"] = np.ascontiguousarray(np.transpose(f(inputs["peer_u"]), (0, 2, 1)))
    shared.update(consts)
    x = f(inputs["x"])
    c = f(inputs["c"])
    maps = []
    for b in range(8):
        m = dict(shared)
        m["x"] = x[b]
        m["c"] = c[b:b + 1]
        maps.append(m)
    return maps


def kernel(**inputs):
    if "nc" not in _CACHE:
        _CACHE["nc"] = Builder().build()
    nc = _CACHE["nc"]
    maps = make_in_maps(inputs)
    res = run_bass_kernel_spmd(nc, maps, core_ids=list(range(8)))
    return np.stack([np.asarray(r["y"], dtype=np.float32) for r in res.results], axis=0)
```

```python
import math
from contextlib import ExitStack
import numpy as np
import concourse.bass as bass
import concourse.mybir as mybir
from concourse.bass_utils import run_bass_kernel_spmd

F32 = mybir.dt.float32
BF16 = mybir.dt.bfloat16
AF = mybir.ActivationFunctionType
ALU = mybir.AluOpType
AX = mybir.AxisListType

S = 2048
D = 1024
NT = 16
BIG = 240000.0
SEM_ROT = 15000


def _conflict(a, b):
    n = min(len(a), len(b))
    return a[:n] == b[:n]


class _Eng:
    def __init__(self, P, name, eng, same_wait):
        self.P = P
        self.name = name
        self.eng = eng
        self.same_wait = same_wait
        self.sem = None
        self.count = 0
        self.waited = {}

    def new_sem(self):
        self.sem = self.P.alloc_sem(self.name)
        self.count = 0


class Prog:
    def __init__(self, nc, stack, n_dma_sems=4):
        self.nc = nc
        self.stack = stack
        self.nsem = 0
        self.engs = {}
        for name, eng, sw in (("pe", nc.tensor, False), ("dve", nc.vector, True),
                              ("act", nc.scalar, True), ("pool", nc.gpsimd, True),
                              ("sp", nc.sync, False)):
            e = _Eng(self, name, eng, sw)
            e.new_sem()
            self.engs[name] = e
        self.dq = {}
        for q, eng_name, ns in (("sp", "sp", 4), ("pool", "pool", 2), ("conv", "pool", 4)):
            self.dq[q] = {"sems": [self.alloc_sem("d" + q) for _ in range(ns)],
                          "vals": [0] * ns, "n": 0, "eng": eng_name}
        self.state = {}
        self.out_events = []
        self.ninst = 0

    def alloc_sem(self, name):
        self.nsem += 1
        return self.stack.enter_context(self.nc.semaphore("s%s%d" % (name, self.nsem)))

    def _deps(self, reads, writes):
        deps = []
        for k in reads:
            for k2, st in self.state.get(k[0], {}).items():
                if st[0] is not None and _conflict(k, k2):
                    deps.append(st[0])
        for k in writes:
            for k2, st in self.state.get(k[0], {}).items():
                if _conflict(k, k2):
                    if st[0] is not None:
                        deps.append(st[0])
                    deps.extend(st[1])
        return deps

    def _record(self, ev, reads, writes):
        for k in reads:
            d = self.state.setdefault(k[0], {})
            st = d.setdefault(k, [None, []])
            st[1] = [e for e in st[1] if e[0] is not ev[0]] + [ev]
        for k in writes:
            d = self.state.setdefault(k[0], {})
            for k2 in [k2 for k2 in d if len(k2) > len(k) and k2[:len(k)] == k]:
                del d[k2]
            d[k] = [ev, []]

    def _wait(self, E, deps):
        need = {}
        for (sem, val, owner) in deps:
            if owner is E and not E.same_wait:
                continue
            if E.waited.get(id(sem), 0) >= val:
                continue
            if need.get(id(sem), (None, 0))[1] < val:
                need[id(sem)] = (sem, val)
        for sem, val in need.values():
            E.eng.wait_ge(sem, val)
            E.waited[id(sem)] = val

    @staticmethod
    def _keys(ks):
        return [k if isinstance(k, tuple) else (k,) for k in ks]

    def op(self, engname, fn, reads=(), writes=()):
        E = self.engs[engname]
        reads = self._keys(reads)
        writes = self._keys(writes)
        self._wait(E, self._deps(reads, writes))
        if E.count >= SEM_ROT:
            E.new_sem()
        inst = fn(E.eng)
        inst.then_inc(E.sem, 1)
        E.count += 1
        self.ninst += 1
        ev = (E.sem, E.count, E)
        self._record(ev, reads, writes)
        return ev

    def dma(self, out, in_, reads=(), writes=(), q="sp", is_output=False, **kw):
        Q = self.dq[q]
        E = self.engs[Q["eng"]]
        reads = self._keys(reads)
        writes = self._keys(writes)
        i = Q["n"] % len(Q["sems"])
        Q["n"] += 1
        sem = Q["sems"][i]
        deps = self._deps(reads, writes)
        if Q["vals"][i] > 0:
            deps.append((sem, Q["vals"][i], None))
        self._wait(E, deps)
        inst = E.eng.dma_start(out=out, in_=in_, **kw)
        Q["vals"][i] += 16
        inst.then_inc(sem, 16)
        self.ninst += 1
        ev = (sem, Q["vals"][i], None)
        self._record(ev, reads, writes)
        if is_output:
            self.out_events.append(ev)
        return ev

    def _all_events(self):
        evs = []
        for e in self.engs.values():
            if e.count > 0:
                evs.append((e.sem, e.count, e))
        for Q in self.dq.values():
            for sem, v in zip(Q["sems"], Q["vals"]):
                if v > 0:
                    evs.append((sem, v, None))
        return evs

    def barrier(self):
        evs = self._all_events()
        for E in self.engs.values():
            self._wait(E, [ev for ev in evs if ev[2] is not E])
        self.state = {}

    def finish(self):
        E = self.engs["sp"]
        self._wait(E, self.out_events + [ev for ev in self._all_events() if ev[2] is not E])


def host_consts():
    c = {}
    dist = np.arange(2048)
    nf = np.maximum(dist, 1).astype(np.float32)
    large = 16 + (np.log(nf / np.float32(16)) / np.float32(math.log(8.0)) * np.float32(16)).astype(np.int32)
    large = np.minimum(large, 31)
    bucket = np.where(dist < 16, dist, large)
    ohb = np.zeros((32, 2048), np.float32)
    ohb[bucket, dist] = 1.0
    c["k_ohb"] = ohb
    p = np.arange(128)[:, None, None]
    i = np.arange(16)[None, :, None]
    m = np.arange(32)[None, None, :]
    t = 128 * i + p
    cur = t // 64
    forced = (m == 0) | (m == cur) | (m == cur - 1)
    allowed = (64 * m <= t)
    c["k_a1"] = (allowed & ~forced).astype(np.float32)
    c["k_a0"] = np.where(forced, 1e6, np.where(allowed, 0.0, -1.0)).astype(np.float32)
    mm = np.arange(32)[:, None, None]
    jj = np.arange(16)[None, :, None]
    sp = np.arange(128)[None, None, :]
    c["k_ej"] = (mm == 2 * jj + sp // 64).astype(np.float32)
    starts = (np.arange(127) * 16)[:, None]
    bstart = (np.arange(32) * 64)[None, :]
    c["k_ovl"] = ((starts < bstart + 64) & (starts + 32 > bstart)).astype(np.float32)
    s_ = np.arange(128)[:, None]
    t_ = np.arange(128)[None, :]
    c["k_caus"] = np.where(s_ > t_, -BIG, 0.0).astype(np.float32)
    sel8 = np.zeros((8, 8, 128), np.float32)
    for h in range(8):
        sel8[h, h, :] = 1.0
    c["k_sel8"] = sel8
    shm = np.zeros((128, 4, 128), np.float32)
    shm[:, 0, :] = (s_ == t_ - 1)
    shm[:, 1, :] = (s_ == t_ - 2)
    shm[127, 2, 0] = 1.0
    shm[126, 3, 0] = 1.0
    shm[127, 3, 1] = 1.0
    c["k_shm"] = shm
    return c


CONST_SHAPES = {"k_ohb": [32, 2048], "k_a1": [128, 16, 32], "k_a0": [128, 16, 32], "k_ej": [32, 16, 128],
                "k_ovl": [127, 32], "k_caus": [128, 128], "k_sel8": [8, 8, 128], "k_shm": [128, 4, 128]}

IN_SHAPES = {
    "x": [S, D], "c": [1, D], "ada_w": [4, D, 6 * D], "ada_b": [1, 4 * 6 * D], "norm_g": [1, 4 * 2 * D],
    "even_w_in": [2, D, 3080], "even_b_f": [2, 8], "even_conv_w": [2, 3 * 512], "even_q_g": [2, 64],
    "even_k_g": [2, 64], "even_w_out": [2, D, D], "odd_w_in": [2, D, 2608], "odd_b_gate": [2, 48],
    "odd_q_g": [2, 64], "odd_k_g": [2, 3 * 64], "odd_cmp_pos": [2, 2, 32, 64], "odd_cmp_w1": [2, 2, 2048, 64],
    "odd_cmp_w2": [2, 2, 64, 64], "odd_w_out": [2, D, D], "rel_table": [32, 16], "peer_w_q": [4, D, D],
    "peer_keys": [4, 2, 128, 64], "peer_ut": [4, D, 16384], "peer_v": [4, 16384, D],
}


class Builder:
    def __init__(self, n_layers=4, stop=None, peer=True, snaps=False):
        self.snaps = snaps
        self.snap_names = []
        self.n_layers = n_layers
        self.stop = stop
        self.do_peer = peer
        nc = self.nc = bass.Bass("TRN2", target_bir_lowering=False)
        self.I = {}
        for k, shp in list(IN_SHAPES.items()) + list(CONST_SHAPES.items()):
            self.I[k] = nc.dram_tensor(k, list(shp), F32, kind="ExternalInput").ap()
        self.y_out = nc.dram_tensor("y", [S, D], F32, kind="ExternalOutput").ap()
        self.MODS = nc.dram_tensor("mods_s", [4, 6, D], F32, kind="Internal").ap()
        self.FV = nc.dram_tensor("fv_s", [16, 4096], BF16, kind="Internal").ap()
        self.FW = nc.dram_tensor("fw_s", [16, 4096], BF16, kind="Internal").ap()
        self.UB = nc.dram_tensor("ub_s", [32, 128, 8, 512], BF16, kind="Internal").ap()
        self.VB = nc.dram_tensor("vb_s", [32, 128, 4, 1024], BF16, kind="Internal").ap()

    def T(self, st, name, shape, dt):
        self._tn = getattr(self, "_tn", 0) + 1
        return st.enter_context(self.nc.sbuf_tensor("%s_%d" % (name, self._tn), list(shape), dt))

    def mm(self, out, lhsT, rhs, start, stop, reads, writes, skip=False):
        self.P.op("pe", lambda e: e.matmul(out, lhsT, rhs, start=start, stop=stop, skip_group_check=skip),
                  reads=reads, writes=writes)

    def tr(self, out, in_, ident, reads, writes):
        self.P.op("pe", lambda e: e.transpose(out, in_, ident), reads=reads, writes=writes)

    def bank_bf(self, b):
        return self.B[b][:].bitcast(BF16)

    def build(self):
        nc = self.nc
        with ExitStack() as g:
            P = self.P = Prog(nc, g)
            self.B = [g.enter_context(nc.psum_tensor("B%d" % i, [128, 512], F32)) for i in range(8)]
            self.X = self.T(g, "X", [128, NT, D], F32)
            self.identf = self.T(g, "identf", [128, 128], F32)
            self.identb = self.T(g, "identb", [128, 128], BF16)
            self.antib = self.T(g, "antib", [128, 128], BF16)
            self.anti127 = self.T(g, "anti127", [128, 128], BF16)
            self.ones_row = self.T(g, "ones_row", [1, 128], BF16)
            self.one11 = self.T(g, "one11", [1, 1], F32)
            self.GB = self.T(g, "GB", [128, D], BF16)
            self.SHB = self.T(g, "SHB", [128, D], BF16)
            self.GTB = self.T(g, "GTB", [128, D], BF16)
            self.onec = self.T(g, "onec", [128, 1], F32)
            self.onesf = self.T(g, "onesf", [8, 128], F32)
            self.rd = self.T(g, "rd", [128, 32], F32)
            self.ntmp = self.T(g, "ntmp", [128, D], F32)
            self.hbf = self.T(g, "hbf", [128, D], BF16)
            self.hT = self.T(g, "hT", [128, 8, 128], BF16)
            self.sm = self.T(g, "sm", [128, 64], F32)
            tmpi = self.T(g, "tmpi", [128, 128], F32)

            P.op("pool", lambda e: e.iota(tmpi[:], [[1, 128]], base=0, channel_multiplier=-1,
                                          allow_small_or_imprecise_dtypes=True), writes=["tmpi"])
            P.op("dve", lambda e: e.tensor_scalar(self.identf[:], tmpi[:], 0.0, None, ALU.is_equal), reads=["tmpi"], writes=["identf"])
            P.op("dve", lambda e: e.tensor_scalar(self.identb[:], tmpi[:], 0.0, None, ALU.is_equal), reads=["tmpi"], writes=["identb"])
            P.op("pool", lambda e: e.iota(tmpi[:], [[1, 128]], base=-127, channel_multiplier=1,
                                          allow_small_or_imprecise_dtypes=True), reads=["identb", "identf"], writes=["tmpi"])
            P.op("dve", lambda e: e.tensor_scalar(self.antib[:], tmpi[:], 0.0, None, ALU.is_equal), reads=["tmpi"], writes=["antib"])
            P.op("dve", lambda e: e.tensor_scalar(self.anti127[:], tmpi[:], -1.0, None, ALU.is_equal), reads=["tmpi"], writes=["anti127"])
            P.op("pool", lambda e: e.memset(self.ones_row[:], 1.0), writes=["ones_row"])
            P.op("pool", lambda e: e.memset(self.one11[:], 1.0), writes=["one11"])
            P.op("pool", lambda e: e.memset(self.onec[:], 1.0), writes=["onec"])
            P.op("pool", lambda e: e.memset(self.onesf[:], 1.0), writes=["onesf"])

            for tb in range(NT):
                P.dma(self.X[:, tb, :], self.I["x"][tb * 128:(tb + 1) * 128, :], writes=[("X", tb)])

            self.adaln()
            P.barrier()
            done = False
            tables_ready = False
            for l in range(self.n_layers):
                if l % 2 == 0:
                    self.even_layer(l)
                else:
                    if not tables_ready:
                        self.rel_tables()
                        tables_ready = True
                    self.odd_layer(l)
                self.snapshot("xm_%d" % l)
                if self.stop == "L%dmix" % l:
                    break
                if self.do_peer:
                    self.peer_layer(l)
                    self.snapshot("x_%d" % l)
                if self.stop == "L%d" % l:
                    break
            P.barrier()
            for tb in range(NT):
                P.dma(self.y_out[tb * 128:(tb + 1) * 128, :], self.X[:, tb, :], reads=[("X", tb)], is_output=True)
            P.finish()
        return nc

    def snapshot(self, name):
        if not self.snaps:
            return
        t = self.nc.dram_tensor("snap_" + name, [S, D], F32, kind="ExternalOutput").ap()
        self.snap_names.append(name)
        for tb in range(NT):
            self.P.dma(t[tb * 128:(tb + 1) * 128, :], self.X[:, tb, :], reads=[("X", tb)], is_output=True)

    def adaln(self):
        P = self.P
        I = self.I
        with ExitStack() as st:
            crow = self.T(st, "crow", [1, D], F32)
            srow = self.T(st, "srow", [1, D], F32)
            scol = self.T(st, "scol", [128, 8], F32)
            brow = self.T(st, "brow", [1, 6 * D], F32)
            grow = self.T(st, "grow", [1, 2 * D], F32)
            mrow = self.T(st, "mrow", [1, 6 * D], F32)
            wts = [self.T(st, "adw%d" % k, [128, 8, 512], F32) for k in range(2)]
            P.dma(crow[:], I["c"], writes=["crow"])
            P.op("act", lambda e: e.activation(srow[:], crow[:], AF.Silu), reads=["crow"], writes=["srow"])
            ps = self.B[0]
            for dc in range(8):
                self.mm(ps[:, dc:dc + 1], srow[0:1, dc * 128:(dc + 1) * 128], self.one11[:], True, True,
                        ["srow", "one11"], [("B0", dc)])
            P.op("dve", lambda e: e.tensor_copy(scol[:], ps[:, 0:8]), reads=["B0"], writes=["scol"])
            n = 0
            for l in range(self.n_layers):
                P.dma(brow[:], I["ada_b"][0:1, l * 6144:(l + 1) * 6144], writes=["brow"])
                P.dma(grow[:], I["norm_g"][0:1, l * 2048:(l + 1) * 2048], writes=["grow"])
                for nt in range(12):
                    wt = wts[n % 2]
                    wk = "adw%d" % (n % 2)
                    P.dma(wt[:], I["ada_w"][l, :, nt * 512:(nt + 1) * 512].rearrange("(c p) n -> p c n", p=128), writes=[wk])
                    pb = self.B[1 + n % 2]
                    pk = "B%d" % (1 + n % 2)
                    for dc in range(8):
                        self.mm(pb[0:1, :], scol[:, dc:dc + 1], wt[:, dc, :], dc == 0, dc == 7, ["scol", wk], [pk])
                    P.op("dve", lambda e: e.tensor_tensor(mrow[0:1, nt * 512:(nt + 1) * 512], pb[0:1, :],
                                                          brow[0:1, nt * 512:(nt + 1) * 512], ALU.add),
                         reads=[pk, "brow"], writes=[("mrow", nt)])
                    n += 1
                for k in range(2):
                    sc = mrow[0:1, (3 * k + 1) * D:(3 * k + 2) * D]
                    ng = grow[0:1, k * D:(k + 1) * D]
                    P.op("dve", lambda e: e.scalar_tensor_tensor(sc, sc, 1.0, ng, ALU.add, ALU.mult), reads=["mrow", "grow"], writes=["mrow"])
                P.dma(self.MODS[l].rearrange("k d -> (k d)").unsqueeze(0), mrow[:], reads=["mrow"], writes=[("MODS", l)])

    def load_mods(self, l, k):
        P = self.P
        for j, (t, nm) in zip((1, 0, 2), ((self.GB, "GB"), (self.SHB, "SHB"), (self.GTB, "GTB"))):
            P.dma(t[:], self.MODS[l, 3 * k + j:3 * k + j + 1, :].broadcast_to([128, D]), reads=[("MODS", l)], writes=[nm], q="pool")

    def norm_hT(self, tb):
        P = self.P
        xt = self.X[:, tb, :]
        sm = self.sm
        P.op("act", lambda e: e.activation(self.ntmp[:], xt, AF.Square, accum_out=sm[:, 0:1]), reads=[("X", tb)], writes=["ntmp", ("sm", 0)])
        P.op("dve", lambda e: e.tensor_scalar(sm[:, 1:2], sm[:, 0:1], 1.0 / D, 1e-6, ALU.mult, ALU.add), reads=[("sm", 0)], writes=[("sm", 1)])
        P.op("act", lambda e: e.activation(sm[:, 2:3], sm[:, 1:2], AF.Sqrt), reads=[("sm", 1)], writes=[("sm", 2)])
        P.op("dve", lambda e: e.reciprocal(sm[:, 3:4], sm[:, 2:3]), reads=[("sm", 2)], writes=[("sm", 3)])
        P.op("dve", lambda e: e.scalar_tensor_tensor(self.ntmp[:], xt, sm[:, 3:4], self.GB[:], ALU.mult, ALU.mult),
             reads=[("X", tb), ("sm", 3), "GB"], writes=["ntmp"])
        P.op("pool", lambda e: e.tensor_tensor(self.hbf[:], self.ntmp[:], self.SHB[:], ALU.add), reads=["ntmp", "SHB"], writes=["hbf"])
        bt = self.bank_bf(0)
        for c in range(8):
            self.tr(bt[:, c * 128:(c + 1) * 128], self.hbf[:, c * 128:(c + 1) * 128], self.identb[:], ["hbf", "identb"], [("B0", c)])
        P.op("act", lambda e: e.copy(self.hT[:], bt[:, 0:1024].rearrange("p (c t) -> p c t", c=8)), reads=["B0"], writes=["hT"])

    def proj(self, bank, ncols, w, wkey, c0):
        for dc in range(8):
            self.mm(self.B[bank][:, 0:ncols], self.hT[:, dc, :], w[:, dc, c0:c0 + ncols], dc == 0, dc == 7,
                    ["hT", wkey], ["B%d" % bank])

    def head_rmsnorm(self, src, srckey, nh, gb, gbkey, out_ap, outkey, sq, rs, npart=128):
        P = self.P
        n = nh * 64
        P.op("act", lambda e: e.activation(sq[:, 0:n], src, AF.Square), reads=[srckey], writes=["sq"])
        P.op("dve", lambda e: e.tensor_reduce(rs[:, 0:nh], sq[:, 0:n].rearrange("p (h d) -> p h d", d=64), AX.X, ALU.add), reads=["sq"], writes=[("rs", 0)])
        P.op("dve", lambda e: e.tensor_scalar(rs[:, 16:16 + nh], rs[:, 0:nh], 1.0 / 64, 1e-6, ALU.mult, ALU.add), reads=[("rs", 0)], writes=[("rs", 1)])
        P.op("act", lambda e: e.activation(rs[:, 32:32 + nh], rs[:, 16:16 + nh], AF.Sqrt), reads=[("rs", 1)], writes=[("rs", 2)])
        P.op("dve", lambda e: e.reciprocal(rs[:, 48:48 + nh], rs[:, 32:32 + nh]), reads=[("rs", 2)], writes=[("rs", 3)])
        P.op("dve", lambda e: e.tensor_tensor(sq[:, 0:n].rearrange("p (h d) -> p h d", d=64), src.rearrange("p (h d) -> p h d", d=64),
                                              rs[:, 48:48 + nh].unsqueeze(2).broadcast_to([npart, nh, 64]), ALU.mult),
             reads=[srckey, ("rs", 3), "sq"], writes=["sq"])
        P.op("pool", lambda e: e.tensor_tensor(out_ap, sq[:, 0:n].rearrange("p (h d) -> p h d", d=64),
                                               gb.unsqueeze(1).broadcast_to([npart, nh, 64]), ALU.mult),
             reads=["sq", gbkey], writes=[outkey])

    def run_attn(self, tasks):
        P = self.P

        def emit_qk(n, t):
            bi = 5 + n % 2
            sb = self.B[bi]
            key = "B%d" % bi
            for (c0, w, nsub, mms) in t["groups"]:
                out = sb[0:t["nrow"], c0:c0 + w]
                if nsub > 1:
                    out = out.rearrange("p (a b) -> p a b", a=nsub)
                for k, (lhsT, rhs, rd) in enumerate(mms):
                    self.mm(out, lhsT, rhs, k == 0, k == len(mms) - 1, rd, [key])

        def emit_rest(n, t):
            bi = 5 + n % 2
            sb = self.B[bi]
            key = "B%d" % bi
            pt = self.PT[n % 3]
            pk = ("PT", n % 3)
            nr, wd = t["nrow"], t["width"]
            exps = t.get("exps") or [(0, wd, None, [])]
            for (c0, w, bias, rd) in exps:
                if bias is None:
                    P.op("act", lambda e: e.activation(pt[0:nr, c0:c0 + w], sb[0:nr, c0:c0 + w], AF.Exp, scale=0.125), reads=[key], writes=[pk])
                else:
                    P.op("act", lambda e: e.activation(pt[0:nr, c0:c0 + w], sb[0:nr, c0:c0 + w], AF.Exp, bias=bias, scale=0.125),
                         reads=[key] + rd, writes=[pk])
            for (pc0, pw, rhs, out, outkey, start, stop, rd) in t["pv"]:
                self.mm(out, pt[0:nr, pc0:pc0 + pw], rhs, start, stop, [pk] + rd, [outkey], skip=True)

        if not tasks:
            return
        emit_qk(0, tasks[0])
        for n, t in enumerate(tasks):
            if n + 1 < len(tasks):
                emit_qk(n + 1, tasks[n + 1])
            emit_rest(n, t)

    def out_proj_residual(self, i, wout):
        P = self.P
        Obf = self.hbf
        bt = self.bank_bf(0)
        for c in range(8):
            self.tr(bt[:, c * 128:(c + 1) * 128], Obf[:, c * 128:(c + 1) * 128], self.identb[:], ["hbf", "identb"], [("B0", c)])
        P.op("act", lambda e: e.copy(self.OT[:], bt[:, 0:1024].rearrange("p (c t) -> p c t", c=8)), reads=["B0"], writes=["OT"])
        for half in range(2):
            bk = 3 + half
            for fc in range(8):
                self.mm(self.B[bk][:, :], self.OT[:, fc, :], wout[:, fc, half * 512:(half + 1) * 512], fc == 0, fc == 7,
                        ["OT", "wout"], ["B%d" % bk])
            P.op("dve", lambda e: e.tensor_tensor(self.ntmp[:, half * 512:(half + 1) * 512], self.B[bk][:, :],
                                                  self.GTB[:, half * 512:(half + 1) * 512], ALU.mult),
                 reads=["B%d" % bk, "GTB"], writes=[("ntmp", half)])
            P.op("pool", lambda e: e.tensor_tensor(self.X[:, i, half * 512:(half + 1) * 512], self.X[:, i, half * 512:(half + 1) * 512],
                                                   self.ntmp[:, half * 512:(half + 1) * 512], ALU.add),
                 reads=[("ntmp", half), ("X", i)], writes=[("X", i)])

    def even_layer(self, l):
        P = self.P
        I = self.I
        li = l // 2
        w_in = I["even_w_in"][li]
        self.load_mods(l, 0)
        with ExitStack() as lay:
            kT = self.T(lay, "kT", [128, 4, S], BF16)
            Vp = self.T(lay, "Vp", [128, NT, 8, 65], BF16)
            cTT = self.T(lay, "cTT", [128, NT, 8], F32)
            rbc = self.T(lay, "rbc", [128, NT, 8], F32)
            bcol = self.T(lay, "bcol", [128, 8, NT], F32)
            kgb = self.T(lay, "kgb", [128, 64], F32)
            qgb = self.T(lay, "qgb", [128, 64], F32)
            sq = self.T(lay, "sq", [128, 1024], F32)
            rs = self.T(lay, "rs", [128, 64], F32)
            kn = self.T(lay, "kn", [128, 512], BF16)
            qT = self.T(lay, "qT", [128, 4, 128], BF16)
            self.OT = self.T(lay, "OT", [128, 8, 128], BF16)
            self.PT = [self.T(lay, "PT%d" % k, [128, 512], BF16) for k in range(3)]
            P.dma(kgb[:], I["even_k_g"][li:li + 1, :].broadcast_to([128, 64]), writes=["kgb"])
            P.dma(qgb[:], I["even_q_g"][li:li + 1, :].broadcast_to([128, 64]), writes=["qgb"])
            P.op("pool", lambda e: e.memset(Vp[:], 1.0), writes=["Vp"])
            with ExitStack() as ph:
                wkv = self.T(ph, "wkv", [128, 8, 1032], BF16)
                fT = self.T(ph, "fT", [8, S], F32)
                cT = self.T(ph, "cT", [8, S], F32)
                rsel = self.T(ph, "rsel", [8, NT, 8], F32)
                fcol = self.T(ph, "fcol", [128, 8], F32)
                negb = self.T(ph, "negb", [8, 1], F32)
                P.dma(wkv[:], w_in[:, 2048:3080].rearrange("(c p) n -> p c n", p=128), writes=["wkv"], q="pool")
                P.dma(negb[:], I["even_b_f"][li].rearrange("(h o) -> h o", o=1), writes=["negb"])
                P.op("dve", lambda e: e.tensor_scalar(negb[:], negb[:], -1.0, None, ALU.mult), reads=["negb"], writes=["negb"])
                for tb in range(NT):
                    self.peer_convert_step(l, tb)
                    self.norm_hT(tb)
                    self.proj(1, 512, wkv, "wkv", 0)
                    self.proj(2, 512, wkv, "wkv", 512)
                    self.proj(3, 8, wkv, "wkv", 1024)
                    self.head_rmsnorm(self.B[1][:, :], "B1", 8, kgb[:], "kgb", kn[:].rearrange("p (h d) -> p h d", d=64), "kn", sq, rs)
                    b4 = self.bank_bf(4)
                    for c in range(4):
                        self.tr(b4[:, c * 128:(c + 1) * 128], kn[:, c * 128:(c + 1) * 128], self.identb[:], ["kn", "identb"], [("B4", c)])
                    P.op("act", lambda e: e.copy(kT[:, :, tb * 128:(tb + 1) * 128], b4[:, 0:512].rearrange("p (c t) -> p c t", c=4)),
                         reads=["B4"], writes=[("kT", tb)])
                    P.op("act", lambda e: e.copy(Vp[:, tb, :, 0:64], self.B[2][:, :].rearrange("p (h d) -> p h d", d=64)),
                         reads=["B2"], writes=[("Vp", tb)])
                    P.op("dve", lambda e: e.tensor_copy(fcol[:], self.B[3][:, 0:8]), reads=["B3"], writes=["fcol"])
                    self.tr(self.B[7][0:8, 0:128], fcol[:], self.identf[:], ["fcol", "identf"], ["B7"])
                    P.op("act", lambda e: e.copy(fT[:, tb * 128:(tb + 1) * 128], self.B[7][0:8, 0:128]), reads=["B7"], writes=[("fT", tb)])
                P.op("act", lambda e: e.activation(fT[:], fT[:], AF.Exp, bias=negb[:], scale=-1.0), reads=["fT", "negb"], writes=["fT"])
                P.op("act", lambda e: e.activation(fT[:], fT[:], AF.Ln, bias=1.0, scale=1.0), reads=["fT"], writes=["fT"])
                P.op("dve", lambda e: e.tensor_scalar(fT[:], fT[:], -1.0, None, ALU.mult), reads=["fT"], writes=["fT"])
                P.op("dve", lambda e: e.tensor_tensor_scan(cT[:], self.onec[0:8, 0:1].broadcast_to([8, S]), fT[:], 0.0, ALU.mult, ALU.add),
                     reads=["onec", "fT"], writes=["cT"])
                for j in range(NT):
                    self.tr(self.B[1][:, j * 8:(j + 1) * 8], cT[:, j * 128:(j + 1) * 128], self.identf[0:8, 0:8], ["cT", "identf"], [("B1", j)])
                P.op("dve", lambda e: e.tensor_copy(cTT[:].rearrange("p j h -> p (j h)"), self.B[1][:, 0:128]), reads=["B1"], writes=["cTT"])
                P.op("dve", lambda e: e.tensor_tensor(rsel[:], cT[:, 64:64 + 128 * 15 + 1:128].unsqueeze(2).broadcast_to([8, NT, 8]),
                                                      self.identf[0:8, 0:8].unsqueeze(1).broadcast_to([8, NT, 8]), ALU.mult),
                     reads=["cT", "identf"], writes=["rsel"])
                self.mm(self.B[2][:, 0:128], self.onesf[:], rsel[:].rearrange("p i h -> p (i h)"), True, True, ["onesf", "rsel"], ["B2"])
                P.op("dve", lambda e: e.tensor_copy(rbc[:].rearrange("p i h -> p (i h)"), self.B[2][:, 0:128]), reads=["B2"], writes=["rbc"])
                P.barrier()
            with ExitStack() as ph:
                wq2 = self.T(ph, "wq2", [128, 8, 2048], BF16)
                wout = self.T(ph, "wout", [128, 8, D], BF16)
                cwb = self.T(ph, "cwb", [128, 3, 512], BF16)
                shm = self.T(ph, "shm", [128, 4, 128], BF16)
                caus = self.T(ph, "caus", [128, 128], BF16)
                uw = [self.T(ph, "uw%d" % k, [128, 3, 512], BF16) for k in range(2)]
                ccs = sq[:, 0:512]
                cbs = sq[:, 512:1024]
                ucur = self.ntmp[:, 0:512]
                Obf = self.hbf
                P.dma(wq2[:], w_in[:, 0:2048].rearrange("(c p) n -> p c n", p=128), writes=["wq2"], q="pool")
                P.dma(wout[:], I["even_w_out"][li].rearrange("(c p) n -> p c n", p=128), writes=["wout"], q="pool")
                P.dma(cwb[:].rearrange("p k c -> p (k c)"), I["even_conv_w"][li:li + 1, :].broadcast_to([128, 1536]), writes=["cwb"], q="pool")
                P.dma(shm[:], I["k_shm"], writes=["shm"], q="pool")
                P.dma(caus[:], I["k_caus"], writes=["caus"], q="pool")
                for i in range(NT):
                    self.norm_hT(i)
                    for k in range(4):
                        self.proj(1 + k, 512, wq2, "wq2", 512 * k)
                    P.op("act", lambda e: e.copy(ccs, self.B[2][:, :]), reads=["B2"], writes=["sq"])
                    P.op("act", lambda e: e.copy(cbs, self.B[1][:, :]), reads=["B1"], writes=["sq"])
                    P.op("dve", lambda e: e.tensor_tensor(ucur, ccs, self.B[3][:, :], ALU.mult), reads=["sq", "B3"], writes=["ntmp"])
                    uwc, uwp = uw[i % 2], uw[(i + 1) % 2]
                    kc_, kp_ = "uw%d" % (i % 2), "uw%d" % ((i + 1) % 2)
                    P.op("pool", lambda e: e.tensor_tensor(uwc[:], ucur.unsqueeze(1).broadcast_to([128, 3, 512]), cwb[:], ALU.mult),
                         reads=["ntmp", "cwb"], writes=[kc_])
                    mms = [(self.identb[:], uwc[:, 2, :], [kc_]), (shm[:, 0, :], uwc[:, 1, :], [kc_, "shm"]), (shm[:, 1, :], uwc[:, 0, :], [kc_, "shm"])]
                    if i > 0:
                        mms += [(shm[:, 2, :], uwp[:, 1, :], [kp_, "shm"]), (shm[:, 3, :], uwp[:, 0, :], [kp_, "shm"])]
                    for k, (lt, rh, rd) in enumerate(mms):
                        self.mm(self.B[2][:, :], lt, rh, k == 0, k == len(mms) - 1, rd + ["identb"], ["B2"])
                    P.op("dve", lambda e: e.tensor_tensor(Obf[:, 0:512], cbs, self.B[2][:, :], ALU.mult), reads=["sq", "B2"], writes=["hbf"])
                    self.head_rmsnorm(self.B[4][:, :], "B4", 8, qgb[:], "qgb", kn[:].rearrange("p (h d) -> p h d", d=64), "kn", sq, rs)
                    b1 = self.bank_bf(1)
                    for c in range(4):
                        self.tr(b1[:, c * 128:(c + 1) * 128], kn[:, c * 128:(c + 1) * 128], self.identb[:], ["kn", "identb"], [("B1", c)])
                    P.op("act", lambda e: e.copy(qT[:], b1[:, 0:512].rearrange("p (c t) -> p c t", c=4)), reads=["B1"], writes=["qT"])
                    for h in range(8):
                        base = (h % 2) * 64
                        pr = h // 2
                        P.op("dve", lambda e: e.tensor_scalar(bcol[:, h, 0:i + 1], cTT[:, 0:i + 1, h], rbc[:, i, h:h + 1], -1.0, ALU.subtract, ALU.mult),
                             reads=["cTT", "rbc"], writes=[("bcol", h)])
                        tasks = []
                        oi = self.B[7][:, (h % 4) * 65:(h % 4) * 65 + 65]
                        oik = ("B7", h % 4)
                        for j0 in range(0, i + 1, 4):
                            js = list(range(j0, min(j0 + 4, i + 1)))
                            groups = []
                            exps = []
                            pv = []
                            for jj, j in enumerate(js):
                                mm_ = [(kT[base:base + 64, pr, j * 128:(j + 1) * 128], qT[base:base + 64, pr, :], ["kT", "qT"])]
                                if j == i:
                                    mm_.append((self.identb[:], caus[:], ["identb", "caus"]))
                                groups.append((jj * 128, 128, 1, mm_))
                                exps.append((jj * 128, 128, bcol[:, h, j:j + 1], [("bcol", h)]))
                                pv.append((jj * 128, 128, Vp[:, j, h, :], oi, oik, j == 0, j == i, ["Vp"]))
                            tasks.append(dict(nrow=128, width=len(js) * 128, groups=groups, exps=exps, pv=pv))
                        self.run_attn(tasks)
                        P.op("dve", lambda e: e.reciprocal(self.rd[:, h:h + 1], oi[:, 64:65]), reads=[oik], writes=[("rd", h)])
                        P.op("dve", lambda e: e.tensor_scalar(Obf[:, 512 + h * 64:512 + (h + 1) * 64], oi[:, 0:64], self.rd[:, h:h + 1], None, ALU.mult),
                             reads=[oik, ("rd", h)], writes=["hbf"])
                    self.out_proj_residual(i, wout)
                P.barrier()

    def rel_tables(self):
        P = self.P
        I = self.I
        with ExitStack() as st:
            tab = self.T(st, "tab", [32, 16], F32)
            ohb = self.T(st, "ohb", [32, 2048], F32)
            fvr = self.T(st, "fvr", [16, 4096], BF16)
            P.dma(tab[:], I["rel_table"], writes=["tab"])
            P.dma(ohb[:], I["k_ohb"], writes=["ohb"])
            P.op("pool", lambda e: e.memset(fvr[:], -BIG), writes=["fvr"])
            for q in range(4):
                self.mm(self.B[1][0:16, :], tab[:], ohb[:, q * 512:(q + 1) * 512], True, True, ["tab", "ohb"], ["B1"])
                P.op("dve", lambda e: e.tensor_scalar(fvr[:, 2048 + q * 512:2048 + (q + 1) * 512], self.B[1][0:16, :], 8.0, None, ALU.mult),
                     reads=["B1"], writes=["fvr"])
            P.dma(self.FV, fvr[:], reads=["fvr"], writes=["FV"])
            P.op("pool", lambda e: e.memset(fvr[:, 2048 + 512:4096], -BIG), reads=["fvr"], writes=["fvr"])
            P.dma(self.FW, fvr[:], reads=["fvr"], writes=["FW"])
            P.barrier()

    def odd_layer(self, l):
        P = self.P
        I = self.I
        li = l // 2
        w_in = I["odd_w_in"][li]
        rd = self.rd
        self.load_mods(l, 0)
        with ExitStack() as lay:
            ksT = self.T(lay, "ksT", [128, 2, S], BF16)
            kwT = self.T(lay, "kwT", [128, 2, S], BF16)
            Vs = self.T(lay, "Vs", [128, NT, 4, 65], BF16)
            Vw = self.T(lay, "Vw", [128, NT, 4, 65], BF16)
            KcT = self.T(lay, "KcT", [128, 2, 128], BF16)
            VcX = self.T(lay, "VcX", [128, 4, 97], BF16)
            gbs = self.T(lay, "gbs", [128, 4, 64], F32)
            sq = self.T(lay, "sq", [128, 1024], F32)
            rs = self.T(lay, "rs", [128, 64], F32)
            D0 = self.T(lay, "D0", [128, 16, 128], BF16)
            D1 = self.T(lay, "D1", [128, 16, 128], BF16)
            D4 = self.T(lay, "D4", [128, 16, 128], BF16)
            CROW = self.T(lay, "CROW", [1, 16, 128], BF16)
            for (t, nm, src, k) in ((D0, "D0", self.FV, 0), (D1, "D1", self.FV, 1), (D4, "D4", self.FW, 4)):
                ap = bass.AP(tensor=src.tensor, offset=2048 + 128 * k - 127, ap=[[1, 128], [4096, 16], [1, 128]])
                P.dma(t[:], ap, writes=[nm])
            ap = bass.AP(tensor=self.FV.tensor, offset=2048 + 1000, ap=[[0, 1], [4096, 16], [1, 128]])
            P.dma(CROW[:], ap, writes=["CROW"])
            P.dma(gbs[:, 0, :], I["odd_q_g"][li:li + 1, :].broadcast_to([128, 64]), writes=[("gbs", 0)])
            P.dma(gbs[:, 1:4, :].rearrange("p k d -> p (k d)"), I["odd_k_g"][li:li + 1, :].broadcast_to([128, 192]), writes=[("gbs", 1)])
            P.op("pool", lambda e: e.memset(Vs[:], 1.0), writes=["Vs"])
            P.op("pool", lambda e: e.memset(Vw[:], 1.0), writes=["Vw"])
            P.op("pool", lambda e: e.memset(VcX[:], 1.0), writes=["VcX"])
            with ExitStack() as ph:
                wkv = self.T(ph, "wkv", [128, 8, 1536], BF16)
                kcT = self.T(ph, "kcT", [128, 2, S], BF16)
                vcT = self.T(ph, "vcT", [128, 2, S], BF16)
                kvb = self.T(ph, "kvb", [128, 4, 256], BF16)
                w1b = [self.T(ph, "w1b%d" % a, [128, 32, 64], BF16) for a in range(2)]
                w2b = [self.T(ph, "w2b%d" % a, [64, 64], BF16) for a in range(2)]
                pos = self.T(ph, "pos", [32, 2, 64], F32)
                posT = self.T(ph, "posT", [64, 2, 32], BF16)
                cst = self.T(ph, "cst", [64, 2], F32)
                HT = self.T(ph, "HT", [64, 128], BF16)
                KcN = self.T(ph, "KcN", [128, 4, 64], BF16)
                ovl = self.T(ph, "ovl", [127, 32], F32)
                P.dma(wkv[:], w_in[:, 1024:2560].rearrange("(c p) n -> p c n", p=128), writes=["wkv"], q="pool")
                for a in range(2):
                    for hf in range(2):
                        P.dma(w1b[a][hf * 64:(hf + 1) * 64, :, :], I["odd_cmp_w1"][li, a].rearrange("(l d) o -> d l o", d=64),
                              writes=[("w1b%d" % a, hf)], q="pool")
                    P.dma(w2b[a][:], I["odd_cmp_w2"][li, a], writes=["w2b%d" % a], q="pool")
                    P.dma(pos[:, a, :], I["odd_cmp_pos"][li, a], writes=[("pos", a)])
                P.dma(ovl[:], I["k_ovl"], writes=["ovl"])
                P.op("dve", lambda e: e.tensor_copy(VcX[0:127, :, 65:97], ovl[:].unsqueeze(1).broadcast_to([127, 4, 32])),
                     reads=["ovl", "VcX"], writes=["VcX"])
                for tb in range(NT):
                    self.peer_convert_step(l, tb)
                    self.norm_hT(tb)
                    self.proj(1, 512, wkv, "wkv", 0)
                    self.proj(2, 512, wkv, "wkv", 512)
                    self.proj(3, 512, wkv, "wkv", 1024)
                    P.op("act", lambda e: e.copy(kvb[:, 0:2, :], self.B[1][:, :].rearrange("p (a n) -> p a n", a=2)), reads=["B1"], writes=[("kvb", 0)])
                    self.head_rmsnorm(self.B[2][:, 0:256], "B2", 4, gbs[:, 2, :], ("gbs", 1), kvb[:, 2, :].rearrange("p (h d) -> p h d", d=64), ("kvb", 2), sq, rs)
                    self.head_rmsnorm(self.B[3][:, 0:256], "B3", 4, gbs[:, 3, :], ("gbs", 1), kvb[:, 3, :].rearrange("p (h d) -> p h d", d=64), ("kvb", 3), sq, rs)
                    P.op("act", lambda e: e.copy(Vs[:, tb, :, 0:64], self.B[2][:, 256:512].rearrange("p (h d) -> p h d", d=64)), reads=["B2"], writes=[("Vs", tb)])
                    P.op("act", lambda e: e.copy(Vw[:, tb, :, 0:64], self.B[3][:, 256:512].rearrange("p (h d) -> p h d", d=64)), reads=["B3"], writes=[("Vw", tb)])
                    b4 = self.bank_bf(4)
                    for a in range(4):
                        for c in range(2):
                            self.tr(b4[:, (a * 2 + c) * 128:(a * 2 + c + 1) * 128], kvb[:, a, c * 128:(c + 1) * 128], self.identb[:],
                                    ["kvb", "identb"], [("B4", a * 2 + c)])
                    for a, (dst, nm) in enumerate(((kcT, "kcT"), (vcT, "vcT"), (ksT, "ksT"), (kwT, "kwT"))):
                        P.op("act", lambda e: e.copy(dst[:, :, tb * 128:(tb + 1) * 128], b4[:, a * 256:(a + 1) * 256].rearrange("p (c t) -> p c t", c=2)),
                             reads=["B4"], writes=[(nm, tb)])
                for a in range(2):
                    self.tr(self.B[1][0:64, 0:32], pos[:, a, :], self.identf[0:32, 0:32], ["pos", "identf"], ["B1"])
                    P.op("act", lambda e: e.copy(posT[:, a, :], self.B[1][0:64, 0:32]), reads=["B1"], writes=[("posT", a)])
                    for lq in range(32):
                        self.mm(self.B[2][0:64, 0:1], w1b[a][0:64, lq, :], posT[:, a, lq:lq + 1], lq == 0, lq == 31, ["w1b%d" % a, ("posT", a)], ["B2"])
                    P.op("dve", lambda e: e.tensor_copy(cst[:, a:a + 1], self.B[2][0:64, 0:1]), reads=["B2"], writes=[("cst", a)])
                for a, srcT in enumerate((kcT, vcT)):
                    for gq in range(4):
                        base = (gq % 2) * 64
                        ch = gq // 2
                        for lq in range(32):
                            rhs = srcT[base:base + 64, ch, lq:lq + 16 * 126 + 1:16]
                            self.mm(self.B[5][0:64, 0:127], w1b[a][base:base + 64, lq, :], rhs, lq == 0, lq == 31,
                                    ["w1b%d" % a, "kcT", "vcT"], ["B5"])
                        P.op("act", lambda e: e.activation(HT[:, 0:127], self.B[5][0:64, 0:127], AF.Gelu_apprx_tanh, bias=cst[:, a:a + 1], scale=1.0),
                             reads=["B5", ("cst", a)], writes=["HT"])
                        self.mm(self.B[6][0:127, 0:64], HT[:, 0:127], w2b[a][:], True, True, ["HT", "w2b%d" % a], ["B6"])
                        if a == 0:
                            self.head_rmsnorm(self.B[6][0:127, 0:64], "B6", 1, gbs[0:127, 1, :], ("gbs", 1), KcN[0:127, gq:gq + 1, :], ("KcN", gq),
                                              sq[0:127], rs[0:127], npart=127)
                        else:
                            P.op("act", lambda e: e.copy(VcX[0:127, gq, 0:64], self.B[6][0:127, 0:64]), reads=["B6"], writes=[("VcX", gq)])
                b4 = self.bank_bf(4)
                for c in range(2):
                    self.tr(b4[:, c * 128:c * 128 + 127], KcN[0:127, 2 * c:2 * c + 2, :].rearrange("p g d -> p (g d)"), self.identb[0:127, 0:127],
                            ["KcN", "identb"], [("B4", c)])
                    P.op("act", lambda e: e.copy(KcT[:, c, 0:127], b4[:, c * 128:c * 128 + 127]), reads=[("B4", c)], writes=[("KcT", c)])
                P.barrier()
            with ExitStack() as ph:
                wq = self.T(ph, "wq", [128, 8, 1072], BF16)
                wout = self.T(ph, "wout", [128, 8, D], BF16)
                bgb = self.T(ph, "bgb", [128, 48], F32)
                gates = self.T(ph, "gates", [128, 48], F32)
                a1 = self.T(ph, "a1", [128, 16, 32], BF16)
                a0 = self.T(ph, "a0", [128, 16, 32], BF16)
                ej = self.T(ph, "ej", [32, 16, 128], BF16)
                qn = self.T(ph, "qn", [128, D], BF16)
                qT = self.T(ph, "qT", [128, 8, 128], BF16)
                Of = self.ntmp
                imp = self.T(ph, "imp", [128, 4, 32], F32)
                impw = self.T(ph, "impw", [128, 4, 32], F32)
                nmk = self.T(ph, "nmk", [128, 4, 32], BF16)
                NMT = self.T(ph, "NMT", [32, 4, 128], BF16)
                bci = self.T(ph, "bci", [127, 16, 128], BF16)
                self.OT = self.T(ph, "OT", [128, 8, 128], BF16)
                self.PT = [self.T(ph, "PT%d" % k, [128, 512], BF16) for k in range(3)]
                P.dma(wq[:, :, 0:1024], w_in[:, 0:1024].rearrange("(c p) n -> p c n", p=128), writes=[("wq", 0)], q="pool")
                P.dma(wq[:, :, 1024:1072], w_in[:, 2560:2608].rearrange("(c p) n -> p c n", p=128), writes=[("wq", 1)], q="pool")
                P.dma(wout[:], I["odd_w_out"][li].rearrange("(c p) n -> p c n", p=128), writes=["wout"], q="pool")
                P.dma(bgb[:], I["odd_b_gate"][li:li + 1, :].broadcast_to([128, 48]), writes=["bgb"])
                P.dma(a1[:], I["k_a1"], writes=["a1"], q="pool")
                P.dma(a0[:], I["k_a0"], writes=["a0"], q="pool")
                P.dma(ej[:], I["k_ej"], writes=["ej"], q="pool")
                for i in range(NT):
                    self.norm_hT(i)
                    self.proj(1, 512, wq, "wq", 0)
                    self.proj(2, 512, wq, "wq", 512)
                    self.proj(3, 48, wq, "wq", 1024)
                    P.op("dve", lambda e: e.tensor_tensor(gates[:], self.B[3][:, 0:48], bgb[:], ALU.add), reads=["B3", "bgb"], writes=["gates"])
                    P.op("act", lambda e: e.activation(gates[:], gates[:], AF.Sigmoid), reads=["gates"], writes=["gates"])
                    for hb in range(2):
                        src = self.B[1 + hb][:, :]
                        sk = "B%d" % (1 + hb)
                        P.op("act", lambda e: e.activation(sq[:, 0:512], src, AF.Square), reads=[sk], writes=["sq"])
                        P.op("dve", lambda e: e.tensor_reduce(rs[:, 0:8], sq[:, 0:512].rearrange("p (h d) -> p h d", d=64), AX.X, ALU.add), reads=["sq"], writes=[("rs", 0)])
                        P.op("dve", lambda e: e.tensor_scalar(rs[:, 16:24], rs[:, 0:8], 1.0 / 64, 1e-6, ALU.mult, ALU.add), reads=[("rs", 0)], writes=[("rs", 1)])
                        P.op("act", lambda e: e.activation(rs[:, 32:40], rs[:, 16:24], AF.Sqrt), reads=[("rs", 1)], writes=[("rs", 2)])
                        P.op("dve", lambda e: e.reciprocal(rs[:, 48:56], rs[:, 32:40]), reads=[("rs", 2)], writes=[("rs", 3)])
                        P.op("dve", lambda e: e.tensor_tensor(sq[:, 0:512].rearrange("p (h d) -> p h d", d=64), src.rearrange("p (h d) -> p h d", d=64),
                                                              rs[:, 48:56].unsqueeze(2).broadcast_to([128, 8, 64]), ALU.mult),
                             reads=[sk, ("rs", 3), "sq"], writes=["sq"])
                        for gh in range(2):
                            dst = qn[:, hb * 512:(hb + 1) * 512].rearrange("p (j gh d) -> p gh j d", j=4, gh=2)[:, gh, :, :]
                            srcv = sq[:, gh * 256:(gh + 1) * 256].rearrange("p (j d) -> p j d", j=4)
                            P.op("pool", lambda e: e.tensor_tensor(dst, srcv, gbs[:, 0, :].unsqueeze(1).broadcast_to([128, 4, 64]), ALU.mult),
                                 reads=["sq", ("gbs", 0)], writes=[("qn", hb, gh)])
                    b4 = self.bank_bf(4)
                    for c in range(8):
                        self.tr(b4[:, c * 128:(c + 1) * 128], qn[:, c * 128:(c + 1) * 128], self.identb[:], ["qn", "identb"], [("B4", c)])
                    P.op("act", lambda e: e.copy(qT[:], b4[:, 0:1024].rearrange("p (c t) -> p c t", c=8)), reads=["B4"], writes=["qT"])
                    ap = bass.AP(tensor=self.FV.tensor, offset=2048 + 128 * i - 2047, ap=[[16, 127], [4096, 16], [1, 128]])
                    P.dma(bci[:], ap, writes=["bci"])
                    oi3 = self.B[7][:, 0:388].rearrange("p (a b) -> p a b", a=4)
                    for gq in range(4):
                        base = (gq % 2) * 64
                        cq0 = (gq // 2) * 4
                        mms = [(KcT[base:base + 64, gq // 2, 0:127], qT[base:base + 64, cq0:cq0 + 4, :], ["KcT", "qT"]),
                               (self.anti127[0:127, 0:127], bci[:, 4 * gq:4 * gq + 4, :], ["anti127", "bci"])]
                        pv = [(jh * 128, 128, VcX[0:127, gq, :], oi3[:, jh, :], "B7", True, True, ["VcX"]) for jh in range(4)]
                        self.run_attn([dict(nrow=127, width=512, groups=[(0, 512, 4, mms)], pv=pv)])
                        P.op("dve", lambda e: e.tensor_scalar(rd[:, 0:4], oi3[:, :, 64], 1e-30, None, ALU.max), reads=["B7"], writes=[("rd", "den")])
                        P.op("dve", lambda e: e.reciprocal(rd[:, 4:8], rd[:, 0:4]), reads=[("rd", "den")], writes=[("rd", "rden")])
                        gsl = gates[:, 12 * gq:12 * gq + 12].rearrange("p (j b) -> p j b", b=3)
                        P.op("dve", lambda e: e.tensor_tensor(rd[:, 8:12], rd[:, 4:8], gsl[:, :, 0], ALU.mult), reads=[("rd", "rden"), "gates"], writes=[("rd", "gr")])
                        P.op("dve", lambda e: e.tensor_tensor(Of[:, gq * 256:(gq + 1) * 256].rearrange("p (j d) -> p j d", j=4), oi3[:, :, 0:64],
                                                              rd[:, 8:12].unsqueeze(2).broadcast_to([128, 4, 64]), ALU.mult),
                             reads=["B7", ("rd", "gr")], writes=["ntmp"])
                        P.op("dve", lambda e: e.tensor_tensor(impw[:], oi3[:, :, 65:97], rd[:, 4:8].unsqueeze(2).broadcast_to([128, 4, 32]), ALU.mult),
                             reads=["B7", ("rd", "rden")], writes=["impw"])
                        P.op("dve", lambda e: e.tensor_reduce(imp[:, gq, :], impw[:].rearrange("p j m -> p m j"), AX.X, ALU.add), reads=["impw"], writes=[("imp", gq)])
                    P.op("dve", lambda e: e.tensor_tensor(imp[:], imp[:], a1[:, i:i + 1, :].broadcast_to([128, 4, 32]), ALU.mult), reads=["imp", "a1"], writes=["imp"])
                    P.op("dve", lambda e: e.tensor_tensor(imp[:], imp[:], a0[:, i:i + 1, :].broadcast_to([128, 4, 32]), ALU.add), reads=["imp", "a0"], writes=["imp"])
                    for gq in range(4):
                        P.op("dve", lambda e: e.max(rd[:, 12:20], imp[:, gq, :]), reads=["imp"], writes=[("rd", "m8a")])
                        P.op("dve", lambda e: e.match_replace(impw[:, gq, :], rd[:, 12:20], imp[:, gq, :], -1e30), reads=["imp", ("rd", "m8a")], writes=["impw"])
                        P.op("dve", lambda e: e.max(rd[:, 20:28], impw[:, gq, :]), reads=["impw"], writes=[("rd", "m8b")])
                        P.op("dve", lambda e: e.tensor_scalar(rd[:, 28:29], rd[:, 27:28], 0.0, None, ALU.max), reads=[("rd", "m8b")], writes=[("rd", "thr")])
                        P.op("dve", lambda e: e.tensor_scalar(impw[:, gq, :], imp[:, gq, :], rd[:, 28:29], 1.0, ALU.is_ge, ALU.subtract),
                             reads=["imp", ("rd", "thr"), "impw"], writes=["impw"])
                        P.op("dve", lambda e: e.tensor_scalar(nmk[:, gq, :], impw[:, gq, :], BIG, None, ALU.mult), reads=["impw"], writes=[("nmk", gq)])
                    b3 = self.bank_bf(3)
                    for gq in range(4):
                        self.tr(b3[0:32, 512 + gq * 128:512 + (gq + 1) * 128], nmk[:, gq, :], self.identb[:], ["nmk", "identb"], [("B3", gq)])
                    P.op("act", lambda e: e.copy(NMT[:], b3[0:32, 512:1024].rearrange("p (g t) -> p g t", g=4)), reads=["B3"], writes=["NMT"])
                    oi4 = self.B[7][:, 0:260].rearrange("p (a b) -> p a b", a=4)
                    for br, (kTt, knm, Vt, vnm, jlo) in enumerate(((ksT, "ksT", Vs, "Vs", 0), (kwT, "kwT", Vw, "Vw", max(0, i - 4)))):
                        for gq in range(4):
                            base = (gq % 2) * 64
                            cq0 = (gq // 2) * 4
                            tasks = []
                            for j in range(jlo, i + 1):
                                dl = i - j
                                mms = [(kTt[base:base + 64, gq // 2, j * 128:(j + 1) * 128], qT[base:base + 64, cq0:cq0 + 4, :], [knm, "qT"])]
                                if br == 0:
                                    mms.append((ej[:, j, :], NMT[:, gq:gq + 1, :].broadcast_to([32, 4, 128]), ["ej", "NMT"]))
                                if dl == 0:
                                    mms.append((self.antib[:], D0[:, 4 * gq:4 * gq + 4, :], ["antib", "D0"]))
                                elif dl == 1:
                                    mms.append((self.antib[:], D1[:, 4 * gq:4 * gq + 4, :], ["antib", "D1"]))
                                elif dl == 4 and br == 1:
                                    mms.append((self.antib[:], D4[:, 4 * gq:4 * gq + 4, :], ["antib", "D4"]))
                                else:
                                    mms.append((self.ones_row[:], CROW[0:1, 4 * gq:4 * gq + 4, :], ["ones_row", "CROW"]))
                                pv = [(jh * 128, 128, Vt[:, j, gq, :], oi4[:, jh, :], "B7", (j == jlo and jh == 0), j == i, [vnm]) for jh in range(4)]
                                tasks.append(dict(nrow=128, width=512, groups=[(0, 512, 4, mms)], pv=pv))
                            self.run_attn(tasks)
                            gsl = gates[:, 12 * gq:12 * gq + 12].rearrange("p (j b) -> p j b", b=3)
                            P.op("dve", lambda e: e.reciprocal(rd[:, 4:8], oi4[:, :, 64]), reads=["B7"], writes=[("rd", "rden")])
                            P.op("dve", lambda e: e.tensor_tensor(rd[:, 8:12], rd[:, 4:8], gsl[:, :, 1 + br], ALU.mult), reads=[("rd", "rden"), "gates"], writes=[("rd", "gr")])
                            P.op("dve", lambda e: e.tensor_tensor(sq[:, 0:256].rearrange("p (j d) -> p j d", j=4), oi4[:, :, 0:64],
                                                                  rd[:, 8:12].unsqueeze(2).broadcast_to([128, 4, 64]), ALU.mult),
                                 reads=["B7", ("rd", "gr")], writes=["sq"])
                            P.op("pool", lambda e: e.tensor_tensor(Of[:, gq * 256:(gq + 1) * 256], Of[:, gq * 256:(gq + 1) * 256], sq[:, 0:256], ALU.add),
                                 reads=["sq", "ntmp"], writes=["ntmp"])
                    P.op("act", lambda e: e.copy(self.hbf[:], Of[:]), reads=["ntmp"], writes=["hbf"])
                    self.out_proj_residual(i, wout)
                P.barrier()

    def peer_convert_step(self, l, k):
        if not self.do_peer:
            return
        P = self.P
        I = self.I
        for et in (2 * k, 2 * k + 1):
            P.dma(self.UB[et], I["peer_ut"][l, :, et * 512:(et + 1) * 512].rearrange("(c p) e -> p c e", p=128), writes=[("UB", et)], q="conv")
            P.dma(self.VB[et], I["peer_v"][l, et * 512:(et + 1) * 512, :].rearrange("(c p) d -> p c d", p=128), writes=[("VB", et)], q="conv")

    def peer_layer(self, l):
        P = self.P
        I = self.I
        self.load_mods(l, 1)
        with ExitStack() as ph:
            wq = self.T(ph, "wq", [128, 8, D], BF16)
            kin = self.T(ph, "kin", [128, 2, 128], F32)
            kbd = self.T(ph, "kbd", [128, 256], BF16)
            qTp = self.T(ph, "qTp", [128, 8, 128], BF16)
            sc = self.T(ph, "sc", [128, 256], F32)
            scr = self.T(ph, "scr", [128, 256], F32)
            tk = self.T(ph, "tk", [128, 8, 64], F32)
            e01 = self.T(ph, "e01", [128, 8, 256], F32)
            prod = [self.T(ph, "prod%d" % k, [128, 512], F32) for k in range(3)]
            tmp2 = [self.T(ph, "tmp2%d" % k, [128, 512], BF16) for k in range(16)]
            ub = [self.T(ph, "ub%d" % k, [128, 8, 512], BF16) for k in range(2)]
            vb = [self.T(ph, "vb%d" % k, [128, 4, D], BF16) for k in range(2)]
            gA = [self.T(ph, "gA%d" % k, [128, 512], BF16) for k in range(2)]
            G = [self.T(ph, "G%d" % k, [128, 512], BF16) for k in range(2)]
            GT = [self.T(ph, "GT%d" % k, [128, 4, 128], BF16) for k in range(2)]
            P.dma(wq[:], I["peer_w_q"][l].rearrange("(c p) n -> p c n", p=128), writes=["wq"], q="pool")
            P.op("pool", lambda e: e.memset(kin[:], 0.0), writes=["kin"])
            P.dma(kin[:, 0, 0:64], I["peer_keys"][l, 0], reads=["kin"], writes=[("kin", 0)])
            P.dma(kin[:, 1, 64:128], I["peer_keys"][l, 1], reads=["kin"], writes=[("kin", 1)])
            for pq in range(2):
                self.tr(self.B[1][:, pq * 128:(pq + 1) * 128], kin[:, pq, :], self.identf[:], ["kin", "identf"], [("B1", pq)])
            P.op("act", lambda e: e.copy(kbd[:], self.B[1][:, 0:256]), reads=["B1"], writes=["kbd"])
            n_et = 0
            for tb in range(NT):
                self.norm_hT(tb)
                for h in range(8):
                    bk = 1 + h // 4
                    for dc in range(8):
                        self.mm(self.B[bk][:, (h % 4) * 128:(h % 4 + 1) * 128], wq[:, dc, h * 128:(h + 1) * 128], self.hT[:, dc, :], dc == 0, dc == 7,
                                ["wq", "hT"], [("B%d" % bk, h % 4)])
                for bk in (1, 2):
                    P.op("act", lambda e: e.copy(qTp[:, (bk - 1) * 4:(bk - 1) * 4 + 4, :], self.B[bk][:, :].rearrange("p (h t) -> p h t", h=4)),
                         reads=["B%d" % bk], writes=[("qTp", bk)])
                for h in range(8):
                    self.mm(self.B[3][:, (h % 2) * 256:(h % 2 + 1) * 256], qTp[:, h, :], kbd[:], True, True, ["qTp", "kbd"], [("B3", h % 2)])
                    P.op("act", lambda e: e.copy(sc[:], self.B[3][:, (h % 2) * 256:(h % 2 + 1) * 256]), reads=[("B3", h % 2)], writes=["sc"])
                    for pq in range(2):
                        s_ = sc[:, pq * 128:(pq + 1) * 128]
                        o = pq * 16
                        P.op("dve", lambda e: e.max(tk[:, h, o:o + 8], s_), reads=["sc"], writes=[("tk", h, pq)])
                        P.op("dve", lambda e: e.match_replace(scr[:, 0:128], tk[:, h, o:o + 8], s_, -1e30), reads=["sc", ("tk", h, pq)], writes=["scr"])
                        P.op("dve", lambda e: e.max(tk[:, h, o + 8:o + 16], scr[:, 0:128]), reads=["scr"], writes=[("tk", h, pq)])
                    cand = scr[:, 0:256].rearrange("p (a b) -> p a b", a=16)
                    P.op("dve", lambda e: e.tensor_tensor(cand, tk[:, h, 0:16].unsqueeze(2).broadcast_to([128, 16, 16]),
                                                          tk[:, h, 16:32].unsqueeze(1).broadcast_to([128, 16, 16]), ALU.add),
                         reads=[("tk", h)], writes=["scr"])
                    P.op("dve", lambda e: e.max(tk[:, h, 32:40], scr[:, 0:256]), reads=["scr"], writes=[("tk", h, 2)])
                    P.op("dve", lambda e: e.match_replace(scr[:, 0:256], tk[:, h, 32:40], scr[:, 0:256], -1e30), reads=["scr", ("tk", h, 2)], writes=["scr"])
                    P.op("dve", lambda e: e.max(tk[:, h, 40:48], scr[:, 0:256]), reads=["scr"], writes=[("tk", h, 2)])
                    P.op("dve", lambda e: e.tensor_scalar(tk[:, h, 48:49], tk[:, h, 32:33], -1.0, None, ALU.mult), reads=[("tk", h, 2)], writes=[("tk", h, 3)])
                    P.op("act", lambda e: e.activation(scr[:, 0:16], tk[:, h, 32:48], AF.Exp, bias=tk[:, h, 48:49], scale=1.0, accum_out=tk[:, h, 49:50]),
                         reads=[("tk", h, 2), ("tk", h, 3), "scr"], writes=["scr", ("tk", h, 4)])
                    P.op("dve", lambda e: e.reciprocal(tk[:, h, 50:51], tk[:, h, 49:50]), reads=[("tk", h, 4)], writes=[("tk", h, 5)])
                    P.op("dve", lambda e: e.tensor_scalar(tk[:, h, 51:52], tk[:, h, 0:1], -1.0, None, ALU.mult), reads=[("tk", h, 0)], writes=[("tk", h, 6)])
                    P.op("dve", lambda e: e.tensor_scalar(tk[:, h, 52:53], tk[:, h, 16:17], -1.0, None, ALU.mult), reads=[("tk", h, 1)], writes=[("tk", h, 7)])
                    P.op("act", lambda e: e.activation(tk[:, h, 54:55], tk[:, h, 47:48], AF.Exp, bias=tk[:, h, 48:49], scale=1.0),
                         reads=[("tk", h, 2), ("tk", h, 3)], writes=[("tk", h, 8)])
                    P.op("dve", lambda e: e.tensor_scalar(tk[:, h, 54:55], tk[:, h, 54:55], tk[:, h, 50:51], 0.9995, ALU.mult, ALU.mult),
                         reads=[("tk", h, 8), ("tk", h, 5)], writes=[("tk", h, 8)])
                    P.op("act", lambda e: e.activation(e01[:, h, 0:128], sc[:, 0:128], AF.Exp, bias=tk[:, h, 51:52], scale=1.0),
                         reads=["sc", ("tk", h, 6)], writes=[("e01", h, 0)])
                    P.op("dve", lambda e: e.tensor_scalar(e01[:, h, 0:128], e01[:, h, 0:128], tk[:, h, 50:51], None, ALU.mult),
                         reads=[("e01", h, 0), ("tk", h, 5)], writes=[("e01", h, 0)])
                    P.op("act", lambda e: e.activation(e01[:, h, 128:256], sc[:, 128:256], AF.Exp, bias=tk[:, h, 52:53], scale=1.0),
                         reads=["sc", ("tk", h, 7)], writes=[("e01", h, 1)])
                def grid(et):
                    for h in range(8):
                        pr = prod[self._npr % 3]
                        pk = "prod%d" % (self._npr % 3)
                        self._npr += 1
                        slot = (et % 2) * 8 + h
                        t2 = tmp2[slot]
                        tk2 = "tmp2%d" % slot
                        if h < 5:
                            for ii in range(4):
                                P.op("act", lambda e: e.activation(pr[:, ii * 128:(ii + 1) * 128], e01[:, h, 128:256], AF.Identity,
                                                                   scale=e01[:, h, et * 4 + ii:et * 4 + ii + 1]),
                                     reads=[("e01", h)], writes=[(pk, ii)])
                        else:
                            P.op("pool", lambda e: e.tensor_tensor(pr[:].rearrange("p (a b) -> p a b", a=4),
                                                                   e01[:, h, et * 4:(et + 1) * 4].unsqueeze(2).broadcast_to([128, 4, 128]),
                                                                   e01[:, h, 128:256].unsqueeze(1).broadcast_to([128, 4, 128]), ALU.mult),
                                 reads=[("e01", h)], writes=[pk])
                        P.op("dve", lambda e: e.scalar_tensor_tensor(t2[:], pr[:], tk[:, h, 54:55], pr[:], ALU.is_ge, ALU.mult),
                             reads=[pk, ("tk", h, 8)], writes=[tk2])

                def amm(et):
                    k2 = et % 2
                    P.dma(ub[k2][:], self.UB[et], reads=[("UB", et)], writes=["ub%d" % k2])
                    P.dma(vb[k2][:], self.VB[et], reads=[("VB", et)], writes=["vb%d" % k2])
                    ab = 4 + k2
                    for dc in range(8):
                        self.mm(self.B[ab][:, :], self.hT[:, dc, :], ub[k2][:, dc, :], dc == 0, dc == 7, ["hT", "ub%d" % k2], ["B%d" % ab])
                    P.op("act", lambda e: e.activation(gA[k2][:], self.B[ab][:, :], AF.Gelu_apprx_tanh), reads=["B%d" % ab], writes=["gA%d" % k2])
                    wb = 1 + k2
                    for h in range(8):
                        slot = k2 * 8 + h
                        self.mm(self.B[wb][:, :], self.identb[:], tmp2[slot][:], h == 0, h == 7, ["identb", "tmp2%d" % slot], ["B%d" % wb])

                def gmul(et):
                    k2 = et % 2
                    P.op("dve", lambda e: e.tensor_tensor(G[k2][:], gA[k2][:], self.B[1 + k2][:, :], ALU.mult),
                         reads=["gA%d" % k2, "B%d" % (1 + k2)], writes=["G%d" % k2])

                def ymm(et):
                    k2 = et % 2
                    b0 = self.bank_bf(0)
                    for c in range(4):
                        self.tr(b0[:, k2 * 512 + c * 128:k2 * 512 + (c + 1) * 128], G[k2][:, c * 128:(c + 1) * 128], self.identb[:],
                                ["G%d" % k2, "identb"], [("B0", k2 * 4 + c)])
                    P.op("act", lambda e: e.copy(GT[k2][:], b0[:, k2 * 512:(k2 + 1) * 512].rearrange("p (c t) -> p c t", c=4)),
                         reads=[("B0", k2 * 4), ("B0", k2 * 4 + 1), ("B0", k2 * 4 + 2), ("B0", k2 * 4 + 3)], writes=["GT%d" % k2])
                    for c in range(4):
                        for half in range(2):
                            self.mm(self.B[6 + half][:, :], GT[k2][:, c, :], vb[k2][:, c, half * 512:(half + 1) * 512],
                                    et == 0 and c == 0, et == 31 and c == 3, ["GT%d" % k2, "vb%d" % k2], ["B%d" % (6 + half)])

                self._npr = getattr(self, "_npr", 0)
                for k in range(-2, 32):
                    if k >= 0:
                        gmul(k)
                    if k + 2 <= 31:
                        grid(k + 2)
                    if 0 <= k + 1 <= 31:
                        amm(k + 1)
                    if k >= 0:
                        ymm(k)
                for half in range(2):
                    P.op("dve", lambda e: e.tensor_tensor(self.ntmp[:, half * 512:(half + 1) * 512], self.B[6 + half][:, :],
                                                          self.GTB[:, half * 512:(half + 1) * 512], ALU.mult),
                         reads=["B%d" % (6 + half), "GTB"], writes=[("ntmp", half)])
                    P.op("pool", lambda e: e.tensor_tensor(self.X[:, tb, half * 512:(half + 1) * 512], self.X[:, tb, half * 512:(half + 1) * 512],
                                                           self.ntmp[:, half * 512:(half + 1) * 512], ALU.add),
                         reads=[("ntmp", half), ("X", tb)], writes=[("X", tb)])
            P.barrier()


_CACHE = {}


def make_in_maps(inputs):
    consts = host_consts()
    shared = {}
    f = lambda a: np.ascontiguousarray(np.asarray(a, dtype=np.float32))
    shared["ada_w"] = f(inputs["ada_w"])
    shared["ada_b"] = f(inputs["ada_b"]).reshape(1, -1)
    shared["norm_g"] = f(inputs["norm_g"]).reshape(1, -1)
    for k in ("even_w_in", "even_b_f", "even_q_g", "even_k_g", "even_w_out", "odd_w_in", "odd_b_gate", "odd_q_g",
              "odd_cmp_pos", "odd_cmp_w1", "odd_cmp_w2", "odd_w_out", "rel_table", "peer_w_q", "peer_keys", "peer_v"):
        shared[k] = f(inputs[k])
    shared["even_conv_w"] = f(inputs["even_conv_w"]).reshape(2, -1)
    shared["odd_k_g"] = f(inputs["odd_k_g"]).reshape(2, -1)
    shared["peer_ut"] = np.ascontiguousarray(np.transpose(f(inputs["peer_u"]), (0, 2, 1)))
    shared.update(consts)
    x = f(inputs["x"])
    c = f(inputs["c"])
    maps = []
    for b in range(8):
        m = dict(shared)
        m["x"] = x[b]
        m["c"] = c[b:b + 1]
        maps.append(m)
    return maps


def kernel(**inputs):
    if "nc" not in _CACHE:
        _CACHE["nc"] = Builder().build()
    nc = _CACHE["nc"]
    maps = make_in_maps(inputs)
    res = run_bass_kernel_spmd(nc, maps, core_ids=list(range(8)))
    return np.stack([np.asarray(r["y"], dtype=np.float32) for r in res.results], axis=0)
```

```python
import math
from contextlib import ExitStack
import numpy as np
import concourse.bass as bass
import concourse.mybir as mybir
from concourse.bass_utils import run_bass_kernel_spmd

F32 = mybir.dt.float32
BF16 = mybir.dt.bfloat16
AF = mybir.ActivationFunctionType
ALU = mybir.AluOpType
AX = mybir.AxisListType

S = 2048
D = 1024
NT = 16
BIG = 240000.0
SEM_ROT = 15000


def _conflict(a, b):
    n = min(len(a), len(b))
    return a[:n] == b[:n]


class _Eng:
    def __init__(self, P, name, eng, same_wait):
        self.P = P
        self.name = name
        self.eng = eng
        self.same_wait = same_wait
        self.sem = None
        self.count = 0
        self.waited = {}

    def new_sem(self):
        self.sem = self.P.alloc_sem(self.name)
        self.count = 0


class Prog:
    def __init__(self, nc, stack, n_dma_sems=4):
        self.nc = nc
        self.stack = stack
        self.nsem = 0
        self.engs = {}
        for name, eng, sw in (("pe", nc.tensor, False), ("dve", nc.vector, True),
                              ("act", nc.scalar, True), ("pool", nc.gpsimd, True),
                              ("sp", nc.sync, False)):
            e = _Eng(self, name, eng, sw)
            e.new_sem()
            self.engs[name] = e
        self.dq = {}
        for q, eng_name, ns in (("sp", "sp", 4), ("pool", "pool", 1), ("conv", "pool", 4)):
            self.dq[q] = {"sems": [self.alloc_sem("d" + q) for _ in range(ns)],
                          "vals": [0] * ns, "n": 0, "eng": eng_name}
        self.state = {}
        self.out_events = []
        self.ninst = 0

    def alloc_sem(self, name):
        self.nsem += 1
        return self.stack.enter_context(self.nc.semaphore("s%s%d" % (name, self.nsem)))

    def _deps(self, reads, writes):
        deps = []
        for k in reads:
            for k2, st in self.state.get(k[0], {}).items():
                if st[0] is not None and _conflict(k, k2):
                    deps.append(st[0])
        for k in writes:
            for k2, st in self.state.get(k[0], {}).items():
                if _conflict(k, k2):
                    if st[0] is not None:
                        deps.append(st[0])
                    deps.extend(st[1])
        return deps

    def _record(self, ev, reads, writes):
        for k in reads:
            d = self.state.setdefault(k[0], {})
            st = d.setdefault(k, [None, []])
            st[1] = [e for e in st[1] if e[0] is not ev[0]] + [ev]
        for k in writes:
            d = self.state.setdefault(k[0], {})
            for k2 in [k2 for k2 in d if len(k2) > len(k) and k2[:len(k)] == k]:
                del d[k2]
            d[k] = [ev, []]

    def _wait(self, E, deps):
        need = {}
        for (sem, val, owner) in deps:
            if owner is E and not E.same_wait:
                continue
            if E.waited.get(id(sem), 0) >= val:
                continue
            if need.get(id(sem), (None, 0))[1] < val:
                need[id(sem)] = (sem, val)
        for sem, val in need.values():
            E.eng.wait_ge(sem, val)
            E.waited[id(sem)] = val

    @staticmethod
    def _keys(ks):
        return [k if isinstance(k, tuple) else (k,) for k in ks]

    def op(self, engname, fn, reads=(), writes=()):
        E = self.engs[engname]
        reads = self._keys(reads)
        writes = self._keys(writes)
        self._wait(E, self._deps(reads, writes))
        if E.count >= SEM_ROT:
            E.new_sem()
        inst = fn(E.eng)
        inst.then_inc(E.sem, 1)
        E.count += 1
        self.ninst += 1
        ev = (E.sem, E.count, E)
        self._record(ev, reads, writes)
        return ev

    def dma(self, out, in_, reads=(), writes=(), q="sp", is_output=False, **kw):
        Q = self.dq[q]
        E = self.engs[Q["eng"]]
        reads = self._keys(reads)
        writes = self._keys(writes)
        i = Q["n"] % len(Q["sems"])
        Q["n"] += 1
        sem = Q["sems"][i]
        deps = self._deps(reads, writes)
        if Q["vals"][i] > 0:
            deps.append((sem, Q["vals"][i], None))
        self._wait(E, deps)
        inst = E.eng.dma_start(out=out, in_=in_, **kw)
        Q["vals"][i] += 16
        inst.then_inc(sem, 16)
        self.ninst += 1
        ev = (sem, Q["vals"][i], None)
        self._record(ev, reads, writes)
        if is_output:
            self.out_events.append(ev)
        return ev

    def _all_events(self):
        evs = []
        for e in self.engs.values():
            if e.count > 0:
                evs.append((e.sem, e.count, e))
        for Q in self.dq.values():
            for sem, v in zip(Q["sems"], Q["vals"]):
                if v > 0:
                    evs.append((sem, v, None))
        return evs

    def barrier(self):
        evs = self._all_events()
        for E in self.engs.values():
            self._wait(E, [ev for ev in evs if ev[2] is not E])
        self.state = {}

    def finish(self):
        E = self.engs["sp"]
        self._wait(E, self.out_events + [ev for ev in self._all_events() if ev[2] is not E])


def host_consts():
    c = {}
    dist = np.arange(2048)
    nf = np.maximum(dist, 1).astype(np.float32)
    large = 16 + (np.log(nf / np.float32(16)) / np.float32(math.log(8.0)) * np.float32(16)).astype(np.int32)
    large = np.minimum(large, 31)
    bucket = np.where(dist < 16, dist, large)
    ohb = np.zeros((32, 2048), np.float32)
    ohb[bucket, dist] = 1.0
    c["k_ohb"] = ohb
    p = np.arange(128)[:, None, None]
    i = np.arange(16)[None, :, None]
    m = np.arange(32)[None, None, :]
    t = 128 * i + p
    cur = t // 64
    forced = (m == 0) | (m == cur) | (m == cur - 1)
    allowed = (64 * m <= t)
    c["k_a1"] = (allowed & ~forced).astype(np.float32)
    c["k_a0"] = np.where(forced, 1e6, np.where(allowed, 0.0, -1.0)).astype(np.float32)
    mm = np.arange(32)[:, None, None]
    jj = np.arange(16)[None, :, None]
    sp = np.arange(128)[None, None, :]
    c["k_ej"] = (mm == 2 * jj + sp // 64).astype(np.float32)
    starts = (np.arange(127) * 16)[:, None]
    bstart = (np.arange(32) * 64)[None, :]
    c["k_ovl"] = ((starts < bstart + 64) & (starts + 32 > bstart)).astype(np.float32)
    s_ = np.arange(128)[:, None]
    t_ = np.arange(128)[None, :]
    c["k_caus"] = np.where(s_ > t_, -BIG, 0.0).astype(np.float32)
    sel8 = np.zeros((8, 8, 128), np.float32)
    for h in range(8):
        sel8[h, h, :] = 1.0
    c["k_sel8"] = sel8
    shm = np.zeros((128, 4, 128), np.float32)
    shm[:, 0, :] = (s_ == t_ - 1)
    shm[:, 1, :] = (s_ == t_ - 2)
    shm[127, 2, 0] = 1.0
    shm[126, 3, 0] = 1.0
    shm[127, 3, 1] = 1.0
    c["k_shm"] = shm
    return c


CONST_SHAPES = {"k_ohb": [32, 2048], "k_a1": [128, 16, 32], "k_a0": [128, 16, 32], "k_ej": [32, 16, 128],
                "k_ovl": [127, 32], "k_caus": [128, 128], "k_sel8": [8, 8, 128], "k_shm": [128, 4, 128]}

IN_SHAPES = {
    "x": [S, D], "c": [1, D], "ada_w": [4, D, 6 * D], "ada_b": [1, 4 * 6 * D], "norm_g": [1, 4 * 2 * D],
    "even_w_in": [2, D, 3080], "even_b_f": [2, 8], "even_conv_w": [2, 3 * 512], "even_q_g": [2, 64],
    "even_k_g": [2, 64], "even_w_out": [2, D, D], "odd_w_in": [2, D, 2608], "odd_b_gate": [2, 48],
    "odd_q_g": [2, 64], "odd_k_g": [2, 3 * 64], "odd_cmp_pos": [2, 2, 32, 64], "odd_cmp_w1": [2, 2, 2048, 64],
    "odd_cmp_w2": [2, 2, 64, 64], "odd_w_out": [2, D, D], "rel_table": [32, 16], "peer_w_q": [4, D, D],
    "peer_keys": [4, 2, 128, 64], "peer_ut": [4, D, 16384], "peer_v": [4, 16384, D],
}


class Builder:
    def __init__(self, n_layers=4, stop=None, peer=True, snaps=False):
        self.snaps = snaps
        self.snap_names = []
        self.n_layers = n_layers
        self.stop = stop
        self.do_peer = peer
        nc = self.nc = bass.Bass("TRN2", target_bir_lowering=False)
        self.I = {}
        for k, shp in list(IN_SHAPES.items()) + list(CONST_SHAPES.items()):
            self.I[k] = nc.dram_tensor(k, list(shp), F32, kind="ExternalInput").ap()
        self.y_out = nc.dram_tensor("y", [S, D], F32, kind="ExternalOutput").ap()
        self.MODS = nc.dram_tensor("mods_s", [4, 6, D], F32, kind="Internal").ap()
        self.FV = nc.dram_tensor("fv_s", [16, 4096], BF16, kind="Internal").ap()
        self.FW = nc.dram_tensor("fw_s", [16, 4096], BF16, kind="Internal").ap()
        self.UB = nc.dram_tensor("ub_s", [32, 128, 8, 512], BF16, kind="Internal").ap()
        self.VB = nc.dram_tensor("vb_s", [32, 128, 4, 1024], BF16, kind="Internal").ap()

    def T(self, st, name, shape, dt):
        self._tn = getattr(self, "_tn", 0) + 1
        return st.enter_context(self.nc.sbuf_tensor("%s_%d" % (name, self._tn), list(shape), dt))

    def mm(self, out, lhsT, rhs, start, stop, reads, writes, skip=False):
        self.P.op("pe", lambda e: e.matmul(out, lhsT, rhs, start=start, stop=stop, skip_group_check=skip),
                  reads=reads, writes=writes)

    def tr(self, out, in_, ident, reads, writes):
        self.P.op("pe", lambda e: e.transpose(out, in_, ident), reads=reads, writes=writes)

    def bank_bf(self, b):
        return self.B[b][:].bitcast(BF16)

    def build(self):
        nc = self.nc
        with ExitStack() as g:
            P = self.P = Prog(nc, g)
            self.B = [g.enter_context(nc.psum_tensor("B%d" % i, [128, 512], F32)) for i in range(8)]
            self.X = self.T(g, "X", [128, NT, D], F32)
            self.identf = self.T(g, "identf", [128, 128], F32)
            self.identb = self.T(g, "identb", [128, 128], BF16)
            self.antib = self.T(g, "antib", [128, 128], BF16)
            self.anti127 = self.T(g, "anti127", [128, 128], BF16)
            self.ones_row = self.T(g, "ones_row", [1, 128], BF16)
            self.one11 = self.T(g, "one11", [1, 1], F32)
            self.GB = self.T(g, "GB", [128, D], BF16)
            self.SHB = self.T(g, "SHB", [128, D], BF16)
            self.GTB = self.T(g, "GTB", [128, D], BF16)
            self.onec = self.T(g, "onec", [128, 1], F32)
            self.onesf = self.T(g, "onesf", [8, 128], F32)
            self.rd = self.T(g, "rd", [128, 32], F32)
            self.ntmp = self.T(g, "ntmp", [128, D], F32)
            self.hbf = self.T(g, "hbf", [128, D], BF16)
            self.hT = self.T(g, "hT", [128, 8, 128], BF16)
            self.sm = self.T(g, "sm", [128, 64], F32)
            tmpi = self.T(g, "tmpi", [128, 128], F32)

            P.op("pool", lambda e: e.iota(tmpi[:], [[1, 128]], base=0, channel_multiplier=-1,
                                          allow_small_or_imprecise_dtypes=True), writes=["tmpi"])
            P.op("dve", lambda e: e.tensor_scalar(self.identf[:], tmpi[:], 0.0, None, ALU.is_equal), reads=["tmpi"], writes=["identf"])
            P.op("dve", lambda e: e.tensor_scalar(self.identb[:], tmpi[:], 0.0, None, ALU.is_equal), reads=["tmpi"], writes=["identb"])
            P.op("pool", lambda e: e.iota(tmpi[:], [[1, 128]], base=-127, channel_multiplier=1,
                                          allow_small_or_imprecise_dtypes=True), reads=["identb", "identf"], writes=["tmpi"])
            P.op("dve", lambda e: e.tensor_scalar(self.antib[:], tmpi[:], 0.0, None, ALU.is_equal), reads=["tmpi"], writes=["antib"])
            P.op("dve", lambda e: e.tensor_scalar(self.anti127[:], tmpi[:], -1.0, None, ALU.is_equal), reads=["tmpi"], writes=["anti127"])
            P.op("pool", lambda e: e.memset(self.ones_row[:], 1.0), writes=["ones_row"])
            P.op("pool", lambda e: e.memset(self.one11[:], 1.0), writes=["one11"])
            P.op("pool", lambda e: e.memset(self.onec[:], 1.0), writes=["onec"])
            P.op("pool", lambda e: e.memset(self.onesf[:], 1.0), writes=["onesf"])

            for tb in range(NT):
                P.dma(self.X[:, tb, :], self.I["x"][tb * 128:(tb + 1) * 128, :], writes=[("X", tb)])

            self.adaln()
            P.barrier()
            done = False
            tables_ready = False
            for l in range(self.n_layers):
                if l % 2 == 0:
                    self.even_layer(l)
                else:
                    if not tables_ready:
                        self.rel_tables()
                        tables_ready = True
                    self.odd_layer(l)
                self.snapshot("xm_%d" % l)
                if self.stop == "L%dmix" % l:
                    break
                if self.do_peer:
                    self.peer_layer(l)
                    self.snapshot("x_%d" % l)
                if self.stop == "L%d" % l:
                    break
            P.barrier()
            for tb in range(NT):
                P.dma(self.y_out[tb * 128:(tb + 1) * 128, :], self.X[:, tb, :], reads=[("X", tb)], is_output=True)
            P.finish()
        return nc

    def snapshot(self, name):
        if not self.snaps:
            return
        t = self.nc.dram_tensor("snap_" + name, [S, D], F32, kind="ExternalOutput").ap()
        self.snap_names.append(name)
        for tb in range(NT):
            self.P.dma(t[tb * 128:(tb + 1) * 128, :], self.X[:, tb, :], reads=[("X", tb)], is_output=True)

    def adaln(self):
        P = self.P
        I = self.I
        with ExitStack() as st:
            crow = self.T(st, "crow", [1, D], F32)
            srow = self.T(st, "srow", [1, D], F32)
            scol = self.T(st, "scol", [128, 8], F32)
            brow = self.T(st, "brow", [1, 6 * D], F32)
            grow = self.T(st, "grow", [1, 2 * D], F32)
            mrow = self.T(st, "mrow", [1, 6 * D], F32)
            wts = [self.T(st, "adw%d" % k, [128, 8, 512], F32) for k in range(2)]
            P.dma(crow[:], I["c"], writes=["crow"])
            P.op("act", lambda e: e.activation(srow[:], crow[:], AF.Silu), reads=["crow"], writes=["srow"])
            ps = self.B[0]
            for dc in range(8):
                self.mm(ps[:, dc:dc + 1], srow[0:1, dc * 128:(dc + 1) * 128], self.one11[:], True, True,
                        ["srow", "one11"], [("B0", dc)])
            P.op("dve", lambda e: e.tensor_copy(scol[:], ps[:, 0:8]), reads=["B0"], writes=["scol"])
            n = 0
            for l in range(self.n_layers):
                P.dma(brow[:], I["ada_b"][0:1, l * 6144:(l + 1) * 6144], writes=["brow"])
                P.dma(grow[:], I["norm_g"][0:1, l * 2048:(l + 1) * 2048], writes=["grow"])
                for nt in range(12):
                    wt = wts[n % 2]
                    wk = "adw%d" % (n % 2)
                    P.dma(wt[:], I["ada_w"][l, :, nt * 512:(nt + 1) * 512].rearrange("(c p) n -> p c n", p=128), writes=[wk])
                    pb = self.B[1 + n % 2]
                    pk = "B%d" % (1 + n % 2)
                    for dc in range(8):
                        self.mm(pb[0:1, :], scol[:, dc:dc + 1], wt[:, dc, :], dc == 0, dc == 7, ["scol", wk], [pk])
                    P.op("dve", lambda e: e.tensor_tensor(mrow[0:1, nt * 512:(nt + 1) * 512], pb[0:1, :],
                                                          brow[0:1, nt * 512:(nt + 1) * 512], ALU.add),
                         reads=[pk, "brow"], writes=[("mrow", nt)])
                    n += 1
                for k in range(2):
                    sc = mrow[0:1, (3 * k + 1) * D:(3 * k + 2) * D]
                    ng = grow[0:1, k * D:(k + 1) * D]
                    P.op("dve", lambda e: e.scalar_tensor_tensor(sc, sc, 1.0, ng, ALU.add, ALU.mult), reads=["mrow", "grow"], writes=["mrow"])
                P.dma(self.MODS[l].rearrange("k d -> (k d)").unsqueeze(0), mrow[:], reads=["mrow"], writes=[("MODS", l)])

    def load_mods(self, l, k):
        P = self.P
        for j, (t, nm) in zip((1, 0, 2), ((self.GB, "GB"), (self.SHB, "SHB"), (self.GTB, "GTB"))):
            P.dma(t[:], self.MODS[l, 3 * k + j:3 * k + j + 1, :].broadcast_to([128, D]), reads=[("MODS", l)], writes=[nm], q="pool")

    def norm_hT(self, tb):
        P = self.P
        xt = self.X[:, tb, :]
        sm = self.sm
        P.op("act", lambda e: e.activation(self.ntmp[:], xt, AF.Square, accum_out=sm[:, 0:1]), reads=[("X", tb)], writes=["ntmp", ("sm", 0)])
        P.op("dve", lambda e: e.tensor_scalar(sm[:, 1:2], sm[:, 0:1], 1.0 / D, 1e-6, ALU.mult, ALU.add), reads=[("sm", 0)], writes=[("sm", 1)])
        P.op("act", lambda e: e.activation(sm[:, 2:3], sm[:, 1:2], AF.Sqrt), reads=[("sm", 1)], writes=[("sm", 2)])
        P.op("dve", lambda e: e.reciprocal(sm[:, 3:4], sm[:, 2:3]), reads=[("sm", 2)], writes=[("sm", 3)])
        P.op("dve", lambda e: e.scalar_tensor_tensor(self.ntmp[:], xt, sm[:, 3:4], self.GB[:], ALU.mult, ALU.mult),
             reads=[("X", tb), ("sm", 3), "GB"], writes=["ntmp"])
        P.op("pool", lambda e: e.tensor_tensor(self.hbf[:], self.ntmp[:], self.SHB[:], ALU.add), reads=["ntmp", "SHB"], writes=["hbf"])
        bt = self.bank_bf(0)
        for c in range(8):
            self.tr(bt[:, c * 128:(c + 1) * 128], self.hbf[:, c * 128:(c + 1) * 128], self.identb[:], ["hbf", "identb"], [("B0", c)])
        P.op("act", lambda e: e.copy(self.hT[:], bt[:, 0:1024].rearrange("p (c t) -> p c t", c=8)), reads=["B0"], writes=["hT"])

    def proj(self, bank, ncols, w, wkey, c0):
        for dc in range(8):
            self.mm(self.B[bank][:, 0:ncols], self.hT[:, dc, :], w[:, dc, c0:c0 + ncols], dc == 0, dc == 7,
                    ["hT", wkey], ["B%d" % bank])

    def head_rmsnorm(self, src, srckey, nh, gb, gbkey, out_ap, outkey, sq, rs, npart=128):
        P = self.P
        n = nh * 64
        P.op("act", lambda e: e.activation(sq[:, 0:n], src, AF.Square), reads=[srckey], writes=["sq"])
        P.op("dve", lambda e: e.tensor_reduce(rs[:, 0:nh], sq[:, 0:n].rearrange("p (h d) -> p h d", d=64), AX.X, ALU.add), reads=["sq"], writes=[("rs", 0)])
        P.op("dve", lambda e: e.tensor_scalar(rs[:, 16:16 + nh], rs[:, 0:nh], 1.0 / 64, 1e-6, ALU.mult, ALU.add), reads=[("rs", 0)], writes=[("rs", 1)])
        P.op("act", lambda e: e.activation(rs[:, 32:32 + nh], rs[:, 16:16 + nh], AF.Sqrt), reads=[("rs", 1)], writes=[("rs", 2)])
        P.op("dve", lambda e: e.reciprocal(rs[:, 48:48 + nh], rs[:, 32:32 + nh]), reads=[("rs", 2)], writes=[("rs", 3)])
        P.op("dve", lambda e: e.tensor_tensor(sq[:, 0:n].rearrange("p (h d) -> p h d", d=64), src.rearrange("p (h d) -> p h d", d=64),
                                              rs[:, 48:48 + nh].unsqueeze(2).broadcast_to([npart, nh, 64]), ALU.mult),
             reads=[srckey, ("rs", 3), "sq"], writes=["sq"])
        P.op("pool", lambda e: e.tensor_tensor(out_ap, sq[:, 0:n].rearrange("p (h d) -> p h d", d=64),
                                               gb.unsqueeze(1).broadcast_to([npart, nh, 64]), ALU.mult),
             reads=["sq", gbkey], writes=[outkey])

    def run_attn(self, tasks):
        P = self.P

        def emit_qk(n, t):
            bi = 5 + n % 2
            sb = self.B[bi]
            key = "B%d" % bi
            for (c0, w, nsub, mms) in t["groups"]:
                out = sb[0:t["nrow"], c0:c0 + w]
                if nsub > 1:
                    out = out.rearrange("p (a b) -> p a b", a=nsub)
                for k, (lhsT, rhs, rd) in enumerate(mms):
                    self.mm(out, lhsT, rhs, k == 0, k == len(mms) - 1, rd, [key])

        def emit_rest(n, t):
            bi = 5 + n % 2
            sb = self.B[bi]
            key = "B%d" % bi
            pt = self.PT[n % 3]
            pk = ("PT", n % 3)
            nr, wd = t["nrow"], t["width"]
            exps = t.get("exps") or [(0, wd, None, [])]
            for (c0, w, bias, rd) in exps:
                if bias is None:
                    P.op("act", lambda e: e.activation(pt[0:nr, c0:c0 + w], sb[0:nr, c0:c0 + w], AF.Exp, scale=0.125), reads=[key], writes=[pk])
                else:
                    P.op("act", lambda e: e.activation(pt[0:nr, c0:c0 + w], sb[0:nr, c0:c0 + w], AF.Exp, bias=bias, scale=0.125),
                         reads=[key] + rd, writes=[pk])
            for (pc0, pw, rhs, out, outkey, start, stop, rd) in t["pv"]:
                self.mm(out, pt[0:nr, pc0:pc0 + pw], rhs, start, stop, [pk] + rd, [outkey], skip=True)

        if not tasks:
            return
        emit_qk(0, tasks[0])
        for n, t in enumerate(tasks):
            if n + 1 < len(tasks):
                emit_qk(n + 1, tasks[n + 1])
            emit_rest(n, t)

    def out_proj_residual(self, i, wout):
        P = self.P
        Obf = self.hbf
        bt = self.bank_bf(0)
        for c in range(8):
            self.tr(bt[:, c * 128:(c + 1) * 128], Obf[:, c * 128:(c + 1) * 128], self.identb[:], ["hbf", "identb"], [("B0", c)])
        P.op("act", lambda e: e.copy(self.OT[:], bt[:, 0:1024].rearrange("p (c t) -> p c t", c=8)), reads=["B0"], writes=["OT"])
        for half in range(2):
            bk = 3 + half
            for fc in range(8):
                self.mm(self.B[bk][:, :], self.OT[:, fc, :], wout[:, fc, half * 512:(half + 1) * 512], fc == 0, fc == 7,
                        ["OT", "wout"], ["B%d" % bk])
            P.op("dve", lambda e: e.tensor_tensor(self.ntmp[:, half * 512:(half + 1) * 512], self.B[bk][:, :],
                                                  self.GTB[:, half * 512:(half + 1) * 512], ALU.mult),
                 reads=["B%d" % bk, "GTB"], writes=[("ntmp", half)])
            P.op("pool", lambda e: e.tensor_tensor(self.X[:, i, half * 512:(half + 1) * 512], self.X[:, i, half * 512:(half + 1) * 512],
                                                   self.ntmp[:, half * 512:(half + 1) * 512], ALU.add),
                 reads=[("ntmp", half), ("X", i)], writes=[("X", i)])

    def even_layer(self, l):
        P = self.P
        I = self.I
        li = l // 2
        w_in = I["even_w_in"][li]
        self.load_mods(l, 0)
        with ExitStack() as lay:
            kT = self.T(lay, "kT", [128, 4, S], BF16)
            Vp = self.T(lay, "Vp", [128, NT, 8, 65], BF16)
            cTT = self.T(lay, "cTT", [128, NT, 8], F32)
            rbc = self.T(lay, "rbc", [128, NT, 8], F32)
            bcol = self.T(lay, "bcol", [128, 8, NT], F32)
            kgb = self.T(lay, "kgb", [128, 64], F32)
            qgb = self.T(lay, "qgb", [128, 64], F32)
            sq = self.T(lay, "sq", [128, 1024], F32)
            rs = self.T(lay, "rs", [128, 64], F32)
            kn = self.T(lay, "kn", [128, 512], BF16)
            qT = self.T(lay, "qT", [128, 4, 128], BF16)
            self.OT = self.T(lay, "OT", [128, 8, 128], BF16)
            self.PT = [self.T(lay, "PT%d" % k, [128, 512], BF16) for k in range(3)]
            P.dma(kgb[:], I["even_k_g"][li:li + 1, :].broadcast_to([128, 64]), writes=["kgb"])
            P.dma(qgb[:], I["even_q_g"][li:li + 1, :].broadcast_to([128, 64]), writes=["qgb"])
            P.op("pool", lambda e: e.memset(Vp[:], 1.0), writes=["Vp"])
            with ExitStack() as ph:
                wkv = self.T(ph, "wkv", [128, 8, 1032], BF16)
                fT = self.T(ph, "fT", [8, S], F32)
                cT = self.T(ph, "cT", [8, S], F32)
                rsel = self.T(ph, "rsel", [8, NT, 8], F32)
                fcol = self.T(ph, "fcol", [128, 8], F32)
                negb = self.T(ph, "negb", [8, 1], F32)
                P.dma(wkv[:], w_in[:, 2048:3080].rearrange("(c p) n -> p c n", p=128), writes=["wkv"], q="pool")
                P.dma(negb[:], I["even_b_f"][li].rearrange("(h o) -> h o", o=1), writes=["negb"])
                P.op("dve", lambda e: e.tensor_scalar(negb[:], negb[:], -1.0, None, ALU.mult), reads=["negb"], writes=["negb"])
                for tb in range(NT):
                    self.peer_convert_step(l, tb)
                    self.norm_hT(tb)
                    self.proj(1, 512, wkv, "wkv", 0)
                    self.proj(2, 512, wkv, "wkv", 512)
                    self.proj(3, 8, wkv, "wkv", 1024)
                    self.head_rmsnorm(self.B[1][:, :], "B1", 8, kgb[:], "kgb", kn[:].rearrange("p (h d) -> p h d", d=64), "kn", sq, rs)
                    b4 = self.bank_bf(4)
                    for c in range(4):
                        self.tr(b4[:, c * 128:(c + 1) * 128], kn[:, c * 128:(c + 1) * 128], self.identb[:], ["kn", "identb"], [("B4", c)])
                    P.op("act", lambda e: e.copy(kT[:, :, tb * 128:(tb + 1) * 128], b4[:, 0:512].rearrange("p (c t) -> p c t", c=4)),
                         reads=["B4"], writes=[("kT", tb)])
                    P.op("act", lambda e: e.copy(Vp[:, tb, :, 0:64], self.B[2][:, :].rearrange("p (h d) -> p h d", d=64)),
                         reads=["B2"], writes=[("Vp", tb)])
                    P.op("dve", lambda e: e.tensor_copy(fcol[:], self.B[3][:, 0:8]), reads=["B3"], writes=["fcol"])
                    self.tr(self.B[7][0:8, 0:128], fcol[:], self.identf[:], ["fcol", "identf"], ["B7"])
                    P.op("act", lambda e: e.copy(fT[:, tb * 128:(tb + 1) * 128], self.B[7][0:8, 0:128]), reads=["B7"], writes=[("fT", tb)])
                P.op("act", lambda e: e.activation(fT[:], fT[:], AF.Exp, bias=negb[:], scale=-1.0), reads=["fT", "negb"], writes=["fT"])
                P.op("act", lambda e: e.activation(fT[:], fT[:], AF.Ln, bias=1.0, scale=1.0), reads=["fT"], writes=["fT"])
                P.op("dve", lambda e: e.tensor_scalar(fT[:], fT[:], -1.0, None, ALU.mult), reads=["fT"], writes=["fT"])
                P.op("dve", lambda e: e.tensor_tensor_scan(cT[:], self.onec[0:8, 0:1].broadcast_to([8, S]), fT[:], 0.0, ALU.mult, ALU.add),
                     reads=["onec", "fT"], writes=["cT"])
                for j in range(NT):
                    self.tr(self.B[1][:, j * 8:(j + 1) * 8], cT[:, j * 128:(j + 1) * 128], self.identf[0:8, 0:8], ["cT", "identf"], [("B1", j)])
                P.op("dve", lambda e: e.tensor_copy(cTT[:].rearrange("p j h -> p (j h)"), self.B[1][:, 0:128]), reads=["B1"], writes=["cTT"])
                P.op("dve", lambda e: e.tensor_tensor(rsel[:], cT[:, 64:64 + 128 * 15 + 1:128].unsqueeze(2).broadcast_to([8, NT, 8]),
                                                      self.identf[0:8, 0:8].unsqueeze(1).broadcast_to([8, NT, 8]), ALU.mult),
                     reads=["cT", "identf"], writes=["rsel"])
                self.mm(self.B[2][:, 0:128], self.onesf[:], rsel[:].rearrange("p i h -> p (i h)"), True, True, ["onesf", "rsel"], ["B2"])
                P.op("dve", lambda e: e.tensor_copy(rbc[:].rearrange("p i h -> p (i h)"), self.B[2][:, 0:128]), reads=["B2"], writes=["rbc"])
                P.barrier()
            with ExitStack() as ph:
                wq2 = self.T(ph, "wq2", [128, 8, 2048], BF16)
                wout = self.T(ph, "wout", [128, 8, D], BF16)
                cwb = self.T(ph, "cwb", [128, 3, 512], BF16)
                shm = self.T(ph, "shm", [128, 4, 128], BF16)
                caus = self.T(ph, "caus", [128, 128], BF16)
                uw = [self.T(ph, "uw%d" % k, [128, 3, 512], BF16) for k in range(2)]
                ccs = sq[:, 0:512]
                cbs = sq[:, 512:1024]
                ucur = self.ntmp[:, 0:512]
                Obf = self.hbf
                P.dma(wq2[:], w_in[:, 0:2048].rearrange("(c p) n -> p c n", p=128), writes=["wq2"], q="pool")
                P.dma(wout[:], I["even_w_out"][li].rearrange("(c p) n -> p c n", p=128), writes=["wout"], q="pool")
                P.dma(cwb[:].rearrange("p k c -> p (k c)"), I["even_conv_w"][li:li + 1, :].broadcast_to([128, 1536]), writes=["cwb"], q="pool")
                P.dma(shm[:], I["k_shm"], writes=["shm"], q="pool")
                P.dma(caus[:], I["k_caus"], writes=["caus"], q="pool")
                for i in range(NT):
                    self.norm_hT(i)
                    for k in range(4):
                        self.proj(1 + k, 512, wq2, "wq2", 512 * k)
                    P.op("act", lambda e: e.copy(ccs, self.B[2][:, :]), reads=["B2"], writes=["sq"])
                    P.op("act", lambda e: e.copy(cbs, self.B[1][:, :]), reads=["B1"], writes=["sq"])
                    P.op("dve", lambda e: e.tensor_tensor(ucur, ccs, self.B[3][:, :], ALU.mult), reads=["sq", "B3"], writes=["ntmp"])
                    uwc, uwp = uw[i % 2], uw[(i + 1) % 2]
                    kc_, kp_ = "uw%d" % (i % 2), "uw%d" % ((i + 1) % 2)
                    P.op("pool", lambda e: e.tensor_tensor(uwc[:], ucur.unsqueeze(1).broadcast_to([128, 3, 512]), cwb[:], ALU.mult),
                         reads=["ntmp", "cwb"], writes=[kc_])
                    mms = [(self.identb[:], uwc[:, 2, :], [kc_]), (shm[:, 0, :], uwc[:, 1, :], [kc_, "shm"]), (shm[:, 1, :], uwc[:, 0, :], [kc_, "shm"])]
                    if i > 0:
                        mms += [(shm[:, 2, :], uwp[:, 1, :], [kp_, "shm"]), (shm[:, 3, :], uwp[:, 0, :], [kp_, "shm"])]
                    for k, (lt, rh, rd) in enumerate(mms):
                        self.mm(self.B[2][:, :], lt, rh, k == 0, k == len(mms) - 1, rd + ["identb"], ["B2"])
                    P.op("dve", lambda e: e.tensor_tensor(Obf[:, 0:512], cbs, self.B[2][:, :], ALU.mult), reads=["sq", "B2"], writes=["hbf"])
                    self.head_rmsnorm(self.B[4][:, :], "B4", 8, qgb[:], "qgb", kn[:].rearrange("p (h d) -> p h d", d=64), "kn", sq, rs)
                    b1 = self.bank_bf(1)
                    for c in range(4):
                        self.tr(b1[:, c * 128:(c + 1) * 128], kn[:, c * 128:(c + 1) * 128], self.identb[:], ["kn", "identb"], [("B1", c)])
                    P.op("act", lambda e: e.copy(qT[:], b1[:, 0:512].rearrange("p (c t) -> p c t", c=4)), reads=["B1"], writes=["qT"])
                    for h in range(8):
                        base = (h % 2) * 64
                        pr = h // 2
                        P.op("dve", lambda e: e.tensor_scalar(bcol[:, h, 0:i + 1], cTT[:, 0:i + 1, h], rbc[:, i, h:h + 1], -1.0, ALU.subtract, ALU.mult),
                             reads=["cTT", "rbc"], writes=[("bcol", h)])
                        tasks = []
                        oi = self.B[7][:, (h % 4) * 65:(h % 4) * 65 + 65]
                        oik = ("B7", h % 4)
                        for j0 in range(0, i + 1, 4):
                            js = list(range(j0, min(j0 + 4, i + 1)))
                            groups = []
                            exps = []
                            pv = []
                            for jj, j in enumerate(js):
                                mm_ = [(kT[base:base + 64, pr, j * 128:(j + 1) * 128], qT[base:base + 64, pr, :], ["kT", "qT"])]
                                if j == i:
                                    mm_.append((self.identb[:], caus[:], ["identb", "caus"]))
                                groups.append((jj * 128, 128, 1, mm_))
                                exps.append((jj * 128, 128, bcol[:, h, j:j + 1], [("bcol", h)]))
                                pv.append((jj * 128, 128, Vp[:, j, h, :], oi, oik, j == 0, j == i, ["Vp"]))
                            tasks.append(dict(nrow=128, width=len(js) * 128, groups=groups, exps=exps, pv=pv))
                        self.run_attn(tasks)
                        P.op("dve", lambda e: e.reciprocal(self.rd[:, h:h + 1], oi[:, 64:65]), reads=[oik], writes=[("rd", h)])
                        P.op("dve", lambda e: e.tensor_scalar(Obf[:, 512 + h * 64:512 + (h + 1) * 64], oi[:, 0:64], self.rd[:, h:h + 1], None, ALU.mult),
                             reads=[oik, ("rd", h)], writes=["hbf"])
                    self.out_proj_residual(i, wout)
                P.barrier()

    def rel_tables(self):
        P = self.P
        I = self.I
        with ExitStack() as st:
            tab = self.T(st, "tab", [32, 16], F32)
            ohb = self.T(st, "ohb", [32, 2048], F32)
            fvr = self.T(st, "fvr", [16, 4096], BF16)
            P.dma(tab[:], I["rel_table"], writes=["tab"])
            P.dma(ohb[:], I["k_ohb"], writes=["ohb"])
            P.op("pool", lambda e: e.memset(fvr[:], -BIG), writes=["fvr"])
            for q in range(4):
                self.mm(self.B[1][0:16, :], tab[:], ohb[:, q * 512:(q + 1) * 512], True, True, ["tab", "ohb"], ["B1"])
                P.op("dve", lambda e: e.tensor_scalar(fvr[:, 2048 + q * 512:2048 + (q + 1) * 512], self.B[1][0:16, :], 8.0, None, ALU.mult),
                     reads=["B1"], writes=["fvr"])
            P.dma(self.FV, fvr[:], reads=["fvr"], writes=["FV"])
            P.op("pool", lambda e: e.memset(fvr[:, 2048 + 512:4096], -BIG), reads=["fvr"], writes=["fvr"])
            P.dma(self.FW, fvr[:], reads=["fvr"], writes=["FW"])
            P.barrier()

    def odd_layer(self, l):
        P = self.P
        I = self.I
        li = l // 2
        w_in = I["odd_w_in"][li]
        rd = self.rd
        self.load_mods(l, 0)
        with ExitStack() as lay:
            ksT = self.T(lay, "ksT", [128, 2, S], BF16)
            kwT = self.T(lay, "kwT", [128, 2, S], BF16)
            Vs = self.T(lay, "Vs", [128, NT, 4, 65], BF16)
            Vw = self.T(lay, "Vw", [128, NT, 4, 65], BF16)
            KcT = self.T(lay, "KcT", [128, 2, 128], BF16)
            VcX = self.T(lay, "VcX", [128, 4, 97], BF16)
            gbs = self.T(lay, "gbs", [128, 4, 64], F32)
            sq = self.T(lay, "sq", [128, 1024], F32)
            rs = self.T(lay, "rs", [128, 64], F32)
            D0 = self.T(lay, "D0", [128, 16, 128], BF16)
            D1 = self.T(lay, "D1", [128, 16, 128], BF16)
            D4 = self.T(lay, "D4", [128, 16, 128], BF16)
            CROW = self.T(lay, "CROW", [1, 16, 128], BF16)
            for (t, nm, src, k) in ((D0, "D0", self.FV, 0), (D1, "D1", self.FV, 1), (D4, "D4", self.FW, 4)):
                ap = bass.AP(tensor=src.tensor, offset=2048 + 128 * k - 127, ap=[[1, 128], [4096, 16], [1, 128]])
                P.dma(t[:], ap, writes=[nm])
            ap = bass.AP(tensor=self.FV.tensor, offset=2048 + 1000, ap=[[0, 1], [4096, 16], [1, 128]])
            P.dma(CROW[:], ap, writes=["CROW"])
            P.dma(gbs[:, 0, :], I["odd_q_g"][li:li + 1, :].broadcast_to([128, 64]), writes=[("gbs", 0)])
            P.dma(gbs[:, 1:4, :].rearrange("p k d -> p (k d)"), I["odd_k_g"][li:li + 1, :].broadcast_to([128, 192]), writes=[("gbs", 1)])
            P.op("pool", lambda e: e.memset(Vs[:], 1.0), writes=["Vs"])
            P.op("pool", lambda e: e.memset(Vw[:], 1.0), writes=["Vw"])
            P.op("pool", lambda e: e.memset(VcX[:], 1.0), writes=["VcX"])
            with ExitStack() as ph:
                wkv = self.T(ph, "wkv", [128, 8, 1536], BF16)
                kcT = self.T(ph, "kcT", [128, 2, S], BF16)
                vcT = self.T(ph, "vcT", [128, 2, S], BF16)
                kvb = self.T(ph, "kvb", [128, 4, 256], BF16)
                w1b = [self.T(ph, "w1b%d" % a, [128, 32, 64], BF16) for a in range(2)]
                w2b = [self.T(ph, "w2b%d" % a, [64, 64], BF16) for a in range(2)]
                pos = self.T(ph, "pos", [32, 2, 64], F32)
                posT = self.T(ph, "posT", [64, 2, 32], BF16)
                cst = self.T(ph, "cst", [64, 2], F32)
                HT = self.T(ph, "HT", [64, 128], BF16)
                KcN = self.T(ph, "KcN", [128, 4, 64], BF16)
                ovl = self.T(ph, "ovl", [127, 32], F32)
                P.dma(wkv[:], w_in[:, 1024:2560].rearrange("(c p) n -> p c n", p=128), writes=["wkv"], q="pool")
                for a in range(2):
                    for hf in range(2):
                        P.dma(w1b[a][hf * 64:(hf + 1) * 64, :, :], I["odd_cmp_w1"][li, a].rearrange("(l d) o -> d l o", d=64),
                              writes=[("w1b%d" % a, hf)], q="pool")
                    P.dma(w2b[a][:], I["odd_cmp_w2"][li, a], writes=["w2b%d" % a], q="pool")
                    P.dma(pos[:, a, :], I["odd_cmp_pos"][li, a], writes=[("pos", a)])
                P.dma(ovl[:], I["k_ovl"], writes=["ovl"])
                P.op("dve", lambda e: e.tensor_copy(VcX[0:127, :, 65:97], ovl[:].unsqueeze(1).broadcast_to([127, 4, 32])),
                     reads=["ovl", "VcX"], writes=["VcX"])
                for tb in range(NT):
                    self.peer_convert_step(l, tb)
                    self.norm_hT(tb)
                    self.proj(1, 512, wkv, "wkv", 0)
                    self.proj(2, 512, wkv, "wkv", 512)
                    self.proj(3, 512, wkv, "wkv", 1024)
                    P.op("act", lambda e: e.copy(kvb[:, 0:2, :], self.B[1][:, :].rearrange("p (a n) -> p a n", a=2)), reads=["B1"], writes=[("kvb", 0)])
                    self.head_rmsnorm(self.B[2][:, 0:256], "B2", 4, gbs[:, 2, :], ("gbs", 1), kvb[:, 2, :].rearrange("p (h d) -> p h d", d=64), ("kvb", 2), sq, rs)
                    self.head_rmsnorm(self.B[3][:, 0:256], "B3", 4, gbs[:, 3, :], ("gbs", 1), kvb[:, 3, :].rearrange("p (h d) -> p h d", d=64), ("kvb", 3), sq, rs)
                    P.op("act", lambda e: e.copy(Vs[:, tb, :, 0:64], self.B[2][:, 256:512].rearrange("p (h d) -> p h d", d=64)), reads=["B2"], writes=[("Vs", tb)])
                    P.op("act", lambda e: e.copy(Vw[:, tb, :, 0:64], self.B[3][:, 256:512].rearrange("p (h d) -> p h d", d=64)), reads=["B3"], writes=[("Vw", tb)])
                    b4 = self.bank_bf(4)
                    for a in range(4):
                        for c in range(2):
                            self.tr(b4[:, (a * 2 + c) * 128:(a * 2 + c + 1) * 128], kvb[:, a, c * 128:(c + 1) * 128], self.identb[:],
                                    ["kvb", "identb"], [("B4", a * 2 + c)])
                    for a, (dst, nm) in enumerate(((kcT, "kcT"), (vcT, "vcT"), (ksT, "ksT"), (kwT, "kwT"))):
                        P.op("act", lambda e: e.copy(dst[:, :, tb * 128:(tb + 1) * 128], b4[:, a * 256:(a + 1) * 256].rearrange("p (c t) -> p c t", c=2)),
                             reads=["B4"], writes=[(nm, tb)])
                for a in range(2):
                    self.tr(self.B[1][0:64, 0:32], pos[:, a, :], self.identf[0:32, 0:32], ["pos", "identf"], ["B1"])
                    P.op("act", lambda e: e.copy(posT[:, a, :], self.B[1][0:64, 0:32]), reads=["B1"], writes=[("posT", a)])
                    for lq in range(32):
                        self.mm(self.B[2][0:64, 0:1], w1b[a][0:64, lq, :], posT[:, a, lq:lq + 1], lq == 0, lq == 31, ["w1b%d" % a, ("posT", a)], ["B2"])
                    P.op("dve", lambda e: e.tensor_copy(cst[:, a:a + 1], self.B[2][0:64, 0:1]), reads=["B2"], writes=[("cst", a)])
                for a, srcT in enumerate((kcT, vcT)):
                    for gq in range(4):
                        base = (gq % 2) * 64
                        ch = gq // 2
                        for lq in range(32):
                            rhs = srcT[base:base + 64, ch, lq:lq + 16 * 126 + 1:16]
                            self.mm(self.B[5][0:64, 0:127], w1b[a][base:base + 64, lq, :], rhs, lq == 0, lq == 31,
                                    ["w1b%d" % a, "kcT", "vcT"], ["B5"])
                        P.op("act", lambda e: e.activation(HT[:, 0:127], self.B[5][0:64, 0:127], AF.Gelu_apprx_tanh, bias=cst[:, a:a + 1], scale=1.0),
                             reads=["B5", ("cst", a)], writes=["HT"])
                        self.mm(self.B[6][0:127, 0:64], HT[:, 0:127], w2b[a][:], True, True, ["HT", "w2b%d" % a], ["B6"])
                        if a == 0:
                            self.head_rmsnorm(self.B[6][0:127, 0:64], "B6", 1, gbs[0:127, 1, :], ("gbs", 1), KcN[0:127, gq:gq + 1, :], ("KcN", gq),
                                              sq[0:127], rs[0:127], npart=127)
                        else:
                            P.op("act", lambda e: e.copy(VcX[0:127, gq, 0:64], self.B[6][0:127, 0:64]), reads=["B6"], writes=[("VcX", gq)])
                b4 = self.bank_bf(4)
                for c in range(2):
                    self.tr(b4[:, c * 128:c * 128 + 127], KcN[0:127, 2 * c:2 * c + 2, :].rearrange("p g d -> p (g d)"), self.identb[0:127, 0:127],
                            ["KcN", "identb"], [("B4", c)])
                    P.op("act", lambda e: e.copy(KcT[:, c, 0:127], b4[:, c * 128:c * 128 + 127]), reads=[("B4", c)], writes=[("KcT", c)])
                P.barrier()
            with ExitStack() as ph:
                wq = self.T(ph, "wq", [128, 8, 1072], BF16)
                wout = self.T(ph, "wout", [128, 8, D], BF16)
                bgb = self.T(ph, "bgb", [128, 48], F32)
                gates = self.T(ph, "gates", [128, 48], F32)
                a1 = self.T(ph, "a1", [128, 16, 32], BF16)
                a0 = self.T(ph, "a0", [128, 16, 32], BF16)
                ej = self.T(ph, "ej", [32, 16, 128], BF16)
                qn = self.T(ph, "qn", [128, D], BF16)
                qT = self.T(ph, "qT", [128, 8, 128], BF16)
                Of = self.ntmp
                imp = self.T(ph, "imp", [128, 4, 32], F32)
                impw = self.T(ph, "impw", [128, 4, 32], F32)
                nmk = self.T(ph, "nmk", [128, 4, 32], BF16)
                NMT = self.T(ph, "NMT", [32, 4, 128], BF16)
                bci = self.T(ph, "bci", [127, 16, 128], BF16)
                self.OT = self.T(ph, "OT", [128, 8, 128], BF16)
                self.PT = [self.T(ph, "PT%d" % k, [128, 512], BF16) for k in range(3)]
                P.dma(wq[:, :, 0:1024], w_in[:, 0:1024].rearrange("(c p) n -> p c n", p=128), writes=[("wq", 0)], q="pool")
                P.dma(wq[:, :, 1024:1072], w_in[:, 2560:2608].rearrange("(c p) n -> p c n", p=128), writes=[("wq", 1)], q="pool")
                P.dma(wout[:], I["odd_w_out"][li].rearrange("(c p) n -> p c n", p=128), writes=["wout"], q="pool")
                P.dma(bgb[:], I["odd_b_gate"][li:li + 1, :].broadcast_to([128, 48]), writes=["bgb"])
                P.dma(a1[:], I["k_a1"], writes=["a1"], q="pool")
                P.dma(a0[:], I["k_a0"], writes=["a0"], q="pool")
                P.dma(ej[:], I["k_ej"], writes=["ej"], q="pool")
                for i in range(NT):
                    self.norm_hT(i)
                    self.proj(1, 512, wq, "wq", 0)
                    self.proj(2, 512, wq, "wq", 512)
                    self.proj(3, 48, wq, "wq", 1024)
                    P.op("dve", lambda e: e.tensor_tensor(gates[:], self.B[3][:, 0:48], bgb[:], ALU.add), reads=["B3", "bgb"], writes=["gates"])
                    P.op("act", lambda e: e.activation(gates[:], gates[:], AF.Sigmoid), reads=["gates"], writes=["gates"])
                    for hb in range(2):
                        src = self.B[1 + hb][:, :]
                        sk = "B%d" % (1 + hb)
                        P.op("act", lambda e: e.activation(sq[:, 0:512], src, AF.Square), reads=[sk], writes=["sq"])
                        P.op("dve", lambda e: e.tensor_reduce(rs[:, 0:8], sq[:, 0:512].rearrange("p (h d) -> p h d", d=64), AX.X, ALU.add), reads=["sq"], writes=[("rs", 0)])
                        P.op("dve", lambda e: e.tensor_scalar(rs[:, 16:24], rs[:, 0:8], 1.0 / 64, 1e-6, ALU.mult, ALU.add), reads=[("rs", 0)], writes=[("rs", 1)])
                        P.op("act", lambda e: e.activation(rs[:, 32:40], rs[:, 16:24], AF.Sqrt), reads=[("rs", 1)], writes=[("rs", 2)])
                        P.op("dve", lambda e: e.reciprocal(rs[:, 48:56], rs[:, 32:40]), reads=[("rs", 2)], writes=[("rs", 3)])
                        P.op("dve", lambda e: e.tensor_tensor(sq[:, 0:512].rearrange("p (h d) -> p h d", d=64), src.rearrange("p (h d) -> p h d", d=64),
                                                              rs[:, 48:56].unsqueeze(2).broadcast_to([128, 8, 64]), ALU.mult),
                             reads=[sk, ("rs", 3), "sq"], writes=["sq"])
                        for gh in range(2):
                            dst = qn[:, hb * 512:(hb + 1) * 512].rearrange("p (j gh d) -> p gh j d", j=4, gh=2)[:, gh, :, :]
                            srcv = sq[:, gh * 256:(gh + 1) * 256].rearrange("p (j d) -> p j d", j=4)
                            P.op("pool", lambda e: e.tensor_tensor(dst, srcv, gbs[:, 0, :].unsqueeze(1).broadcast_to([128, 4, 64]), ALU.mult),
                                 reads=["sq", ("gbs", 0)], writes=[("qn", hb, gh)])
                    b4 = self.bank_bf(4)
                    for c in range(8):
                        self.tr(b4[:, c * 128:(c + 1) * 128], qn[:, c * 128:(c + 1) * 128], self.identb[:], ["qn", "identb"], [("B4", c)])
                    P.op("act", lambda e: e.copy(qT[:], b4[:, 0:1024].rearrange("p (c t) -> p c t", c=8)), reads=["B4"], writes=["qT"])
                    ap = bass.AP(tensor=self.FV.tensor, offset=2048 + 128 * i - 2047, ap=[[16, 127], [4096, 16], [1, 128]])
                    P.dma(bci[:], ap, writes=["bci"])
                    oi3 = self.B[7][:, 0:388].rearrange("p (a b) -> p a b", a=4)
                    for gq in range(4):
                        base = (gq % 2) * 64
                        cq0 = (gq // 2) * 4
                        mms = [(KcT[base:base + 64, gq // 2, 0:127], qT[base:base + 64, cq0:cq0 + 4, :], ["KcT", "qT"]),
                               (self.anti127[0:127, 0:127], bci[:, 4 * gq:4 * gq + 4, :], ["anti127", "bci"])]
                        pv = [(jh * 128, 128, VcX[0:127, gq, :], oi3[:, jh, :], "B7", True, True, ["VcX"]) for jh in range(4)]
                        self.run_attn([dict(nrow=127, width=512, groups=[(0, 512, 4, mms)], pv=pv)])
                        P.op("dve", lambda e: e.tensor_scalar(rd[:, 0:4], oi3[:, :, 64], 1e-30, None, ALU.max), reads=["B7"], writes=[("rd", "den")])
                        P.op("dve", lambda e: e.reciprocal(rd[:, 4:8], rd[:, 0:4]), reads=[("rd", "den")], writes=[("rd", "rden")])
                        gsl = gates[:, 12 * gq:12 * gq + 12].rearrange("p (j b) -> p j b", b=3)
                        P.op("dve", lambda e: e.tensor_tensor(rd[:, 8:12], rd[:, 4:8], gsl[:, :, 0], ALU.mult), reads=[("rd", "rden"), "gates"], writes=[("rd", "gr")])
                        P.op("dve", lambda e: e.tensor_tensor(Of[:, gq * 256:(gq + 1) * 256].rearrange("p (j d) -> p j d", j=4), oi3[:, :, 0:64],
                                                              rd[:, 8:12].unsqueeze(2).broadcast_to([128, 4, 64]), ALU.mult),
                             reads=["B7", ("rd", "gr")], writes=["ntmp"])
                        P.op("dve", lambda e: e.tensor_tensor(impw[:], oi3[:, :, 65:97], rd[:, 4:8].unsqueeze(2).broadcast_to([128, 4, 32]), ALU.mult),
                             reads=["B7", ("rd", "rden")], writes=["impw"])
                        P.op("dve", lambda e: e.tensor_reduce(imp[:, gq, :], impw[:].rearrange("p j m -> p m j"), AX.X, ALU.add), reads=["impw"], writes=[("imp", gq)])
                    P.op("dve", lambda e: e.tensor_tensor(imp[:], imp[:], a1[:, i:i + 1, :].broadcast_to([128, 4, 32]), ALU.mult), reads=["imp", "a1"], writes=["imp"])
                    P.op("dve", lambda e: e.tensor_tensor(imp[:], imp[:], a0[:, i:i + 1, :].broadcast_to([128, 4, 32]), ALU.add), reads=["imp", "a0"], writes=["imp"])
                    for gq in range(4):
                        P.op("dve", lambda e: e.max(rd[:, 12:20], imp[:, gq, :]), reads=["imp"], writes=[("rd", "m8a")])
                        P.op("dve", lambda e: e.match_replace(impw[:, gq, :], rd[:, 12:20], imp[:, gq, :], -1e30), reads=["imp", ("rd", "m8a")], writes=["impw"])
                        P.op("dve", lambda e: e.max(rd[:, 20:28], impw[:, gq, :]), reads=["impw"], writes=[("rd", "m8b")])
                        P.op("dve", lambda e: e.tensor_scalar(rd[:, 28:29], rd[:, 27:28], 0.0, None, ALU.max), reads=[("rd", "m8b")], writes=[("rd", "thr")])
                        P.op("dve", lambda e: e.tensor_scalar(impw[:, gq, :], imp[:, gq, :], rd[:, 28:29], 1.0, ALU.is_ge, ALU.subtract),
                             reads=["imp", ("rd", "thr"), "impw"], writes=["impw"])
                        P.op("dve", lambda e: e.tensor_scalar(nmk[:, gq, :], impw[:, gq, :], BIG, None, ALU.mult), reads=["impw"], writes=[("nmk", gq)])
                    b3 = self.bank_bf(3)
                    for gq in range(4):
                        self.tr(b3[0:32, 512 + gq * 128:512 + (gq + 1) * 128], nmk[:, gq, :], self.identb[:], ["nmk", "identb"], [("B3", gq)])
                    P.op("act", lambda e: e.copy(NMT[:], b3[0:32, 512:1024].rearrange("p (g t) -> p g t", g=4)), reads=["B3"], writes=["NMT"])
                    oi4 = self.B[7][:, 0:260].rearrange("p (a b) -> p a b", a=4)
                    for br, (kTt, knm, Vt, vnm, jlo) in enumerate(((ksT, "ksT", Vs, "Vs", 0), (kwT, "kwT", Vw, "Vw", max(0, i - 4)))):
                        for gq in range(4):
                            base = (gq % 2) * 64
                            cq0 = (gq // 2) * 4
                            tasks = []
                            for j in range(jlo, i + 1):
                                dl = i - j
                                mms = [(kTt[base:base + 64, gq // 2, j * 128:(j + 1) * 128], qT[base:base + 64, cq0:cq0 + 4, :], [knm, "qT"])]
                                if br == 0:
                                    mms.append((ej[:, j, :], NMT[:, gq:gq + 1, :].broadcast_to([32, 4, 128]), ["ej", "NMT"]))
                                if dl == 0:
                                    mms.append((self.antib[:], D0[:, 4 * gq:4 * gq + 4, :], ["antib", "D0"]))
                                elif dl == 1:
                                    mms.append((self.antib[:], D1[:, 4 * gq:4 * gq + 4, :], ["antib", "D1"]))
                                elif dl == 4 and br == 1:
                                    mms.append((self.antib[:], D4[:, 4 * gq:4 * gq + 4, :], ["antib", "D4"]))
                                else:
                                    mms.append((self.ones_row[:], CROW[0:1, 4 * gq:4 * gq + 4, :], ["ones_row", "CROW"]))
                                pv = [(jh * 128, 128, Vt[:, j, gq, :], oi4[:, jh, :], "B7", (j == jlo and jh == 0), j == i, [vnm]) for jh in range(4)]
                                tasks.append(dict(nrow=128, width=512, groups=[(0, 512, 4, mms)], pv=pv))
                            self.run_attn(tasks)
                            gsl = gates[:, 12 * gq:12 * gq + 12].rearrange("p (j b) -> p j b", b=3)
                            P.op("dve", lambda e: e.reciprocal(rd[:, 4:8], oi4[:, :, 64]), reads=["B7"], writes=[("rd", "rden")])
                            P.op("dve", lambda e: e.tensor_tensor(rd[:, 8:12], rd[:, 4:8], gsl[:, :, 1 + br], ALU.mult), reads=[("rd", "rden"), "gates"], writes=[("rd", "gr")])
                            P.op("dve", lambda e: e.tensor_tensor(sq[:, 0:256].rearrange("p (j d) -> p j d", j=4), oi4[:, :, 0:64],
                                                                  rd[:, 8:12].unsqueeze(2).broadcast_to([128, 4, 64]), ALU.mult),
                                 reads=["B7", ("rd", "gr")], writes=["sq"])
                            P.op("pool", lambda e: e.tensor_tensor(Of[:, gq * 256:(gq + 1) * 256], Of[:, gq * 256:(gq + 1) * 256], sq[:, 0:256], ALU.add),
                                 reads=["sq", "ntmp"], writes=["ntmp"])
                    P.op("act", lambda e: e.copy(self.hbf[:], Of[:]), reads=["ntmp"], writes=["hbf"])
                    self.out_proj_residual(i, wout)
                P.barrier()

    def peer_convert_step(self, l, k):
        if not self.do_peer:
            return
        P = self.P
        I = self.I
        for et in (2 * k, 2 * k + 1):
            P.dma(self.UB[et], I["peer_ut"][l, :, et * 512:(et + 1) * 512].rearrange("(c p) e -> p c e", p=128), writes=[("UB", et)], q="conv")
            P.dma(self.VB[et], I["peer_v"][l, et * 512:(et + 1) * 512, :].rearrange("(c p) d -> p c d", p=128), writes=[("VB", et)], q="conv")

    def peer_layer(self, l):
        P = self.P
        I = self.I
        self.load_mods(l, 1)
        with ExitStack() as ph:
            wq = self.T(ph, "wq", [128, 8, D], BF16)
            kin = self.T(ph, "kin", [128, 2, 128], F32)
            kbd = self.T(ph, "kbd", [128, 256], BF16)
            qTp = self.T(ph, "qTp", [128, 8, 128], BF16)
            sc = self.T(ph, "sc", [128, 256], F32)
            scr = self.T(ph, "scr", [128, 256], F32)
            tk = self.T(ph, "tk", [128, 8, 64], F32)
            e01 = self.T(ph, "e01", [128, 8, 256], F32)
            prod = [self.T(ph, "prod%d" % k, [128, 512], F32) for k in range(3)]
            tmp2 = [self.T(ph, "tmp2%d" % k, [128, 512], BF16) for k in range(16)]
            ub = [self.T(ph, "ub%d" % k, [128, 8, 512], BF16) for k in range(2)]
            vb = [self.T(ph, "vb%d" % k, [128, 4, D], BF16) for k in range(2)]
            gA = [self.T(ph, "gA%d" % k, [128, 512], BF16) for k in range(2)]
            G = [self.T(ph, "G%d" % k, [128, 512], BF16) for k in range(2)]
            GT = [self.T(ph, "GT%d" % k, [128, 4, 128], BF16) for k in range(2)]
            P.dma(wq[:], I["peer_w_q"][l].rearrange("(c p) n -> p c n", p=128), writes=["wq"], q="pool")
            P.op("pool", lambda e: e.memset(kin[:], 0.0), writes=["kin"])
            P.dma(kin[:, 0, 0:64], I["peer_keys"][l, 0], reads=["kin"], writes=[("kin", 0)])
            P.dma(kin[:, 1, 64:128], I["peer_keys"][l, 1], reads=["kin"], writes=[("kin", 1)])
            for pq in range(2):
                self.tr(self.B[1][:, pq * 128:(pq + 1) * 128], kin[:, pq, :], self.identf[:], ["kin", "identf"], [("B1", pq)])
            P.op("act", lambda e: e.copy(kbd[:], self.B[1][:, 0:256]), reads=["B1"], writes=["kbd"])
            n_et = 0
            for tb in range(NT):
                self.norm_hT(tb)
                for h in range(8):
                    bk = 1 + h // 4
                    for dc in range(8):
                        self.mm(self.B[bk][:, (h % 4) * 128:(h % 4 + 1) * 128], wq[:, dc, h * 128:(h + 1) * 128], self.hT[:, dc, :], dc == 0, dc == 7,
                                ["wq", "hT"], [("B%d" % bk, h % 4)])
                for bk in (1, 2):
                    P.op("act", lambda e: e.copy(qTp[:, (bk - 1) * 4:(bk - 1) * 4 + 4, :], self.B[bk][:, :].rearrange("p (h t) -> p h t", h=4)),
                         reads=["B%d" % bk], writes=[("qTp", bk)])
                for h in range(8):
                    self.mm(self.B[3][:, (h % 2) * 256:(h % 2 + 1) * 256], qTp[:, h, :], kbd[:], True, True, ["qTp", "kbd"], [("B3", h % 2)])
                    P.op("act", lambda e: e.copy(sc[:], self.B[3][:, (h % 2) * 256:(h % 2 + 1) * 256]), reads=[("B3", h % 2)], writes=["sc"])
                    for pq in range(2):
                        s_ = sc[:, pq * 128:(pq + 1) * 128]
                        o = pq * 16
                        P.op("dve", lambda e: e.max(tk[:, h, o:o + 8], s_), reads=["sc"], writes=[("tk", h, pq)])
                        P.op("dve", lambda e: e.match_replace(scr[:, 0:128], tk[:, h, o:o + 8], s_, -1e30), reads=["sc", ("tk", h, pq)], writes=["scr"])
                        P.op("dve", lambda e: e.max(tk[:, h, o + 8:o + 16], scr[:, 0:128]), reads=["scr"], writes=[("tk", h, pq)])
                    cand = scr[:, 0:256].rearrange("p (a b) -> p a b", a=16)
                    P.op("dve", lambda e: e.tensor_tensor(cand, tk[:, h, 0:16].unsqueeze(2).broadcast_to([128, 16, 16]),
                                                          tk[:, h, 16:32].unsqueeze(1).broadcast_to([128, 16, 16]), ALU.add),
                         reads=[("tk", h)], writes=["scr"])
                    P.op("dve", lambda e: e.max(tk[:, h, 32:40], scr[:, 0:256]), reads=["scr"], writes=[("tk", h, 2)])
                    P.op("dve", lambda e: e.match_replace(scr[:, 0:256], tk[:, h, 32:40], scr[:, 0:256], -1e30), reads=["scr", ("tk", h, 2)], writes=["scr"])
                    P.op("dve", lambda e: e.max(tk[:, h, 40:48], scr[:, 0:256]), reads=["scr"], writes=[("tk", h, 2)])
                    P.op("dve", lambda e: e.tensor_scalar(tk[:, h, 48:49], tk[:, h, 32:33], -1.0, None, ALU.mult), reads=[("tk", h, 2)], writes=[("tk", h, 3)])
                    P.op("act", lambda e: e.activation(scr[:, 0:16], tk[:, h, 32:48], AF.Exp, bias=tk[:, h, 48:49], scale=1.0, accum_out=tk[:, h, 49:50]),
                         reads=[("tk", h, 2), ("tk", h, 3), "scr"], writes=["scr", ("tk", h, 4)])
                    P.op("dve", lambda e: e.reciprocal(tk[:, h, 50:51], tk[:, h, 49:50]), reads=[("tk", h, 4)], writes=[("tk", h, 5)])
                    P.op("dve", lambda e: e.tensor_scalar(tk[:, h, 51:52], tk[:, h, 0:1], -1.0, None, ALU.mult), reads=[("tk", h, 0)], writes=[("tk", h, 6)])
                    P.op("dve", lambda e: e.tensor_scalar(tk[:, h, 52:53], tk[:, h, 16:17], -1.0, None, ALU.mult), reads=[("tk", h, 1)], writes=[("tk", h, 7)])
                    P.op("act", lambda e: e.activation(tk[:, h, 54:55], tk[:, h, 47:48], AF.Exp, bias=tk[:, h, 48:49], scale=1.0),
                         reads=[("tk", h, 2), ("tk", h, 3)], writes=[("tk", h, 8)])
                    P.op("dve", lambda e: e.tensor_scalar(tk[:, h, 54:55], tk[:, h, 54:55], tk[:, h, 50:51], 0.9995, ALU.mult, ALU.mult),
                         reads=[("tk", h, 8), ("tk", h, 5)], writes=[("tk", h, 8)])
                    P.op("act", lambda e: e.activation(e01[:, h, 0:128], sc[:, 0:128], AF.Exp, bias=tk[:, h, 51:52], scale=1.0),
                         reads=["sc", ("tk", h, 6)], writes=[("e01", h, 0)])
                    P.op("dve", lambda e: e.tensor_scalar(e01[:, h, 0:128], e01[:, h, 0:128], tk[:, h, 50:51], None, ALU.mult),
                         reads=[("e01", h, 0), ("tk", h, 5)], writes=[("e01", h, 0)])
                    P.op("act", lambda e: e.activation(e01[:, h, 128:256], sc[:, 128:256], AF.Exp, bias=tk[:, h, 52:53], scale=1.0),
                         reads=["sc", ("tk", h, 7)], writes=[("e01", h, 1)])
                def grid(et):
                    for h in range(8):
                        pr = prod[self._npr % 3]
                        pk = "prod%d" % (self._npr % 3)
                        self._npr += 1
                        slot = (et % 2) * 8 + h
                        t2 = tmp2[slot]
                        tk2 = "tmp2%d" % slot
                        if h < 6:
                            for ii in range(4):
                                P.op("act", lambda e: e.activation(pr[:, ii * 128:(ii + 1) * 128], e01[:, h, 128:256], AF.Identity,
                                                                   scale=e01[:, h, et * 4 + ii:et * 4 + ii + 1]),
                                     reads=[("e01", h)], writes=[(pk, ii)])
                        else:
                            P.op("pool", lambda e: e.tensor_tensor(pr[:].rearrange("p (a b) -> p a b", a=4),
                                                                   e01[:, h, et * 4:(et + 1) * 4].unsqueeze(2).broadcast_to([128, 4, 128]),
                                                                   e01[:, h, 128:256].unsqueeze(1).broadcast_to([128, 4, 128]), ALU.mult),
                                 reads=[("e01", h)], writes=[pk])
                        P.op("dve", lambda e: e.scalar_tensor_tensor(t2[:], pr[:], tk[:, h, 54:55], pr[:], ALU.is_ge, ALU.mult),
                             reads=[pk, ("tk", h, 8)], writes=[tk2])

                def amm(et):
                    k2 = et % 2
                    P.dma(ub[k2][:], self.UB[et], reads=[("UB", et)], writes=["ub%d" % k2])
                    P.dma(vb[k2][:], self.VB[et], reads=[("VB", et)], writes=["vb%d" % k2])
                    ab = 4 + k2
                    for dc in range(8):
                        self.mm(self.B[ab][:, :], self.hT[:, dc, :], ub[k2][:, dc, :], dc == 0, dc == 7, ["hT", "ub%d" % k2], ["B%d" % ab])
                    P.op("act", lambda e: e.activation(gA[k2][:], self.B[ab][:, :], AF.Gelu_apprx_tanh), reads=["B%d" % ab], writes=["gA%d" % k2])
                    wb = 1 + k2
                    for h in range(8):
                        slot = k2 * 8 + h
                        self.mm(self.B[wb][:, :], self.identb[:], tmp2[slot][:], h == 0, h == 7, ["identb", "tmp2%d" % slot], ["B%d" % wb])

                def gmul(et):
                    k2 = et % 2
                    P.op("dve", lambda e: e.tensor_tensor(G[k2][:], gA[k2][:], self.B[1 + k2][:, :], ALU.mult),
                         reads=["gA%d" % k2, "B%d" % (1 + k2)], writes=["G%d" % k2])

                def ymm(et):
                    k2 = et % 2
                    b0 = self.bank_bf(0)
                    for c in range(4):
                        self.tr(b0[:, k2 * 512 + c * 128:k2 * 512 + (c + 1) * 128], G[k2][:, c * 128:(c + 1) * 128], self.identb[:],
                                ["G%d" % k2, "identb"], [("B0", k2 * 4 + c)])
                    P.op("act", lambda e: e.copy(GT[k2][:], b0[:, k2 * 512:(k2 + 1) * 512].rearrange("p (c t) -> p c t", c=4)),
                         reads=[("B0", k2 * 4), ("B0", k2 * 4 + 1), ("B0", k2 * 4 + 2), ("B0", k2 * 4 + 3)], writes=["GT%d" % k2])
                    for c in range(4):
                        for half in range(2):
                            self.mm(self.B[6 + half][:, :], GT[k2][:, c, :], vb[k2][:, c, half * 512:(half + 1) * 512],
                                    et == 0 and c == 0, et == 31 and c == 3, ["GT%d" % k2, "vb%d" % k2], ["B%d" % (6 + half)])

                self._npr = getattr(self, "_npr", 0)
                for k in range(-2, 32):
                    if k >= 0:
                        gmul(k)
                    if k + 2 <= 31:
                        grid(k + 2)
                    if 0 <= k + 1 <= 31:
                        amm(k + 1)
                    if k >= 0:
                        ymm(k)
                for half in range(2):
                    P.op("dve", lambda e: e.tensor_tensor(self.ntmp[:, half * 512:(half + 1) * 512], self.B[6 + half][:, :],
                                                          self.GTB[:, half * 512:(half + 1) * 512], ALU.mult),
                         reads=["B%d" % (6 + half), "GTB"], writes=[("ntmp", half)])
                    P.op("pool", lambda e: e.tensor_tensor(self.X[:, tb, half * 512:(half + 1) * 512], self.X[:, tb, half * 512:(half + 1) * 512],
                                                           self.ntmp[:, half * 512:(half + 1) * 512], ALU.add),
                         reads=[("ntmp", half), ("X", tb)], writes=[("X", tb)])
            P.barrier()


_CACHE = {}


def make_in_maps(inputs):
    consts = host_consts()
    shared = {}
    f = lambda a: np.ascontiguousarray(np.asarray(a, dtype=np.float32))
    shared["ada_w"] = f(inputs["ada_w"])
    shared["ada_b"] = f(inputs["ada_b"]).reshape(1, -1)
    shared["norm_g"] = f(inputs["norm_g"]).reshape(1, -1)
    for k in ("even_w_in", "even_b_f", "even_q_g", "even_k_g", "even_w_out", "odd_w_in", "odd_b_gate", "odd_q_g",
              "odd_cmp_pos", "odd_cmp_w1", "odd_cmp_w2", "odd_w_out", "rel_table", "peer_w_q", "peer_keys", "peer_v"):
        shared[k] = f(inputs[k])
    shared["even_conv_w"] = f(inputs["even_conv_w"]).reshape(2, -1)
    shared["odd_k_g"] = f(inputs["odd_k_g"]).reshape(2, -1)
    shared["peer_ut"] = np.ascontiguousarray(np.transpose(f(inputs["peer_u"]), (0, 2, 1)))
    shared.update(consts)
    x = f(inputs["x"])
    c = f(inputs["c"])
    maps = []
    for b in range(8):
        m = dict(shared)
        m["x"] = x[b]
        m["c"] = c[b:b + 1]
        maps.append(m)
    return maps


def kernel(**inputs):
    if "nc" not in _CACHE:
        _CACHE["nc"] = Builder().build()
    nc = _CACHE["nc"]
    maps = make_in_maps(inputs)
    res = run_bass_kernel_spmd(nc, maps, core_ids=list(range(8)))
    return np.stack([np.asarray(r["y"], dtype=np.float32) for r in res.results], axis=0)
```
